# Optimizing a Trainium2 kernel written in Bass

```python
import jax, jax.numpy as jnp
from jax import lax
import numpy as np

D_MODEL = 1024
BATCH = 4
SEQ = 4096
DEPTH = 1

SB_HEADS = 8
SB_HEAD_DIM = 64
SB_WIDTH = SB_HEADS * SB_HEAD_DIM
Q_BLOCK = 128
CONV_GROUPS = 8
CONV_GROUP_DIM = 64
CONV_WIDTH = CONV_GROUPS * CONV_GROUP_DIM
CONV_KSIZE = 3
N_BRANCHES = 2
N_GROUPS = 4
EXPERTS_PER_GROUP = 8
N_EXPERTS = N_GROUPS * EXPERTS_PER_GROUP
TOP_K_IN_GROUP = 2
D_EXPERT = 256
NORM_EPS = 1e-6
IN_SIZES = (SB_WIDTH, SB_WIDTH, SB_WIDTH, CONV_WIDTH, CONV_WIDTH, CONV_WIDTH, D_MODEL, D_MODEL)
IN_PROJ_WIDTH = sum(IN_SIZES)
IN_SPLITS = tuple(int(s) for s in np.cumsum(IN_SIZES)[:-1])

kernel_name = "hybrid_stickbreak_shortconv_hmoe"


def rms_norm(x, g):
    xf = x.astype(jnp.float32)
    y = xf * lax.rsqrt(jnp.mean(xf * xf, axis=-1, keepdims=True) + NORM_EPS)
    return (y * g.astype(jnp.float32)).astype(x.dtype)


def stick_breaking_attention(q, k, v):
    B, H, S, dh = q.shape
    n_blocks = S // Q_BLOCK
    scale = SB_HEAD_DIM ** -0.5
    kf = k.astype(jnp.float32)
    vf = v.astype(jnp.float32)
    key_pos = jnp.arange(S)

    def one_block(i):
        start = i * Q_BLOCK
        qb = lax.dynamic_slice_in_dim(q, start, Q_BLOCK, axis=2).astype(jnp.float32)
        z = jnp.einsum('bhqd,bhkd->bhqk', qb, kf) * scale
        q_pos = start + jnp.arange(Q_BLOCK)
        mask = key_pos[None, :] < q_pos[:, None]
        neg_log_1m_beta = jnp.where(mask, jax.nn.softplus(z), 0.0)
        tail = lax.cumsum(neg_log_1m_beta, axis=3, reverse=True) - neg_log_1m_beta
        a = jnp.where(mask, jnp.exp(jax.nn.log_sigmoid(z) - tail), 0.0)
        return jnp.einsum('bhqk,bhkd->bhqd', a, vf)

    out = lax.map(one_block, jnp.arange(n_blocks))
    out = jnp.transpose(out, (1, 0, 3, 2, 4)).reshape(B, S, H * dh)
    return out.astype(q.dtype)


def short_conv_mixer(b_gate, c_gate, u, conv_w):
    S = u.shape[1]
    cu = c_gate * u
    cu_pad = jnp.pad(cu, ((0, 0), (CONV_KSIZE - 1, 0), (0, 0)))
    y = conv_w[0] * cu_pad[:, 0:S]
    for j in range(1, CONV_KSIZE):
        y = y + conv_w[j] * cu_pad[:, j:j + S]
    return b_gate * y


def hierarchical_moe(h, w_rg, b_rg, w_re, b_re, w_gate, w_up, w_down):
    N = h.shape[0]
    hf = h.astype(jnp.float32)
    g_logits = hf @ w_rg.astype(jnp.float32) + b_rg.astype(jnp.float32)
    g_prob = jax.nn.softmax(g_logits, axis=-1)
    _, g_idx = lax.top_k(g_logits, 1)
    g_w = jnp.take_along_axis(g_prob, g_idx, axis=-1)
    e_logits = (hf @ w_re.astype(jnp.float32) + b_re.astype(jnp.float32)).reshape(N, N_GROUPS, EXPERTS_PER_GROUP)
    e_in_group = jnp.take_along_axis(e_logits, g_idx[:, :, None], axis=1)[:, 0]
    top_v, top_i = lax.top_k(e_in_group, TOP_K_IN_GROUP)
    top_w = jax.nn.softmax(top_v, axis=-1)
    within = jnp.sum(jax.nn.one_hot(top_i, EXPERTS_PER_GROUP, dtype=jnp.float32) * top_w[..., None], axis=1)
    combine = (jax.nn.one_hot(g_idx[:, 0], N_GROUPS, dtype=jnp.float32)[:, :, None]
               * within[:, None, :] * g_w[:, :, None]).astype(h.dtype)
    y = jnp.zeros_like(h)
    for g in range(N_GROUPS):
        sl = slice(g * EXPERTS_PER_GROUP, (g + 1) * EXPERTS_PER_GROUP)
        act = jax.nn.silu(jnp.einsum('nd,edf->nef', h, w_gate[sl])) * jnp.einsum('nd,edf->nef', h, w_up[sl])
        act = act * combine[:, g, :, None]
        y = y + jnp.einsum('nef,efd->nd', act, w_down[sl])
    return y


def setup_inputs(seed: int = 0) -> dict:
    key = jax.random.key(seed)
    ks = jax.random.split(key, 20)
    f32 = jnp.float32
    nrm = lambda k, shape, fan_in: jax.random.normal(k, shape, f32) * (fan_in ** -0.5)
    gain = lambda k, shape: 1.0 + 0.05 * jax.random.normal(k, shape, f32)
    return {
        "x": jax.random.normal(ks[0], (BATCH, SEQ, D_MODEL), f32),
        "norm_mix_g": gain(ks[1], (DEPTH, D_MODEL)),
        "w_in": nrm(ks[2], (DEPTH, D_MODEL, IN_PROJ_WIDTH), D_MODEL),
        "q_norm_g": gain(ks[3], (DEPTH, SB_HEAD_DIM)),
        "k_norm_g": gain(ks[4], (DEPTH, SB_HEAD_DIM)),
        "conv_w": nrm(ks[5], (DEPTH, CONV_KSIZE, CONV_WIDTH), CONV_KSIZE),
        "w_sb_branch": nrm(ks[6], (DEPTH, SB_WIDTH, D_MODEL), SB_WIDTH),
        "w_conv_branch": nrm(ks[7], (DEPTH, CONV_WIDTH, D_MODEL), CONV_WIDTH),
        "w_out": nrm(ks[8], (DEPTH, D_MODEL, D_MODEL), D_MODEL),
        "norm_ffn_g": gain(ks[9], (DEPTH, D_MODEL)),
        "w_router_group": nrm(ks[10], (DEPTH, D_MODEL, N_GROUPS), D_MODEL),
        "b_router_group": 0.01 * jax.random.normal(ks[11], (DEPTH, N_GROUPS), f32),
        "w_router_expert": nrm(ks[12], (DEPTH, D_MODEL, N_EXPERTS), D_MODEL),
        "b_router_expert": 0.01 * jax.random.normal(ks[13], (DEPTH, N_EXPERTS), f32),
        "w_gate_e": nrm(ks[14], (DEPTH, N_EXPERTS, D_MODEL, D_EXPERT), D_MODEL),
        "w_up_e": nrm(ks[15], (DEPTH, N_EXPERTS, D_MODEL, D_EXPERT), D_MODEL),
        "w_down_e": nrm(ks[16], (DEPTH, N_EXPERTS, D_EXPERT, D_MODEL), D_EXPERT),
    }


def reference(x, norm_mix_g, w_in, q_norm_g, k_norm_g, conv_w, w_sb_branch, w_conv_branch,
              w_out, norm_ffn_g, w_router_group, b_router_group, w_router_expert,
              b_router_expert, w_gate_e, w_up_e, w_down_e):
    B, S, D = x.shape
    for l in range(DEPTH):
        h = rms_norm(x, norm_mix_g[l])
        proj = h @ w_in[l]
        q, k, v, c_b, c_c, c_u, g_a, g_b = jnp.split(proj, IN_SPLITS, axis=-1)
        q = rms_norm(q.reshape(B, S, SB_HEADS, SB_HEAD_DIM), q_norm_g[l]).transpose(0, 2, 1, 3)
        k = rms_norm(k.reshape(B, S, SB_HEADS, SB_HEAD_DIM), k_norm_g[l]).transpose(0, 2, 1, 3)
        v = v.reshape(B, S, SB_HEADS, SB_HEAD_DIM).transpose(0, 2, 1, 3)
        branch_a = stick_breaking_attention(q, k, v) @ w_sb_branch[l]
        branch_b = short_conv_mixer(c_b, c_c, c_u, conv_w[l]) @ w_conv_branch[l]
        merged = jax.nn.sigmoid(g_a) * branch_a + jax.nn.sigmoid(g_b) * branch_b
        x = x + merged @ w_out[l]
        h2 = rms_norm(x, norm_ffn_g[l]).reshape(B * S, D)
        y = hierarchical_moe(h2, w_router_group[l], b_router_group[l], w_router_expert[l],
                             b_router_expert[l], w_gate_e[l], w_up_e[l], w_down_e[l])
        x = x + y.reshape(B, S, D)
    return x
```

```python
import contextlib
import numpy as np
import concourse.bass as bass
import concourse.mybir as mybir
from concourse.bass_utils import run_bass_kernel_spmd

F32 = mybir.dt.float32
BF16 = mybir.dt.bfloat16
AF = mybir.ActivationFunctionType
ALU = mybir.AluOpType
AX = mybir.AxisListType

D = 1024
SEQ = 4096
NOWN = 2048
NE = 32
DE = 256
EPS = 1e-6
CHUNKS = {0: (0, 3, 4, 7), 1: (1, 2, 5, 6)}


class Tok:
    __slots__ = ("sem", "val")

    def __init__(self, sem, val):
        self.sem = sem
        self.val = val


class Res:
    __slots__ = ("w", "r")

    def __init__(self):
        self.w = None
        self.r = []


class Prog:
    ENGS = ("pe", "act", "dve", "pool", "sp")
    SEM_SPLIT = 12000

    def __init__(self, nc):
        self.nc = nc
        self.es = contextlib.ExitStack()
        self.ops = {e: [] for e in self.ENGS}
        self.cnt = {e: 0 for e in self.ENGS}
        self.sems = {e: [] for e in self.ENGS}
        self.dma_sems = {}
        self.dma_cnt = {}
        self.dma_rr = {}
        self.dma_all = []
        self.nsem = 0
        self.gdeps = []

    def new_sem(self, name):
        self.nsem += 1
        return self.es.enter_context(self.nc.semaphore(name))

    def sbuf_at(self, name, shape, dt, off):
        return self.nc.alloc_sbuf_tensor_at(name, list(shape), dt, offset=off)

    def psum(self, name, shape, dt):
        return self.es.enter_context(self.nc.psum_tensor(name, list(shape), dt))

    def _deps(self, reads, writes, extra, nobar=False):
        deps = list(extra)
        if not nobar:
            deps.extend(self.gdeps)
        for r in reads:
            if r.w is not None:
                deps.append(r.w)
        for w in writes:
            if w.w is not None:
                deps.append(w.w)
            deps.extend(w.r)
        return deps

    def _note(self, tok, reads, writes):
        for r in reads:
            r.r.append(tok)
            if len(r.r) > 32:
                r.r = r.r[-32:]
        for w in writes:
            w.w = tok
            w.r = []

    def op(self, eng, fn, reads=(), writes=(), deps=()):
        d = self._deps(reads, writes, deps)
        k = self.cnt[eng]
        si = k // self.SEM_SPLIT
        while len(self.sems[eng]) <= si:
            self.sems[eng].append(self.new_sem(f"c_{eng}_{len(self.sems[eng])}"))
        sem = self.sems[eng][si]
        self.cnt[eng] = k + 1
        tok = Tok(sem, k - si * self.SEM_SPLIT + 1)
        self.ops[eng].append((fn, d, sem, 1, tok.val))
        self._note(tok, reads, writes)
        return tok

    def dma(self, q, out, in_, reads=(), writes=(), deps=(), nsem=6, nobar=False, **kw):
        d = self._deps(reads, writes, deps, nobar)
        if q not in self.dma_sems:
            self.dma_sems[q] = [self.new_sem(f"d_{q}_{i}") for i in range(nsem)]
            self.dma_cnt[q] = [0] * nsem
            self.dma_rr[q] = 0
        i = self.dma_rr[q]
        self.dma_rr[q] = (i + 1) % len(self.dma_sems[q])
        if self.dma_cnt[q][i] >= 3500:
            self.dma_sems[q][i] = self.new_sem(f"d_{q}_{i}_x{self.nsem}")
            self.dma_cnt[q][i] = 0
        self.dma_cnt[q][i] += 1
        sem = self.dma_sems[q][i]
        tok = Tok(sem, 16 * self.dma_cnt[q][i])
        self.dma_all.append(tok)

        def fn(e, out=out, in_=in_, kw=kw):
            return e.dma_start(out=out, in_=in_, **kw)

        self.ops[q].append((fn, d, sem, 16, tok.val))
        self._note(tok, reads, writes)
        return tok

    def begin_expert(self, cnt_ap, deps, k0=0):
        for e in self.ENGS:
            self.ops[e].append(("BEGIN_E", cnt_ap, list(deps), k0))

    def begin_group(self, cnt_ap, deps, thr):
        for e in self.ENGS:
            self.ops[e].append(("BEGIN_G", cnt_ap, list(deps), thr))

    def end_group(self):
        for e in self.ENGS:
            self.ops[e].append(("END_G",))

    def begin_tile(self):
        for e in self.ENGS:
            self.ops[e].append(("TILE",))

    def end_expert(self):
        for e in self.ENGS:
            self.ops[e].append(("END_E",))

    def idma(self, out, out_off, in_, in_off, reads=(), writes=(), deps=()):
        q = "pool"
        d = self._deps(reads, writes, deps)
        if q not in self.dma_sems:
            self.dma_sems[q] = [self.new_sem(f"d_{q}_{i}") for i in range(6)]
            self.dma_cnt[q] = [0] * 6
            self.dma_rr[q] = 0
        i = self.dma_rr[q]
        self.dma_rr[q] = (i + 1) % len(self.dma_sems[q])
        self.dma_cnt[q][i] += 1
        sem = self.dma_sems[q][i]
        tok = Tok(sem, 16 * self.dma_cnt[q][i])

        def fn(e):
            oo = bass.IndirectOffsetOnAxis(ap=out_off, axis=0) if out_off is not None else None
            io = bass.IndirectOffsetOnAxis(ap=in_off, axis=0) if in_off is not None else None
            return e.indirect_dma_start(out=out, out_offset=oo, in_=in_, in_offset=io)
        self.ops[q].append((fn, d, sem, 16, tok.val))
        self._note(tok, reads, writes)
        return tok

    def barrier(self):
        toks = []
        for e in self.ENGS:
            k = self.cnt[e]
            if k > 0:
                si = (k - 1) // self.SEM_SPLIT
                toks.append(Tok(self.sems[e][si], k - si * self.SEM_SPLIT))
        for q in self.dma_sems:
            for s, c in zip(self.dma_sems[q], self.dma_cnt[q]):
                if c > 0:
                    toks.append(Tok(s, 16 * c))
        self.gdeps = toks

    def build(self, final_toks):
        nc = self.nc
        prog = self
        with nc.Block() as block:
            def emit(eng_name, e):
                reg = [None, None]

                def wait_deps(deps, waited):
                    for t in deps:
                        key = id(t.sem)
                        if waited.get(key, 0) < t.val:
                            e.wait_ge(t.sem, t.val)
                            waited[key] = t.val

                def emit_op(item, waited):
                    fn, deps, sem, inc, _val = item
                    wait_deps(deps, waited)
                    ins = fn(e)
                    ins.then_inc(sem, inc)

                def flat_ops(items):
                    return [it for it in items if callable(it[0])]

                def replay(ops_):
                    incs = {}
                    order = []
                    for (_f, _d, sem, inc, val) in ops_:
                        if id(sem) not in incs:
                            incs[id(sem)] = [sem, 0, val - inc]
                            order.append(id(sem))
                        incs[id(sem)][1] += inc
                    for key in order:
                        if incs[key][2] > 0:
                            e.wait_ge(incs[key][0], incs[key][2])
                    for key in order:
                        left = incs[key][1]
                        while left > 0:
                            step = min(left, 400)
                            e.sem_inc(incs[key][0], step)
                            left -= step

                def run(items, waited):
                    i = 0
                    while i < len(items):
                        it = items[i]
                        if it[0] == "BEGIN_G":
                            _, cnt_ap, deps, thr = it
                            depth = 1
                            j = i + 1
                            while True:
                                if items[j][0] == "BEGIN_G":
                                    depth += 1
                                elif items[j][0] == "END_G":
                                    depth -= 1
                                    if depth == 0:
                                        break
                                j += 1
                            inner = items[i + 1:j]
                            i = j + 1
                            ops_ = flat_ops(inner)
                            if not ops_:
                                continue
                            wait_deps(deps, waited)
                            if reg[1] is None:
                                reg[1] = e.alloc_register("gcnt_" + eng_name)
                            e.reg_load(reg[1], cnt_ap)
                            with e.If_lt(reg[1], thr):
                                replay(ops_)
                            with e.Else():
                                run(inner, dict(waited))
                        elif it[0] == "BEGIN_E":
                            _, cnt_ap, deps, k0 = it
                            tiles = []
                            i += 1
                            while items[i][0] != "END_E":
                                if items[i][0] == "TILE":
                                    tiles.append([])
                                else:
                                    tiles[-1].append(items[i])
                                i += 1
                            i += 1
                            if sum(len(t) for t in tiles) == 0:
                                continue
                            wait_deps(deps, waited)
                            if reg[0] is None:
                                reg[0] = e.alloc_register("cnt_" + eng_name)
                            e.reg_load(reg[0], cnt_ap)

                            def rec(k, w_outer):
                                if k == len(tiles):
                                    return
                                with e.If_lt(reg[0], 128 * (k + k0) + 1):
                                    replay([op_ for tl in tiles[k:] for op_ in tl])
                                with e.Else():
                                    w = dict(w_outer)
                                    for op_ in tiles[k]:
                                        emit_op(op_, w)
                                    rec(k + 1, w)
                            rec(0, waited)
                        else:
                            emit_op(it, waited)
                            i += 1

                run(prog.ops[eng_name], {})
                if eng_name == "sp":
                    for t in final_toks:
                        e.wait_ge(t.sem, t.val)

            @block.tensor
            def _(e):
                emit("pe", e)

            @block.scalar
            def _(e):
                emit("act", e)

            @block.vector
            def _(e):
                emit("dve", e)

            @block.gpsimd
            def _(e):
                emit("pool", e)

            @block.sync
            def _(e):
                emit("sp", e)
        self.es.close()


KB = 1024


STATIC_TILES = 1


def build_program(stop_after=None, DEBUG=False):
    nc = bass.Bass("TRN2", target_bir_lowering=False)
    P = Prog(nc)

    def din(name, shape):
        return nc.dram_tensor(name, list(shape), F32, kind="ExternalInput").ap()

    xf = din("xf", [SEQ, D])
    xo = din("xo", [NOWN, D])
    xh = din("xh", [128, D])
    qpos_d = din("qpos", [1, NOWN])
    kpos_d = din("kpos", [128, 32])
    tflag_d = nc.dram_tensor("tflag", [1, 2], mybir.dt.int32, kind="ExternalInput").ap()
    w_in = din("w_in", [D, 5120])
    g1_d = din("g1", [1, D])
    gq_d = din("gq", [1, 64])
    gk_d = din("gk", [1, 64])
    convw_d = din("convw", [3, 512])
    wsb_d = din("wsb", [512, D])
    wcb_d = din("wcb", [512, D])
    wout_d = din("wout", [D, D])
    g2_d = din("g2", [1, D])
    wr_d = din("wr", [D, 36])
    br_d = din("br", [1, 36])
    wge_d = din("wge", [NE, D, DE])
    wue_d = din("wue", [NE, D, DE])
    wde_d = din("wde", [NE, DE, D])
    ident_d = din("ident", [128, 128])
    cst_d = din("cst", [128, 256])
    ebase_d = din("ebase", [1, NE])
    tris_d = din("tris", [128, 128])
    out_d = nc.dram_tensor("out", [NOWN, D], F32, kind="ExternalOutput").ap()
    dbg = {}
    if DEBUG:
        dbg["attnT"] = nc.dram_tensor("dbg_attnT", [128, 4 * NOWN], F32, kind="ExternalOutput").ap()
        dbg["convT"] = nc.dram_tensor("dbg_convT", [128, 4 * NOWN], F32, kind="ExternalOutput").ap()
        dbg["x1"] = nc.dram_tensor("dbg_x1", [NOWN, D], F32, kind="ExternalOutput").ap()
        dbg["comb"] = nc.dram_tensor("dbg_comb", [NOWN, 32], F32, kind="ExternalOutput").ap()

    bank = [P.psum(f"bank{i}", [128, 512], F32) for i in range(8)]
    r_bank = [Res() for _ in range(8)]

    def bank_bf(i):
        return bank[i][:].bitcast(BF16)

    BASE = 16896

    def sb(name, shape, dt, off):
        return P.sbuf_at(name, shape, dt, BASE + off)

    C0 = 192 * KB
    ident = sb("ident", [128, 128], BF16, C0); C0 += 256
    cst = sb("cst", [128, 256], BF16, C0); C0 += 512
    g1t = sb("g1t", [128, D], F32, C0); C0 += 4096
    g2t = sb("g2t", [128, D], F32, C0); C0 += 4096
    gqk = sb("gqk", [128, 64], F32, C0); C0 += 256
    gkt = sb("gkt", [128, 64], F32, C0); C0 += 256
    cw = sb("cw", [128, 3, 4], F32, C0); C0 += 64
    kpos = sb("kpos", [128, 32], F32, C0); C0 += 128
    tflag = sb("tflag", [128, 2], mybir.dt.int32, C0); C0 += 32
    r_tflag = Res()
    epsb = sb("epsb", [128, 2], F32, C0); C0 += 32
    brt = sb("brt", [128, 36], F32, C0); C0 += 160
    small = sb("small", [128, 64], F32, C0); C0 += 256
    rt = sb("rt", [128, 256], F32, C0); C0 += 1024
    assert C0 <= 205 * KB
    r_const = Res()
    r_small = Res()

    kT_all = sb("kT_all", [128, 4, SEQ], BF16, 0)
    V_all = sb("V_all", [128, 32, 512], BF16, 32 * KB)
    qT_all = sb("qT_all", [128, 4, NOWN], BF16, 64 * KB)
    hTo = sb("hTo", [128, 8, NOWN], BF16, 80 * KB)
    convT = sb("convT", [128, 4, NOWN], BF16, 112 * KB)
    attnT = sb("attnT", [128, 4, NOWN], BF16, 128 * KB)
    r_kT = [Res() for _ in range(32)]
    r_V = [Res() for _ in range(32)]
    r_qT = [Res() for _ in range(16)]
    r_hTo = [Res() for _ in range(16)]
    r_convT = [Res() for _ in range(4)]
    r_attnT = [Res() for _ in range(4)]
    W0 = 144 * KB
    X0 = 176 * KB

    negU = cst[:, 0:128]
    negOnes = cst[:, 128:256]

    P.dma("pool", ident[:], ident_d[:, :], writes=[r_const])
    P.dma("pool", cst[:], cst_d[:, :], writes=[r_const])
    P.dma("sp", g1t[:], g1_d.partition_broadcast(128), writes=[r_const])
    P.dma("sp", g2t[:], g2_d.partition_broadcast(128), writes=[r_const])
    P.dma("sp", gqk[:], gq_d.partition_broadcast(128), writes=[r_const])
    P.dma("sp", gkt[:], gk_d.partition_broadcast(128), writes=[r_const])
    for jj in range(3):
        P.dma("sp", cw[:, jj, :], convw_d[jj:jj + 1, :].rearrange("o (c p) -> p (o c)", p=128), writes=[r_const],
              allow_slow_non_contiguous=True)
    P.dma("sp", kpos[:], kpos_d[:, :], writes=[r_const])
    P.dma("sp", tflag[0:1, :], tflag_d[:, :], writes=[r_tflag])
    P.dma("sp", brt[:], br_d.partition_broadcast(128), writes=[r_const])
    P.op("dve", lambda e: e.memset(epsb[:], EPS), writes=[r_const])
    P.op("dve", lambda e: e.scalar_tensor_tensor(out=gqk[:], in0=gqk[:], scalar=0.125, in1=gkt[:],
                                                 op0=ALU.mult, op1=ALU.mult),
         reads=[r_const], writes=[r_const])

    sq_p = [sb("sq0", [128, 512], F32, X0 + 12 * KB), sb("sq1", [128, 512], F32, X0 + 14 * KB)]
    r_sq_p = [Res(), Res()]
    r_small_p = [Res(), Res()]

    def rms_stats(src_ap, r_src, col, n_feat, junk_ap, r_junk, par):
        r_sm = r_small_p[par]
        P.op("act", lambda e: e.activation(out=junk_ap, in_=src_ap, func=AF.Square,
                                           accum_out=small[:, col:col + 1]),
             reads=[r_src], writes=[r_sm, r_junk])
        P.op("act", lambda e: e.activation(out=small[:, col + 1:col + 2], in_=small[:, col:col + 1], func=AF.Ln,
                                           scale=1.0 / n_feat, bias=epsb[:, 0:1]),
             reads=[r_const], writes=[r_sm])
        P.op("act", lambda e: e.activation(out=small[:, col + 2:col + 3], in_=small[:, col + 1:col + 2], func=AF.Exp,
                                           scale=-0.5),
             writes=[r_sm])
        return small[:, col + 2:col + 3]

    def norm_transpose(x_ap, r_x, g_ap, hn_ap, r_hn, pbank, dest_ap, dest_res, par, evac="act"):
        rstd = rms_stats(x_ap, r_x, par * 32, D, hn_ap, r_hn, par)
        P.op("dve", lambda e: e.scalar_tensor_tensor(out=hn_ap, in0=x_ap, scalar=rstd, in1=g_ap,
                                                     op0=ALU.mult, op1=ALU.mult),
             reads=[r_x, r_small_p[par], r_const], writes=[r_hn])
        pT = bank_bf(pbank)

        def tr(e):
            for k in range(8):
                ins = e.transpose(out=pT[:, k * 128:(k + 1) * 128], in_=hn_ap[:, k * 128:(k + 1) * 128],
                                  identity=ident[:])
            return ins
        P.op("pe", tr, reads=[r_hn, r_const], writes=[r_bank[pbank]])
        if evac == "act":
            P.op("act", lambda e: e.copy(out=dest_ap, in_=pT.rearrange("p (k t) -> p k t", k=8)),
                 reads=[r_bank[pbank]], writes=list(dest_res))
        else:
            P.op("dve", lambda e: e.tensor_copy(out=dest_ap, in_=pT.rearrange("p (k t) -> p k t", k=8)),
                 reads=[r_bank[pbank]], writes=list(dest_res))

    def head_norm_a(pb, r_pb, par):
        sq, r_sq, r_sm = sq_p[par], r_sq_p[par], r_small_p[par]
        P.op("act", lambda e: e.activation(out=sq[:], in_=pb, func=AF.Square), reads=[r_pb], writes=[r_sq, r_sm])

    def head_norm_b(par):
        col = par * 32 + 8
        sq, r_sq, r_sm = sq_p[par], r_sq_p[par], r_small_p[par]
        P.op("dve", lambda e: e.tensor_reduce(out=small[:, col:col + 8], in_=sq[:].rearrange("p (h d) -> p h d", h=8),
                                              axis=AX.X, op=ALU.add),
             reads=[r_sq], writes=[r_sm])
        P.op("act", lambda e: e.activation(out=small[:, col:col + 8], in_=small[:, col:col + 8], func=AF.Ln,
                                           scale=1.0 / 64, bias=epsb[:, 0:1]),
             reads=[r_const], writes=[r_sm, r_sq])
        P.op("act", lambda e: e.activation(out=small[:, col + 8:col + 16], in_=small[:, col:col + 8], func=AF.Exp,
                                           scale=-0.5),
             writes=[r_sm])
        return small[:, col + 8:col + 16]

    wkv = sb("wkv", [128, 8, 1024], BF16, 64 * KB)
    r_wkv = Res()
    P.dma("pool", wkv[:], w_in[:, 512:1536].rearrange("(k p) n -> p k n", p=128), writes=[r_wkv])
    wq = sb("wq", [128, 8, 512], BF16, W0)
    wcv = sb("wcv", [128, 8, 1536], BF16, W0 + 8 * KB)
    r_wq = Res()
    r_wcv = Res()
    P.dma("pool", wq[:], w_in[:, 0:512].rearrange("(k p) n -> p k n", p=128), writes=[r_wq])
    P.dma("pool", wcv[:], w_in[:, 1536:3072].rearrange("(k p) n -> p k n", p=128), writes=[r_wcv])
    xb = [sb(f"xb{i}", [128, D], F32, X0 + i * 4 * KB) for i in range(2)]
    r_xb = [Res(), Res()]
    hn_p = [sb("hn0", [128, D], BF16, X0 + 8 * KB), sb("hn1", [128, D], BF16, 80 * KB)]
    r_hn_p = [Res(), Res()]
    hT_p = [sb("hT0", [128, 8, 128], BF16, X0 + 10 * KB), sb("hT1", [128, 8, 128], BF16, 82 * KB)]
    r_hT_p = [Res(), Res()]
    kn_p = [sb("kn0", [128, 512], BF16, 84 * KB), sb("kn1", [128, 512], BF16, 85 * KB)]
    r_kn_p = [Res(), Res()]
    hn, r_hn = hn_p[0], r_hn_p[0]

    def b_s1a(kt):
        par = kt % 2
        P.dma("sp", xb[par][:], xf[kt * 128:(kt + 1) * 128, :], writes=[r_xb[par]])
        rstd = rms_stats(xb[par][:], r_xb[par], par * 32, D, hn_p[par][:], r_hn_p[par], par)
        P.op("dve", lambda e: e.scalar_tensor_tensor(out=hn_p[par][:], in0=xb[par][:], scalar=rstd, in1=g1t[:],
                                                     op0=ALU.mult, op1=ALU.mult),
             reads=[r_xb[par], r_small_p[par], r_const], writes=[r_hn_p[par]])

    def tr8(src, r_src, pbank, dest_ap, dest_res, evac):
        pT = bank_bf(pbank)

        def tr(e):
            for k in range(8):
                ins = e.transpose(out=pT[:, k * 128:(k + 1) * 128], in_=src[:, k * 128:(k + 1) * 128], identity=ident[:])
            return ins
        P.op("pe", tr, reads=[r_src, r_const], writes=[r_bank[pbank]])
        if evac == "act":
            P.op("act", lambda e: e.copy(out=dest_ap, in_=pT.rearrange("p (k t) -> p k t", k=8)),
                 reads=[r_bank[pbank]], writes=list(dest_res))
        else:
            P.op("dve", lambda e: e.tensor_copy(out=dest_ap, in_=pT.rearrange("p (k t) -> p k t", k=8)),
                 reads=[r_bank[pbank]], writes=list(dest_res))

    def b_s1b(kt):
        par = kt % 2
        tr8(hn_p[par], r_hn_p[par], par * 4, hT_p[par][:], [r_hT_p[par]], "act")

    def b_s2a(kt):
        par = kt % 2
        pb = par * 4
        hT, r_hT = hT_p[par], r_hT_p[par]

        def mm_kv(e):
            for half in range(2):
                for k in range(8):
                    ins = e.matmul(bank[pb + 1 + half][:], lhsT=hT[:, k, :], rhs=wkv[:, k, half * 512:(half + 1) * 512],
                                   start=(k == 0), stop=(k == 7))
            return ins
        P.op("pe", mm_kv, reads=[r_hT, r_wkv], writes=[r_bank[pb + 1], r_bank[pb + 2]])
        P.op("dve", lambda e: e.tensor_copy(out=V_all[:, kt, :], in_=bank[pb + 2][:]),
             reads=[r_bank[pb + 2]], writes=[r_V[kt]])
        head_norm_a(bank[pb + 1][:], r_bank[pb + 1], par)

    def b_s2b(kt):
        par = kt % 2
        pb = par * 4
        kn, r_kn = kn_p[par], r_kn_p[par]
        krs = head_norm_b(par)
        P.op("dve", lambda e: e.tensor_tensor(
            out=kn[:].rearrange("p (h d) -> p h d", h=8), in0=bank[pb + 1][:].rearrange("p (h d) -> p h d", h=8),
            in1=krs.unsqueeze(2).to_broadcast([128, 8, 64]), op=ALU.mult),
            reads=[r_bank[pb + 1], r_small_p[par]], writes=[r_kn])

    def b_s2c(kt):
        par = kt % 2
        pb = par * 4
        kn, r_kn = kn_p[par], r_kn_p[par]
        pkT = bank_bf(pb + 3)

        def tr_k(e):
            for a in range(4):
                ins = e.transpose(out=pkT[:, a * 128:(a + 1) * 128], in_=kn[:, a * 128:(a + 1) * 128], identity=ident[:])
            return ins
        P.op("pe", tr_k, reads=[r_kn, r_const], writes=[r_bank[pb + 3]])
        P.op("dve", lambda e: e.tensor_copy(out=kT_all[:, :, kt * 128:(kt + 1) * 128],
                                            in_=pkT[:, 0:512].rearrange("p (a t) -> p a t", a=4)),
             reads=[r_bank[pb + 3]], writes=[r_kT[kt]])

    b_s1a(0)
    b_s1b(0)
    for kt in range(32):
        if kt + 1 < 32:
            b_s1a(kt + 1)
        b_s2a(kt)
        if kt + 1 < 32:
            b_s1b(kt + 1)
        b_s2b(kt)
        if kt >= 1:
            b_s2c(kt - 1)
    b_s2c(31)

    P.barrier()
    qn32 = sb("qn32", [128, 512], F32, X0 + 10 * KB)
    r_qn32 = Res()
    A0 = 128 * KB
    qn = sb("qn", [128, 512], BF16, A0)
    r_qn = Res()
    hTh = sb("hTh", [128, 8, 128], BF16, A0 + 1 * KB)
    r_hTh = Res()
    ccS = sb("ccS", [128, 512], F32, A0 + 3 * KB)
    r_ccS = Res()
    cub = sb("cub", [128, 768], F32, A0 + 5 * KB)
    r_cub = Res()
    y1 = sb("y1", [128, 512], F32, A0 + 8 * KB)
    y2 = sb("y2", [128, 512], F32, A0 + 10 * KB)
    r_y1 = Res()
    r_y2 = Res()
    cuh = sb("cuh", [128, 4, 8], F32, A0 + 12 * KB)
    r_cuh = Res()

    P.dma("sp", xb[0][:], xh[:, :], writes=[r_xb[0]])
    hn_pc = [hn_p[0], sb("hn1c", [128, D], BF16, A0 + 12 * KB + 512)]
    r_hn_pc = [r_hn_p[0], Res()]
    qn_p = [qn, sb("qn1", [128, 512], BF16, A0 + 14 * KB + 512)]
    r_qn_p = [r_qn, Res()]
    norm_transpose(xb[0][:], r_xb[0], g1t[:], hn_pc[0][:], r_hn_pc[0], 0, hTh[:], [r_hTh], 0)
    for c in range(4):
        def mm_h(e, c=c):
            for which in range(2):
                for k in range(8):
                    m = 4 + which * 4 + c
                    ins = e.matmul(bank[1 + which][:, 0:128], lhsT=wcv[:, k, m * 128:(m + 1) * 128], rhs=hTh[:, k, :],
                                   start=(k == 0), stop=(k == 7))
            return ins
        P.op("pe", mm_h, reads=[r_hTh, r_wcv], writes=[r_bank[1], r_bank[2]])
        P.op("act", lambda e: e.copy(out=qn32[:, 0:8], in_=bank[1][:, 0:8]), reads=[r_bank[1]], writes=[r_qn32])
        P.op("dve", lambda e, c=c: e.tensor_tensor(out=cuh[:, c, :], in0=qn32[:, 0:8], in1=bank[2][:, 0:8], op=ALU.mult),
             reads=[r_qn32, r_bank[2]], writes=[r_cuh])

    def c_s1a(ot):
        par = ot % 2
        P.dma("sp", xb[par][:], xo[ot * 128:(ot + 1) * 128, :], writes=[r_xb[par]])
        rstd = rms_stats(xb[par][:], r_xb[par], par * 32, D, hn_pc[par][:], r_hn_pc[par], par)
        P.op("dve", lambda e: e.scalar_tensor_tensor(out=hn_pc[par][:], in0=xb[par][:], scalar=rstd, in1=g1t[:],
                                                     op0=ALU.mult, op1=ALU.mult),
             reads=[r_xb[par], r_small_p[par], r_const], writes=[r_hn_pc[par]])

    def c_s1b(ot):
        par = ot % 2
        tr8(hn_pc[par], r_hn_pc[par], par * 4, hTo[:, :, ot * 128:(ot + 1) * 128], [r_hTo[ot]], "act")

    def c_s2a(ot):
        par = ot % 2
        pb = par * 4

        def mm_q(e):
            for k in range(8):
                ins = e.matmul(bank[pb + 1][:], lhsT=hTo[:, k, ot * 128:(ot + 1) * 128], rhs=wq[:, k, :],
                               start=(k == 0), stop=(k == 7))
            return ins
        P.op("pe", mm_q, reads=[r_hTo[ot], r_wq], writes=[r_bank[pb + 1]])
        head_norm_a(bank[pb + 1][:], r_bank[pb + 1], par)

    def c_s2b(ot):
        par = ot % 2
        pb = par * 4
        qrs = head_norm_b(par)
        qn, r_qn = qn_p[par], r_qn_p[par]
        P.op("dve", lambda e: e.tensor_tensor(
            out=qn32[:].rearrange("p (h d) -> p h d", h=8), in0=bank[pb + 1][:].rearrange("p (h d) -> p h d", h=8),
            in1=qrs.unsqueeze(2).to_broadcast([128, 8, 64]), op=ALU.mult),
            reads=[r_bank[pb + 1], r_small_p[par]], writes=[r_qn32])
        P.op("dve", lambda e: e.tensor_tensor(
            out=qn[:].rearrange("p (h d) -> p h d", h=8), in0=qn32[:].rearrange("p (h d) -> p h d", h=8),
            in1=gqk[:].unsqueeze(1).to_broadcast([128, 8, 64]), op=ALU.mult),
            reads=[r_qn32, r_const], writes=[r_qn])

    def c_s2c(ot):
        par = ot % 2
        pb = par * 4
        qn, r_qn = qn_p[par], r_qn_p[par]
        pqT = bank_bf(pb + 3)

        def tr_q(e):
            for a in range(4):
                ins = e.transpose(out=pqT[:, a * 128:(a + 1) * 128], in_=qn[:, a * 128:(a + 1) * 128], identity=ident[:])
            return ins
        P.op("pe", tr_q, reads=[r_qn, r_const], writes=[r_bank[pb + 3]])
        P.op("dve", lambda e: e.tensor_copy(out=qT_all[:, :, ot * 128:(ot + 1) * 128],
                                            in_=pqT[:, 0:512].rearrange("p (a t) -> p a t", a=4)),
             reads=[r_bank[pb + 3]], writes=[r_qT[ot]])

    def c_conv(j):
        if True:
            cols = slice(j * 512, (j + 1) * 512)
            for c in range(4):
                pbb = 1 + (c % 2) * 4

                def mm_c(e, c=c, pbb=pbb, cols=cols):
                    for which in range(3):
                        m = which * 4 + c
                        for k in range(8):
                            ins = e.matmul(bank[pbb + which][:], lhsT=wcv[:, k, m * 128:(m + 1) * 128], rhs=hTo[:, k, cols],
                                           start=(k == 0), stop=(k == 7))
                    return ins
                P.op("pe", mm_c, reads=[r_hTo[4 * j + i] for i in range(4)] + [r_wcv],
                     writes=[r_bank[pbb], r_bank[pbb + 1], r_bank[pbb + 2]])
                P.op("act", lambda e, pbb=pbb: e.copy(out=ccS[:], in_=bank[pbb + 1][:]), reads=[r_bank[pbb + 1]], writes=[r_ccS])
                P.op("dve", lambda e, pbb=pbb: e.tensor_tensor(out=cub[:, 2:514], in0=ccS[:], in1=bank[pbb + 2][:], op=ALU.mult),
                     reads=[r_ccS, r_bank[pbb + 2]], writes=[r_cub])
                P.op("dve", lambda e, c=c, j=j: e.tensor_copy(out=cub[:, 0:2], in_=cuh[:, c, 2 * j:2 * j + 2]),
                     reads=[r_cuh], writes=[r_cub])
                P.op("dve", lambda e, c=c: e.tensor_scalar(out=y1[:], in0=cub[:, 0:512], scalar1=cw[:, 0, c:c + 1], scalar2=None,
                                                            op0=ALU.mult),
                     reads=[r_cub, r_const], writes=[r_y1])
                P.op("dve", lambda e, c=c: e.scalar_tensor_tensor(out=y2[:], in0=cub[:, 1:513], scalar=cw[:, 1, c:c + 1], in1=y1[:],
                                                                   op0=ALU.mult, op1=ALU.add),
                     reads=[r_cub, r_y1, r_const], writes=[r_y2])
                P.op("dve", lambda e, c=c: e.scalar_tensor_tensor(out=y1[:], in0=cub[:, 2:514], scalar=cw[:, 2, c:c + 1], in1=y2[:],
                                                                   op0=ALU.mult, op1=ALU.add),
                     reads=[r_cub, r_y2, r_const], writes=[r_y1])
                P.op("dve", lambda e, c=c, pbb=pbb, cols=cols: e.tensor_tensor(out=convT[:, c, cols], in0=y1[:], in1=bank[pbb][:],
                                                                               op=ALU.mult),
                     reads=[r_y1, r_bank[pbb]], writes=[r_convT[j]])

    c_s1a(0)
    c_s1b(0)
    for ot in range(16):
        if ot + 1 < 16:
            c_s1a(ot + 1)
        c_s2a(ot)
        if ot + 1 < 16:
            c_s1b(ot + 1)
        c_s2b(ot)
        if ot >= 1:
            c_s2c(ot - 1)
        if ot % 4 == 3:
            c_conv(ot // 4)
    c_s2c(15)

    P.barrier()
    spb = sb("spb", [128, 32, 512], BF16, W0)
    r_sp = [Res() for _ in range(32)]
    masks = sb("masks", [128, 8, 512], BF16, X0)
    r_mask = Res()
    qposb = sb("qposb", [128, 512], F32, X0 + 4 * KB)
    r_qpos = Res()
    NB = 4
    ab = [sb(f"ab{i}", [128, 512], BF16, X0 + 9 * KB + i * KB) for i in range(NB)]
    r_a = [Res() for _ in range(NB)]
    NRB = 4
    Rb = [sb(f"Rb{i}", [128, 512], BF16, X0 + (6 + i) * KB if i < 3 else X0 + 13 * KB) for i in range(NRB)]
    r_R = [Res() for _ in range(NRB)]
    WARM = 1
    r_junk = Res()
    WARM2 = 0

    def gen_attention(Ls):
      for j in range(4):
          L = Ls[j]
          cols = slice(j * 512, (j + 1) * 512)
          P.dma("sp", qposb[:], qpos_d[:, cols].partition_broadcast(128), writes=[r_qpos])
          for i in range(4):
              n = L - 4 + i
              P.op("dve", lambda e, i=i, n=n: e.tensor_scalar(out=masks[:, i, :], in0=qposb[:], scalar1=kpos[:, n:n + 1],
                                                              scalar2=-30000.0, op0=ALU.is_le, op1=ALU.mult),
                   reads=[r_qpos, r_const], writes=[r_mask])
          qres = [r_qT[4 * j + i] for i in range(4)]
          for h in range(8):
              pr = h // 2
              hs = slice((h % 2) * 64, (h % 2) * 64 + 64)
              pO = 4 + (h % 2)
              for it, n in enumerate(range(L - 1, -1, -1)):
                  b = it % NB
                  masked = n >= L - 4

                  def zmm(e, n=n, b=b, hs=hs, pr=pr, cols=cols, masked=masked, i=n - (L - 4)):
                      ins = e.matmul(bank[b][:], lhsT=kT_all[hs, pr, n * 128:(n + 1) * 128], rhs=qT_all[hs, pr, cols],
                                     start=True, stop=not masked)
                      if masked:
                          ins = e.matmul(bank[b][:], lhsT=ident[:], rhs=masks[:, i, :], start=False, stop=True)
                      return ins
                  P.op("pe", zmm, reads=[r_kT[n], r_const] + qres + ([r_mask] if masked else []), writes=[r_bank[b]])
                  if WARM:
                      def warm(e, hs=hs, pr=pr, cols=cols, n=n):
                          for _ in range(WARM):
                              ins = e.matmul(bank[7][:], lhsT=kT_all[hs, pr, n * 128:(n + 1) * 128], rhs=qT_all[hs, pr, cols],
                                             start=True, stop=True)
                          return ins
                      P.op("pe", warm, writes=[r_junk])
                  P.op("act", lambda e, n=n, b=b: e.activation(out=spb[:, n, :], in_=bank[b][:], func=AF.Softplus),
                       reads=[r_bank[b]], writes=[r_sp[n]])
              pend = None
              for it, n in enumerate(range(L - 1, -1, -1)):
                  b = it % NB
                  first = (it == 0)
                  masked = n >= L - 4

                  def cmm(e, n=n, b=b, first=first, rb=it % NRB, hs=hs, pr=pr, cols=cols, masked=masked, i=n - (L - 4)):
                      e.matmul(bank[b][:], lhsT=negU, rhs=spb[:, n, :], start=True, stop=False)
                      if not first:
                          e.matmul(bank[b][:], lhsT=negOnes, rhs=Rb[rb][:], start=False, stop=False)
                      if masked:
                          e.matmul(bank[b][:], lhsT=ident[:], rhs=masks[:, i, :], start=False, stop=False)
                      return e.matmul(bank[b][:], lhsT=kT_all[hs, pr, n * 128:(n + 1) * 128], rhs=qT_all[hs, pr, cols],
                                      start=False, stop=True)
                  P.op("pe", cmm, reads=[r_sp[n], r_const, r_kT[n]] + qres + ([] if first else [r_R[it % NRB]]) +
                       ([r_mask] if masked else []), writes=[r_bank[b]])
                  if pend is not None:
                      P.op("pe", pend[0], reads=pend[1], writes=[r_bank[pO]])
                  if WARM2:
                      def warm2(e, hs=hs, pr=pr, cols=cols, n=n):
                          for _ in range(WARM2):
                              ins = e.matmul(bank[7][:], lhsT=kT_all[hs, pr, n * 128:(n + 1) * 128], rhs=qT_all[hs, pr, cols],
                                             start=True, stop=True)
                          return ins
                      P.op("pe", warm2, writes=[r_junk])
                  ai = it % NB
                  P.op("act", lambda e, b=b, ai=ai: e.activation(out=ab[ai][:], in_=bank[b][:], func=AF.Exp),
                       reads=[r_bank[b]], writes=[r_a[ai]])
                  if n > 0:
                      if first:
                          P.op("dve", lambda e, n=n: e.tensor_copy(out=Rb[1][:], in_=spb[:, n, :]),
                               reads=[r_sp[n]], writes=[r_R[1]])
                      else:
                          P.op("dve", lambda e, n=n, it=it: e.tensor_tensor(out=Rb[(it + 1) % NRB][:], in0=Rb[it % NRB][:],
                                                                            in1=spb[:, n, :], op=ALU.add),
                               reads=[r_sp[n], r_R[it % NRB]], writes=[r_R[(it + 1) % NRB]])
                  kw = {} if h % 2 == 0 else {"tile_position": (0, 64)}
                  pend = ((lambda e, n=n, ai=ai, first=first, last=(n == 0), h=h, hs=hs, pO=pO, kw=kw: e.matmul(
                      bank[pO][hs, :], lhsT=V_all[:, n, h * 64:(h + 1) * 64], rhs=ab[ai][:], start=first, stop=last, **kw)),
                      [r_V[n], r_a[ai]])
              P.op("pe", pend[0], reads=pend[1], writes=[r_bank[pO]])
              P.op("dve", lambda e, hs=hs, pr=pr, cols=cols, pO=pO: e.tensor_copy(out=attnT[hs, pr, cols], in_=bank[pO][hs, :]),
                   reads=[r_bank[pO]], writes=[r_attnT[j]])


    tf_deps = [r_tflag.w]
    P.begin_group(tflag[0:1, 0:1], tf_deps, 1)
    gen_attention((4, 16, 20, 32))
    P.end_group()
    P.begin_group(tflag[0:1, 1:2], tf_deps, 1)
    gen_attention((8, 12, 24, 28))
    P.end_group()

    last = []
    if DEBUG:
        P.barrier()
        stage = sb("stage", [128, 4 * NOWN], F32, 0)
        r_stage = Res()
        P.op("dve", lambda e: e.tensor_copy(out=stage[:], in_=attnT[:].rearrange("p a t -> p (a t)")), writes=[r_stage])
        last.append(P.dma("sp", dbg["attnT"][:, :], stage[:], reads=[r_stage]))
        stage2 = sb("stage2", [128, 4 * NOWN], F32, 32 * KB)
        r_stage2 = Res()
        P.op("dve", lambda e: e.tensor_copy(out=stage2[:], in_=convT[:].rearrange("p a t -> p (a t)")), writes=[r_stage2])
        last.append(P.dma("sp", dbg["convT"][:, :], stage2[:], reads=[r_stage2]))
    if stop_after == "D":
        P.barrier()
        P.build(last + P.gdeps)
        return nc

    P.barrier()
    mT = sb("mT", [128, 8, NOWN], BF16, 0)
    r_mT = [Res() for _ in range(16)]
    wg = sb("wg", [128, 8, 2048], BF16, 32 * KB)
    wsb = sb("wsb", [128, 4, D], BF16, W0)
    wcb = sb("wcb", [128, 4, D], BF16, W0 + 8 * KB)
    r_wg, r_wsb, r_wcb = Res(), Res(), Res()
    P.dma("pool", wsb[:], wsb_d.rearrange("(a p) n -> p a n", p=128), writes=[r_wsb])
    P.dma("pool", wcb[:], wcb_d.rearrange("(a p) n -> p a n", p=128), writes=[r_wcb])
    for half in range(2):
        P.dma("pool", wg[:, :, half * 1024:(half + 1) * 1024],
              w_in[:, 3072 + half * 1024:3072 + (half + 1) * 1024].rearrange("(k p) n -> p k n", p=128), writes=[r_wg])
    wout = sb("wout", [128, 8, D], BF16, 64 * KB)
    r_wout = Res()
    P.dma("pool", wout[:], wout_d.rearrange("(k p) n -> p k n", p=128), writes=[r_wout])
    sgA = [sb(f"sgA{i}", [128, 512], F32, X0 + i * 2 * KB) for i in range(2)]
    sgB = [sb(f"sgB{i}", [128, 512], F32, X0 + 4 * KB + i * 2 * KB) for i in range(2)]
    tA = [sb(f"tA{i}", [128, 512], F32, X0 + 8 * KB + i * 2 * KB) for i in range(2)]
    tB = [sb(f"tB{i}", [128, 512], F32, X0 + 12 * KB + i * 2 * KB) for i in range(2)]
    r_sgA, r_sgB, r_tA, r_tB = [Res(), Res()], [Res(), Res()], [Res(), Res()], [Res(), Res()]
    it = 0
    for dt in range(8):
        dcols = slice(dt * 128, (dt + 1) * 128)
        gacols = slice(dt * 128, (dt + 1) * 128)
        gbcols = slice(1024 + dt * 128, 1024 + (dt + 1) * 128)
        for j in range(4):
            cols = slice(j * 512, (j + 1) * 512)
            s_ = it % 2
            it += 1
            pb = 4 * s_

            def mm_e1(e, pb=pb, dcols=dcols, gacols=gacols, gbcols=gbcols, cols=cols):
                for a in range(4):
                    e.matmul(bank[pb][:], lhsT=wsb[:, a, dcols], rhs=attnT[:, a, cols], start=(a == 0), stop=(a == 3))
                for a in range(4):
                    e.matmul(bank[pb + 1][:], lhsT=wcb[:, a, dcols], rhs=convT[:, a, cols], start=(a == 0), stop=(a == 3))
                for k in range(8):
                    e.matmul(bank[pb + 2][:], lhsT=wg[:, k, gacols], rhs=hTo[:, k, cols], start=(k == 0), stop=(k == 7))
                for k in range(8):
                    ins = e.matmul(bank[pb + 3][:], lhsT=wg[:, k, gbcols], rhs=hTo[:, k, cols], start=(k == 0), stop=(k == 7))
                return ins
            P.op("pe", mm_e1, reads=[r_wsb, r_wcb, r_wg, r_attnT[j], r_convT[j]] + [r_hTo[4 * j + i] for i in range(4)],
                 writes=[r_bank[pb + i] for i in range(4)])
            P.op("act", lambda e, pb=pb, s_=s_: e.activation(out=sgA[s_][:], in_=bank[pb + 2][:], func=AF.Sigmoid),
                 reads=[r_bank[pb + 2]], writes=[r_sgA[s_]])
            P.op("act", lambda e, pb=pb, s_=s_: e.activation(out=sgB[s_][:], in_=bank[pb + 3][:], func=AF.Sigmoid),
                 reads=[r_bank[pb + 3]], writes=[r_sgB[s_]])
            P.op("dve", lambda e, pb=pb, s_=s_: e.tensor_tensor(out=tA[s_][:], in0=sgA[s_][:], in1=bank[pb][:], op=ALU.mult),
                 reads=[r_sgA[s_], r_bank[pb]], writes=[r_tA[s_]])
            P.op("dve", lambda e, pb=pb, s_=s_: e.tensor_tensor(out=tB[s_][:], in0=sgB[s_][:], in1=bank[pb + 1][:], op=ALU.mult),
                 reads=[r_sgB[s_], r_bank[pb + 1]], writes=[r_tB[s_]])
            P.op("pool", lambda e, s_=s_, dt=dt, cols=cols: e.tensor_tensor(out=mT[:, dt, cols], in0=tA[s_][:], in1=tB[s_][:], op=ALU.add),
                 reads=[r_tA[s_], r_tB[s_]], writes=[r_mT[4 * j + i] for i in range(4)])

    P.barrier()
    h2rows = sb("h2rows", [128, 16, D], BF16, 32 * KB)
    r_h2r = [Res() for _ in range(16)]
    x1 = sb("x1", [128, 16, D], F32, 80 * KB)
    r_x1 = [Res() for _ in range(16)]
    wrt = sb("wrt", [128, 8, 36], BF16, W0 + 4 * KB)
    r_wrt = Res()
    P.dma("pool", wrt[:], wr_d.rearrange("(k p) n -> p k n", p=128), writes=[r_wrt])
    lg = sb("lg", [128, 16, 36], F32, W0 + 5 * KB)
    r_lg = Res()
    RT0 = W0 + 8 * KB
    xb2 = [sb(f"xb2_{i}", [128, D], F32, X0 + i * 4 * KB) for i in range(2)]
    r_xb2 = [Res(), Res()]
    h2Tt = [sb(f"h2Tt{i}", [128, 8, 128], BF16, X0 + 8 * KB + i * 2 * KB) for i in range(2)]
    r_h2Tt = [Res(), Res()]
    def e_a(ot):
        par = ot % 2
        x_t, r_x = xb2[par], r_xb2[par]
        P.dma("sp", x_t[:], xo[ot * 128:(ot + 1) * 128, :], writes=[r_x])
        pb = par * 4

        def mm_o(e):
            for half in range(2):
                for dt in range(8):
                    ins = e.matmul(bank[pb + half][:], lhsT=mT[:, dt, ot * 128:(ot + 1) * 128],
                                   rhs=wout[:, dt, half * 512:(half + 1) * 512], start=(dt == 0), stop=(dt == 7))
            return ins
        P.op("pe", mm_o, reads=[r_mT[ot], r_wout], writes=[r_bank[pb], r_bank[pb + 1]])
        for half in range(2):
            P.op("dve", lambda e, half=half: e.tensor_tensor(
                out=x1[:, ot, half * 512:(half + 1) * 512], in0=x_t[:, half * 512:(half + 1) * 512], in1=bank[pb + half][:],
                op=ALU.add), reads=[r_x, r_bank[pb + half]], writes=[r_x1[ot]])

    def e_b(ot):
        par = ot % 2
        rstd = rms_stats(x1[:, ot, :], r_x1[ot], par * 32, D, h2rows[:, ot, :], r_h2r[ot], par)
        P.op("dve", lambda e: e.scalar_tensor_tensor(out=h2rows[:, ot, :], in0=x1[:, ot, :], scalar=rstd, in1=g2t[:],
                                                     op0=ALU.mult, op1=ALU.mult),
             reads=[r_x1[ot], r_small_p[par], r_const], writes=[r_h2r[ot]])

    def e_c(ot):
        par = ot % 2
        pb = par * 4
        hT_t = h2Tt[par]
        tr8(h2rows[:, ot, :], r_h2r[ot], pb + 2, hT_t[:], [r_h2Tt[par]], "act")

        def mm_r(e):
            for k in range(8):
                ins = e.matmul(bank[pb + 3][:, 0:36], lhsT=hT_t[:, k, :], rhs=wrt[:, k, :],
                               start=(k == 0), stop=(k == 7))
            return ins
        P.op("pe", mm_r, reads=[r_h2Tt[par], r_wrt], writes=[r_bank[pb + 3]])
        P.op("dve", lambda e: e.tensor_tensor(out=lg[:, ot, :], in0=bank[pb + 3][:, 0:36], in1=brt[:], op=ALU.add),
             reads=[r_bank[pb + 3], r_const], writes=[r_lg])

    e_a(0)
    e_a(1)
    e_b(0)
    for ot in range(16):
        if ot + 2 < 16:
            e_a(ot + 2)
        if ot + 1 < 16:
            e_b(ot + 1)
        e_c(ot)

    NR = 4
    wgu = [sb(f"wgu{i}", [128, 8, 512], BF16, i * 8 * KB) for i in range(NR)]
    wde = [sb(f"wde{i}", [128, 2, D], BF16, 64 * KB + i * 4 * KB) for i in range(NR)]
    r_wgu = [Res() for _ in range(NR)]
    r_wde = [Res() for _ in range(NR)]
    def load_gu(ex):
        s_ = ex % NR
        P.dma("pool", wgu[s_][:, :, 0:DE], wge_d[ex].rearrange("(k p) f -> p k f", p=128), writes=[r_wgu[s_]])
        P.dma("pool", wgu[s_][:, :, DE:2 * DE], wue_d[ex].rearrange("(k p) f -> p k f", p=128), writes=[r_wgu[s_]])

    def load_d(ex):
        s_ = ex % NR
        P.dma("pool", wde[s_][:], wde_d[ex].rearrange("(f p) d -> p f d", p=128), writes=[r_wde[s_]])

    k_pe = P.cnt["pe"]
    si_pe = (k_pe - 1) // P.SEM_SPLIT
    pe_done = Tok(P.sems["pe"][si_pe], k_pe - si_pe * P.SEM_SPLIT)
    for ex in range(NR):
        P.dma("pool", wgu[ex][:, :, 0:DE], wge_d[ex].rearrange("(k p) f -> p k f", p=128), writes=[r_wgu[ex]], deps=[pe_done])
        P.dma("pool", wgu[ex][:, :, DE:2 * DE], wue_d[ex].rearrange("(k p) f -> p k f", p=128), writes=[r_wgu[ex]], deps=[pe_done])
        P.dma("pool", wde[ex][:], wde_d[ex].rearrange("(f p) d -> p f d", p=128), writes=[r_wde[ex]], deps=[pe_done])

    NT = 16
    off = [RT0]

    def rtile(name, shape, dt=F32):
        n = int(np.prod(shape)) * (2 if dt == BF16 else 4)
        t = sb("rt_" + name, [128] + list(shape), dt, off[0])
        off[0] += (n + 31) // 32 * 32
        return t
    r_rt = Res()

    def dv(fn, reads=(), writes=()):
        P.op("dve", fn, reads=[r_rt] + list(reads), writes=[r_rt] + list(writes))
    gl = lg[:, :, 0:4]
    el = lg[:, :, 4:36]
    gmax = rtile("gmax", [NT, 1])
    gone = rtile("gone", [NT, 4])
    gsh = rtile("gsh", [NT, 4])
    gsum = rtile("gsum", [NT, 1])
    gw = rtile("gw", [NT, 1])
    tmp = rtile("tmp", [NT, 32])
    selx = rtile("selx", [NT, 8])
    m1 = rtile("m1", [NT, 1])
    oh1 = rtile("oh1", [NT, 8])
    sel2 = rtile("sel2", [NT, 8])
    m2 = rtile("m2", [NT, 1])
    oh2 = rtile("oh2", [NT, 8])
    d21 = rtile("d21", [NT, 1])
    e21 = rtile("e21", [NT, 1])
    w1 = rtile("w1", [NT, 1])
    w2 = rtile("w2", [NT, 1])
    ohA = rtile("ohA", [NT, 32])
    ohB = rtile("ohB", [NT, 32])
    ohf = rtile("ohf", [NT, 32], BF16)
    pos = rtile("pos", [NT, 32])
    csum = rtile("csum", [NT, 32])
    carry = rtile("carry", [NT + 1, 32])
    dstA = rtile("dstA", [NT])
    dstB = rtile("dstB", [NT])
    dst_i = rtile("dst_i", [2, NT], mybir.dt.int32)
    cnt_i = rtile("cnt_i", [32], mybir.dt.int32)
    gmax_f = rtile("gmax_f", [4])
    gmax_i = rtile("gmax_i", [4], mybir.dt.int32)
    ebase = rtile("ebase", [32])
    tris = rtile("tris", [128], BF16)
    ones_b = rtile("ones_b", [128], BF16)
    comb = rtile("comb", [NT, 32])
    assert off[0] <= X0, off[0]
    P.dma("sp", ebase[:], ebase_d.partition_broadcast(128), writes=[r_rt])
    P.dma("pool", tris[:], tris_d[:, :], writes=[r_rt])
    P.op("dve", lambda e: e.memset(ones_b[:], 1.0), writes=[r_rt])

    def bc(ap, shape):
        return ap.to_broadcast(shape)
    P.op("dve", lambda e: e.tensor_reduce(out=gmax[:], in_=gl, axis=AX.X, op=ALU.max), reads=[r_lg], writes=[r_rt])
    dv(lambda e: e.tensor_tensor(out=gone[:], in0=gl, in1=bc(gmax[:], [128, NT, 4]), op=ALU.is_equal), reads=[r_lg])
    dv(lambda e: e.tensor_tensor(out=gsh[:], in0=gl, in1=bc(gmax[:], [128, NT, 4]), op=ALU.subtract), reads=[r_lg])
    P.op("act", lambda e: e.activation(out=gsh[:], in_=gsh[:], func=AF.Exp), reads=[r_rt], writes=[r_rt])
    dv(lambda e: e.tensor_reduce(out=gsum[:], in_=gsh[:], axis=AX.X, op=ALU.add))
    dv(lambda e: e.reciprocal(out=gw[:], in_=gsum[:]))
    dv(lambda e: e.tensor_tensor(out=tmp[:].rearrange("p t (g x) -> p t g x", g=4), in0=el.rearrange("p t (g x) -> p t g x", g=4),
                                 in1=bc(gone[:].unsqueeze(3), [128, NT, 4, 8]), op=ALU.mult), reads=[r_lg])
    dv(lambda e: e.tensor_reduce(out=selx[:], in_=tmp[:].rearrange("p t (g x) -> p t x g", g=4), axis=AX.X, op=ALU.add))
    dv(lambda e: e.tensor_reduce(out=m1[:], in_=selx[:], axis=AX.X, op=ALU.max))
    dv(lambda e: e.tensor_tensor(out=oh1[:], in0=selx[:], in1=bc(m1[:], [128, NT, 8]), op=ALU.is_equal))
    dv(lambda e: e.scalar_tensor_tensor(out=sel2[:], in0=oh1[:], scalar=-1e30, in1=selx[:], op0=ALU.mult, op1=ALU.add))
    dv(lambda e: e.tensor_reduce(out=m2[:], in_=sel2[:], axis=AX.X, op=ALU.max))
    dv(lambda e: e.tensor_tensor(out=oh2[:], in0=sel2[:], in1=bc(m2[:], [128, NT, 8]), op=ALU.is_equal))
    dv(lambda e: e.tensor_tensor(out=d21[:], in0=m2[:], in1=m1[:], op=ALU.subtract))
    P.op("act", lambda e: e.activation(out=e21[:], in_=d21[:], func=AF.Exp), reads=[r_rt], writes=[r_rt])
    dv(lambda e: e.tensor_scalar(out=w1[:], in0=e21[:], scalar1=1.0, scalar2=None, op0=ALU.add))
    dv(lambda e: e.reciprocal(out=w1[:], in_=w1[:]))
    dv(lambda e: e.tensor_tensor(out=w2[:], in0=e21[:], in1=w1[:], op=ALU.mult))
    dv(lambda e: e.tensor_tensor(out=w1[:], in0=w1[:], in1=gw[:], op=ALU.mult))
    dv(lambda e: e.tensor_tensor(out=w2[:], in0=w2[:], in1=gw[:], op=ALU.mult))
    dv(lambda e: e.tensor_tensor(out=ohA[:].rearrange("p t (g x) -> p t g x", g=4),
                                 in0=bc(gone[:].unsqueeze(3), [128, NT, 4, 8]),
                                 in1=bc(oh1[:].unsqueeze(2), [128, NT, 4, 8]), op=ALU.mult))
    dv(lambda e: e.tensor_tensor(out=ohB[:].rearrange("p t (g x) -> p t g x", g=4),
                                 in0=bc(gone[:].unsqueeze(3), [128, NT, 4, 8]),
                                 in1=bc(oh2[:].unsqueeze(2), [128, NT, 4, 8]), op=ALU.mult))
    dv(lambda e: e.tensor_tensor(out=ohf[:], in0=ohA[:], in1=ohB[:], op=ALU.add))
    ohf2 = ohf[:].rearrange("p t x -> p (t x)")

    def mm_pos(e):
        e.matmul(bank[0][:], lhsT=tris[:], rhs=ohf2, start=True, stop=True)
        return e.matmul(bank[1][:], lhsT=ones_b[:], rhs=ohf2, start=True, stop=True)
    P.op("pe", mm_pos, reads=[r_rt], writes=[r_bank[0], r_bank[1]])
    P.op("dve", lambda e: e.tensor_copy(out=csum[:].rearrange("p t x -> p (t x)"), in_=bank[1][:]),
         reads=[r_bank[1]], writes=[r_rt])
    dv(lambda e: e.memset(carry[:, 0, :], 0.0))
    for t_ in range(NT):
        dv(lambda e, t_=t_: e.tensor_tensor(out=carry[:, t_ + 1, :], in0=carry[:, t_, :], in1=csum[:, t_, :], op=ALU.add))
    P.op("dve", lambda e: e.tensor_tensor(out=pos[:].rearrange("p t x -> p (t x)"), in0=bank[0][:],
                                          in1=carry[:, 0:NT, :].rearrange("p t x -> p (t x)"), op=ALU.add),
         reads=[r_bank[0], r_rt], writes=[r_rt])
    dv(lambda e: e.tensor_tensor(out=pos[:], in0=pos[:], in1=bc(ebase[:].unsqueeze(1), [128, NT, 32]), op=ALU.add))
    dv(lambda e: e.tensor_tensor(out=tmp[:], in0=pos[:], in1=ohA[:], op=ALU.mult))
    dv(lambda e: e.tensor_reduce(out=dstA[:], in_=tmp[:], axis=AX.X, op=ALU.add))
    dv(lambda e: e.tensor_tensor(out=tmp[:], in0=pos[:], in1=ohB[:], op=ALU.mult))
    dv(lambda e: e.tensor_reduce(out=dstB[:], in_=tmp[:], axis=AX.X, op=ALU.add))
    dv(lambda e: e.tensor_copy(out=dst_i[:, 0, :], in_=dstA[:]))
    dv(lambda e: e.tensor_copy(out=dst_i[:, 1, :], in_=dstB[:]))
    dv(lambda e: e.tensor_copy(out=cnt_i[:], in_=carry[:, NT, :]))
    dv(lambda e: e.tensor_reduce(out=gmax_f[:, 0:1], in_=carry[:, NT, :], axis=AX.X, op=ALU.max))
    dv(lambda e: e.tensor_copy(out=gmax_i[:, 0:1], in_=gmax_f[:, 0:1]))
    if DEBUG:
        dv(lambda e: e.tensor_tensor(out=comb[:], in0=ohA[:], in1=bc(w1[:], [128, NT, 32]), op=ALU.mult))
        dv(lambda e: e.scalar_tensor_tensor(out=tmp[:], in0=ohB[:], scalar=1.0, in1=bc(w2[:], [128, NT, 32]), op0=ALU.mult, op1=ALU.mult))
        dv(lambda e: e.tensor_tensor(out=comb[:], in0=comb[:], in1=tmp[:], op=ALU.add))
        last.append(P.dma("sp", dbg["x1"].rearrange("(t p) d -> p t d", p=128), x1[:], reads=r_x1))
        last.append(P.dma("sp", dbg["comb"].rearrange("(t p) c -> p t c", p=128), comb[:], reads=[r_rt]))
    if stop_after == "E":
        P.barrier()
        P.build(last + P.gdeps)
        return nc

    CAP = NOWN
    Gx = nc.dram_tensor("Gx", [NE * CAP, D], BF16).ap()
    Yg = nc.dram_tensor("Yg", [NE * CAP, D], F32).ap()
    r_Gx, r_Yg = Res(), Res()
    for ot in range(16):
        for s_ in range(2):
            P.idma(Gx[:, :], dst_i[:, s_, ot:ot + 1], h2rows[:, ot, :], None, reads=[r_rt, r_h2r[ot]])
    P.barrier()
    if stop_after == "F0":
        P.build(last + P.gdeps)
        return nc
    F0 = 32 * KB
    Xg = [sb(f"Xg{i}", [128, D], BF16, F0 + i * 2 * KB) for i in range(3)]
    XgT = [sb(f"XgT{i}", [128, 8, 128], BF16, F0 + 6 * KB + i * 2 * KB) for i in range(2)]
    sgs = [sb(f"sgs{i}", [128, DE], F32, F0 + 10 * KB + i * KB) for i in range(2)]
    actb = [sb(f"actb{i}", [128, DE], BF16, F0 + 12 * KB + i * 512) for i in range(2)]
    actT = [sb(f"actT{i}", [128, 2, 128], BF16, F0 + 13 * KB + i * 512) for i in range(2)]
    Ys = [sb(f"Ys{i}", [128, D], F32, 144 * KB + i * 4 * KB) for i in range(2)]
    E0 = 48 * KB
    XgX = sb("XgX", [128, D], BF16, E0)
    XgTX = sb("XgTX", [128, 8, 128], BF16, E0 + 2 * KB)
    sgsX = sb("sgsX", [128, DE], F32, E0 + 4 * KB)
    actbX = sb("actbX", [128, DE], BF16, E0 + 5 * KB)
    actTX = sb("actTX", [128, 2, 128], BF16, E0 + 5 * KB + 512)
    r_Xg = [Res() for _ in range(3)]
    r_XgT, r_sgs, r_actb, r_actT, r_Ys = ([Res(), Res()] for _ in range(5))
    r_XgX, r_XgTX, r_sgsX, r_actbX, r_actTX = (Res() for _ in range(5))
    ys_i = [0]

    def stage_x(xg, r_xg, xgt, r_xgt, pbank):
        pT = bank_bf(pbank)

        def tr_x(e):
            for k in range(8):
                ins = e.transpose(out=pT[:, k * 128:(k + 1) * 128], in_=xg[:, k * 128:(k + 1) * 128], identity=ident[:])
            return ins
        P.op("pe", tr_x, reads=[r_xg, r_const], writes=[r_bank[pbank]])
        P.op("dve", lambda e: e.tensor_copy(out=xgt[:], in_=pT.rearrange("p (k t) -> p k t", k=8)),
             reads=[r_bank[pbank]], writes=[r_xgt])

    def stage_gu(ex, xgt, r_xgt, sg_, r_sg, ab_, r_ab, pbank):
        s_ = ex % NR

        def mm_gu(e):
            for k in range(8):
                ins = e.matmul(bank[pbank][:], lhsT=xgt[:, k, :], rhs=wgu[s_][:, k, :], start=(k == 0), stop=(k == 7))
            return ins
        P.op("pe", mm_gu, reads=[r_xgt, r_wgu[s_]], writes=[r_bank[pbank]])
        P.op("act", lambda e: e.activation(out=sg_[:], in_=bank[pbank][:, 0:DE], func=AF.Silu),
             reads=[r_bank[pbank]], writes=[r_sg])
        P.op("dve", lambda e: e.tensor_tensor(out=ab_[:], in0=sg_[:], in1=bank[pbank][:, DE:2 * DE], op=ALU.mult),
             reads=[r_sg, r_bank[pbank]], writes=[r_ab])

    def stage_d(ex, row0, ab_, r_ab, at_, r_at):
        s_ = ex % NR
        pA = bank_bf(4)

        def tr_a(e):
            for f in range(2):
                ins = e.transpose(out=pA[:, f * 128:(f + 1) * 128], in_=ab_[:, f * 128:(f + 1) * 128], identity=ident[:])
            return ins
        P.op("pe", tr_a, reads=[r_ab, r_const], writes=[r_bank[4]])
        P.op("dve", lambda e: e.tensor_copy(out=at_[:], in_=pA[:, 0:256].rearrange("p (f t) -> p f t", f=2)),
             reads=[r_bank[4]], writes=[r_at])

        def mm_d(e):
            for half in range(2):
                for f in range(2):
                    ins = e.matmul(bank[5 + half][:], lhsT=at_[:, f, :], rhs=wde[s_][:, f, half * 512:(half + 1) * 512],
                                   start=(f == 0), stop=(f == 1))
            return ins
        P.op("pe", mm_d, reads=[r_at, r_wde[s_]], writes=[r_bank[5], r_bank[6]])
        yb = ys_i[0] % 2
        ys_i[0] += 1
        P.op("dve", lambda e: e.tensor_copy(out=Ys[yb][:, 0:512], in_=bank[5][:]), reads=[r_bank[5]], writes=[r_Ys[yb]])
        P.op("dve", lambda e: e.tensor_copy(out=Ys[yb][:, 512:1024], in_=bank[6][:]), reads=[r_bank[6]], writes=[r_Ys[yb]])
        P.dma("sp", Yg[row0:row0 + 128, :], Ys[yb][:], reads=[r_Ys[yb]])

    ST = STATIC_TILES

    def load_x(u):
        ex, half = u // ST, u % ST
        r0 = ex * CAP + half * 128
        P.dma("act", Xg[u % 3][:], Gx[r0:r0 + 128, :], reads=[r_Gx], writes=[r_Xg[u % 3]])

    NU = ST * NE
    load_x(0)
    for it in range(NU + 2):
        if it + 1 < NU:
            load_x(it + 1)
        if it < NU:
            u = it
            stage_x(Xg[u % 3], r_Xg[u % 3], XgT[u % 2], r_XgT[u % 2], u % 2)
        if 0 <= it - 2 < NU:
            u = it - 2
            stage_d(u // ST, (u // ST) * CAP + (u % ST) * 128, actb[u % 2], r_actb[u % 2], actT[u % 2], r_actT[u % 2])
        if 0 <= it - 1 < NU:
            u = it - 1
            stage_gu(u // ST, XgT[u % 2], r_XgT[u % 2], sgs[u % 2], r_sgs[u % 2], actb[u % 2], r_actb[u % 2], 2 + u % 2)
        if it >= ST + 1 and (it - ST - 1) % ST == 0:
            exn = (it - ST - 1) // ST + NR
            if exn < NE:
                load_gu(exn)
                load_d(exn)

    cdeps = [r_rt.w]
    for g in range(1):
        P.begin_group(gmax_i[0:1, 0:1], cdeps, 128 * ST + 1)
        for ex in range(NE):
            P.begin_expert(cnt_i[0:1, ex:ex + 1], cdeps, k0=ST)
            for kt in range(ST, CAP // 128):
                P.begin_tile()
                if kt == ST:
                    load_gu(ex)
                    load_d(ex)
                row0 = ex * CAP + kt * 128
                P.dma("sp", XgX[:], Gx[row0:row0 + 128, :], reads=[r_Gx], writes=[r_XgX])
                stage_x(XgX, r_XgX, XgTX, r_XgTX, 7)
                stage_gu(ex, XgTX, r_XgTX, sgsX, r_sgsX, actbX, r_actbX, 7)
                stage_d(ex, row0, actbX, r_actbX, actTX, r_actTX)
            P.end_expert()
        P.end_group()

    P.barrier()
    if stop_after == "F1":
        P.build(last + P.gdeps)
        return nc
    NG = 8
    gbuf = [sb(f"gbuf{i}", [128, D], F32, i * 4 * KB) for i in range(NG)]
    r_gb = [Res() for _ in range(NG)]
    gi = 0
    for ot in range(16):
        for s_ in range(2):
            g_ = gbuf[gi % NG]
            r_g = r_gb[gi % NG]
            gi += 1
            P.idma(g_[:], None, Yg[:, :], dst_i[:, s_, ot:ot + 1], reads=[r_Yg, r_rt], writes=[r_g])
            wsc = (w1 if s_ == 0 else w2)
            P.op("dve", lambda e, g_=g_, ot=ot, wsc=wsc: e.scalar_tensor_tensor(
                out=x1[:, ot, :], in0=g_[:], scalar=wsc[:, ot, :], in1=x1[:, ot, :], op0=ALU.mult, op1=ALU.add),
                reads=[r_g, r_rt], writes=[r_x1[ot]])
        last.append(P.dma("sp", out_d[ot * 128:(ot + 1) * 128, :], x1[:, ot, :], reads=[r_x1[ot]]))
    P.barrier()
    P.build(last + P.gdeps)
    return nc


def make_inputs(core, inputs):
    b = core // 2
    chunks = CHUNKS[core % 2]
    x = np.asarray(inputs["x"], dtype=np.float32)
    xb = x[b]
    xo = np.concatenate([xb[c * 512:(c + 1) * 512] for c in chunks], axis=0)
    xh = np.zeros((128, D), np.float32)
    for j, c in enumerate(chunks):
        if c > 0:
            xh[2 * j:2 * j + 2] = xb[c * 512 - 2:c * 512]
    qpos = np.concatenate([np.arange(c * 512, (c + 1) * 512) for c in chunks]).astype(np.float32)[None, :]
    kpos = (np.arange(32)[None, :] * 128 + np.arange(128)[:, None]).astype(np.float32)
    negU = -(np.arange(128)[:, None] >= np.arange(128)[None, :]).astype(np.float32)
    cst = np.concatenate([negU, -np.ones((128, 128), np.float32)], axis=1)
    ebase = (np.arange(NE) * NOWN).astype(np.float32)[None, :]
    tris = (np.arange(128)[:, None] < np.arange(128)[None, :]).astype(np.float32)
    f = lambda k: np.ascontiguousarray(np.asarray(inputs[k], dtype=np.float32)[0])
    wr = np.concatenate([f("w_router_group"), f("w_router_expert")], axis=1)
    br = np.concatenate([f("b_router_group"), f("b_router_expert")])[None, :]
    return {
        "tflag": np.array([[1 - core % 2, core % 2]], dtype=np.int32),
        "xf": np.ascontiguousarray(xb), "xo": np.ascontiguousarray(xo), "xh": xh, "qpos": qpos, "kpos": kpos,
        "w_in": f("w_in"), "g1": f("norm_mix_g")[None, :], "gq": f("q_norm_g")[None, :], "gk": f("k_norm_g")[None, :],
        "convw": f("conv_w"), "wsb": f("w_sb_branch"), "wcb": f("w_conv_branch"), "wout": f("w_out"),
        "g2": f("norm_ffn_g")[None, :], "wr": np.ascontiguousarray(wr), "br": np.ascontiguousarray(br),
        "wge": f("w_gate_e"), "wue": f("w_up_e"), "wde": f("w_down_e"),
        "ident": np.eye(128, dtype=np.float32), "cst": cst, "ebase": ebase, "tris": tris,
    }


def kernel(**inputs):
    nc = build_program()
    in_maps = [make_inputs(c, inputs) for c in range(8)]
    res = run_bass_kernel_spmd(nc, in_maps, core_ids=list(range(8)))
    out = np.zeros((4, SEQ, D), np.float32)
    for c in range(8):
        b = c // 2
        o = res.results[c]["out"]
        for j, ch in enumerate(CHUNKS[c % 2]):
            out[b, ch * 512:(ch + 1) * 512] = o[j * 512:(j + 1) * 512]
    return out
```

```python
import contextlib
import numpy as np
import concourse.bass as bass
import concourse.mybir as mybir
from concourse.bass_utils import run_bass_kernel_spmd

F32 = mybir.dt.float32
BF16 = mybir.dt.bfloat16
AF = mybir.ActivationFunctionType
ALU = mybir.AluOpType
AX = mybir.AxisListType

D = 1024
SEQ = 4096
NOWN = 2048
NE = 32
DE = 256
EPS = 1e-6
CHUNKS = {0: (0, 3, 4, 7), 1: (1, 2, 5, 6)}


class Tok:
    __slots__ = ("sem", "val")

    def __init__(self, sem, val):
        self.sem = sem
        self.val = val


class Res:
    __slots__ = ("w", "r")

    def __init__(self):
        self.w = None
        self.r = []


class Prog:
    ENGS = ("pe", "act", "dve", "pool", "sp")
    SEM_SPLIT = 12000

    def __init__(self, nc):
        self.nc = nc
        self.es = contextlib.ExitStack()
        self.ops = {e: [] for e in self.ENGS}
        self.cnt = {e: 0 for e in self.ENGS}
        self.sems = {e: [] for e in self.ENGS}
        self.dma_sems = {}
        self.dma_cnt = {}
        self.dma_rr = {}
        self.dma_all = []
        self.nsem = 0
        self.gdeps = []

    def new_sem(self, name):
        self.nsem += 1
        return self.es.enter_context(self.nc.semaphore(name))

    def sbuf_at(self, name, shape, dt, off):
        return self.nc.alloc_sbuf_tensor_at(name, list(shape), dt, offset=off)

    def psum(self, name, shape, dt):
        return self.es.enter_context(self.nc.psum_tensor(name, list(shape), dt))

    def _deps(self, reads, writes, extra, nobar=False):
        deps = list(extra)
        if not nobar:
            deps.extend(self.gdeps)
        for r in reads:
            if r.w is not None:
                deps.append(r.w)
        for w in writes:
            if w.w is not None:
                deps.append(w.w)
            deps.extend(w.r)
        return deps

    def _note(self, tok, reads, writes):
        for r in reads:
            r.r.append(tok)
            if len(r.r) > 32:
                r.r = r.r[-32:]
        for w in writes:
            w.w = tok
            w.r = []

    def op(self, eng, fn, reads=(), writes=(), deps=()):
        d = self._deps(reads, writes, deps)
        k = self.cnt[eng]
        si = k // self.SEM_SPLIT
        while len(self.sems[eng]) <= si:
            self.sems[eng].append(self.new_sem(f"c_{eng}_{len(self.sems[eng])}"))
        sem = self.sems[eng][si]
        self.cnt[eng] = k + 1
        tok = Tok(sem, k - si * self.SEM_SPLIT + 1)
        self.ops[eng].append((fn, d, sem, 1, tok.val))
        self._note(tok, reads, writes)
        return tok

    def dma(self, q, out, in_, reads=(), writes=(), deps=(), nsem=6, nobar=False, **kw):
        d = self._deps(reads, writes, deps, nobar)
        if q not in self.dma_sems:
            self.dma_sems[q] = [self.new_sem(f"d_{q}_{i}") for i in range(nsem)]
            self.dma_cnt[q] = [0] * nsem
            self.dma_rr[q] = 0
        i = self.dma_rr[q]
        self.dma_rr[q] = (i + 1) % len(self.dma_sems[q])
        if self.dma_cnt[q][i] >= 3500:
            self.dma_sems[q][i] = self.new_sem(f"d_{q}_{i}_x{self.nsem}")
            self.dma_cnt[q][i] = 0
        self.dma_cnt[q][i] += 1
        sem = self.dma_sems[q][i]
        tok = Tok(sem, 16 * self.dma_cnt[q][i])
        self.dma_all.append(tok)

        def fn(e, out=out, in_=in_, kw=kw):
            return e.dma_start(out=out, in_=in_, **kw)

        self.ops[q].append((fn, d, sem, 16, tok.val))
        self._note(tok, reads, writes)
        return tok

    def begin_expert(self, cnt_ap, deps, k0=0):
        for e in self.ENGS:
            self.ops[e].append(("BEGIN_E", cnt_ap, list(deps), k0))

    def begin_group(self, cnt_ap, deps, thr):
        for e in self.ENGS:
            self.ops[e].append(("BEGIN_G", cnt_ap, list(deps), thr))

    def end_group(self):
        for e in self.ENGS:
            self.ops[e].append(("END_G",))

    def begin_tile(self):
        for e in self.ENGS:
            self.ops[e].append(("TILE",))

    def end_expert(self):
        for e in self.ENGS:
            self.ops[e].append(("END_E",))

    def idma(self, out, out_off, in_, in_off, reads=(), writes=(), deps=()):
        q = "pool"
        d = self._deps(reads, writes, deps)
        if q not in self.dma_sems:
            self.dma_sems[q] = [self.new_sem(f"d_{q}_{i}") for i in range(6)]
            self.dma_cnt[q] = [0] * 6
            self.dma_rr[q] = 0
        i = self.dma_rr[q]
        self.dma_rr[q] = (i + 1) % len(self.dma_sems[q])
        self.dma_cnt[q][i] += 1
        sem = self.dma_sems[q][i]
        tok = Tok(sem, 16 * self.dma_cnt[q][i])

        def fn(e):
            oo = bass.IndirectOffsetOnAxis(ap=out_off, axis=0) if out_off is not None else None
            io = bass.IndirectOffsetOnAxis(ap=in_off, axis=0) if in_off is not None else None
            return e.indirect_dma_start(out=out, out_offset=oo, in_=in_, in_offset=io)
        self.ops[q].append((fn, d, sem, 16, tok.val))
        self._note(tok, reads, writes)
        return tok

    def barrier(self):
        toks = []
        for e in self.ENGS:
            k = self.cnt[e]
            if k > 0:
                si = (k - 1) // self.SEM_SPLIT
                toks.append(Tok(self.sems[e][si], k - si * self.SEM_SPLIT))
        for q in self.dma_sems:
            for s, c in zip(self.dma_sems[q], self.dma_cnt[q]):
                if c > 0:
                    toks.append(Tok(s, 16 * c))
        self.gdeps = toks

    def build(self, final_toks):
        nc = self.nc
        prog = self
        with nc.Block() as block:
            def emit(eng_name, e):
                reg = [None, None]

                def wait_deps(deps, waited):
                    for t in deps:
                        key = id(t.sem)
                        if waited.get(key, 0) < t.val:
                            e.wait_ge(t.sem, t.val)
                            waited[key] = t.val

                def emit_op(item, waited):
                    fn, deps, sem, inc, _val = item
                    wait_deps(deps, waited)
                    ins = fn(e)
                    ins.then_inc(sem, inc)

                def flat_ops(items):
                    return [it for it in items if callable(it[0])]

                def replay(ops_):
                    incs = {}
                    order = []
                    for (_f, _d, sem, inc, val) in ops_:
                        if id(sem) not in incs:
                            incs[id(sem)] = [sem, 0, val - inc]
                            order.append(id(sem))
                        incs[id(sem)][1] += inc
                    for key in order:
                        if incs[key][2] > 0:
                            e.wait_ge(incs[key][0], incs[key][2])
                    for key in order:
                        left = incs[key][1]
                        while left > 0:
                            step = min(left, 400)
                            e.sem_inc(incs[key][0], step)
                            left -= step

                def run(items, waited):
                    i = 0
                    while i < len(items):
                        it = items[i]
                        if it[0] == "BEGIN_G":
                            _, cnt_ap, deps, thr = it
                            depth = 1
                            j = i + 1
                            while True:
                                if items[j][0] == "BEGIN_G":
                                    depth += 1
                                elif items[j][0] == "END_G":
                                    depth -= 1
                                    if depth == 0:
                                        break
                                j += 1
                            inner = items[i + 1:j]
                            i = j + 1
                            ops_ = flat_ops(inner)
                            if not ops_:
                                continue
                            wait_deps(deps, waited)
                            if reg[1] is None:
                                reg[1] = e.alloc_register("gcnt_" + eng_name)
                            e.reg_load(reg[1], cnt_ap)
                            with e.If_lt(reg[1], thr):
                                replay(ops_)
                            with e.Else():
                                run(inner, dict(waited))
                        elif it[0] == "BEGIN_E":
                            _, cnt_ap, deps, k0 = it
                            tiles = []
                            i += 1
                            while items[i][0] != "END_E":
                                if items[i][0] == "TILE":
                                    tiles.append([])
                                else:
                                    tiles[-1].append(items[i])
                                i += 1
                            i += 1
                            if sum(len(t) for t in tiles) == 0:
                                continue
                            wait_deps(deps, waited)
                            if reg[0] is None:
                                reg[0] = e.alloc_register("cnt_" + eng_name)
                            e.reg_load(reg[0], cnt_ap)

                            def rec(k, w_outer):
                                if k == len(tiles):
                                    return
                                with e.If_lt(reg[0], 128 * (k + k0) + 1):
                                    replay([op_ for tl in tiles[k:] for op_ in tl])
                                with e.Else():
                                    w = dict(w_outer)
                                    for op_ in tiles[k]:
                                        emit_op(op_, w)
                                    rec(k + 1, w)
                            rec(0, waited)
                        else:
                            emit_op(it, waited)
                            i += 1

                run(prog.ops[eng_name], {})
                if eng_name == "sp":
                    for t in final_toks:
                        e.wait_ge(t.sem, t.val)

            @block.tensor
            def _(e):
                emit("pe", e)

            @block.scalar
            def _(e):
                emit("act", e)

            @block.vector
            def _(e):
                emit("dve", e)

            @block.gpsimd
            def _(e):
                emit("pool", e)

            @block.sync
            def _(e):
                emit("sp", e)
        self.es.close()


KB = 1024


STATIC_TILES = 2


def build_program(stop_after=None, DEBUG=False):
    nc = bass.Bass("TRN2", target_bir_lowering=False)
    P = Prog(nc)

    def din(name, shape):
        return nc.dram_tensor(name, list(shape), F32, kind="ExternalInput").ap()

    xf = din("xf", [SEQ, D])
    xo = din("xo", [NOWN, D])
    xh = din("xh", [128, D])
    qpos_d = din("qpos", [1, NOWN])
    kpos_d = din("kpos", [128, 32])
    tflag_d = nc.dram_tensor("tflag", [1, 2], mybir.dt.int32, kind="ExternalInput").ap()
    w_in = din("w_in", [D, 5120])
    g1_d = din("g1", [1, D])
    gq_d = din("gq", [1, 64])
    gk_d = din("gk", [1, 64])
    convw_d = din("convw", [3, 512])
    wsb_d = din("wsb", [512, D])
    wcb_d = din("wcb", [512, D])
    wout_d = din("wout", [D, D])
    g2_d = din("g2", [1, D])
    wr_d = din("wr", [D, 36])
    br_d = din("br", [1, 36])
    wge_d = din("wge", [NE, D, DE])
    wue_d = din("wue", [NE, D, DE])
    wde_d = din("wde", [NE, DE, D])
    ident_d = din("ident", [128, 128])
    cst_d = din("cst", [128, 256])
    ebase_d = din("ebase", [1, NE])
    tris_d = din("tris", [128, 128])
    out_d = nc.dram_tensor("out", [NOWN, D], F32, kind="ExternalOutput").ap()
    dbg = {}
    if DEBUG:
        dbg["attnT"] = nc.dram_tensor("dbg_attnT", [128, 4 * NOWN], F32, kind="ExternalOutput").ap()
        dbg["convT"] = nc.dram_tensor("dbg_convT", [128, 4 * NOWN], F32, kind="ExternalOutput").ap()
        dbg["x1"] = nc.dram_tensor("dbg_x1", [NOWN, D], F32, kind="ExternalOutput").ap()
        dbg["comb"] = nc.dram_tensor("dbg_comb", [NOWN, 32], F32, kind="ExternalOutput").ap()

    bank = [P.psum(f"bank{i}", [128, 512], F32) for i in range(8)]
    r_bank = [Res() for _ in range(8)]

    def bank_bf(i):
        return bank[i][:].bitcast(BF16)

    BASE = 16896

    def sb(name, shape, dt, off):
        return P.sbuf_at(name, shape, dt, BASE + off)

    C0 = 192 * KB
    ident = sb("ident", [128, 128], BF16, C0); C0 += 256
    cst = sb("cst", [128, 256], BF16, C0); C0 += 512
    g1t = sb("g1t", [128, D], F32, C0); C0 += 4096
    g2t = sb("g2t", [128, D], F32, C0); C0 += 4096
    gqk = sb("gqk", [128, 64], F32, C0); C0 += 256
    gkt = sb("gkt", [128, 64], F32, C0); C0 += 256
    cw = sb("cw", [128, 3, 4], F32, C0); C0 += 64
    kpos = sb("kpos", [128, 32], F32, C0); C0 += 128
    tflag = sb("tflag", [128, 2], mybir.dt.int32, C0); C0 += 32
    r_tflag = Res()
    epsb = sb("epsb", [128, 2], F32, C0); C0 += 32
    brt = sb("brt", [128, 36], F32, C0); C0 += 160
    small = sb("small", [128, 64], F32, C0); C0 += 256
    rt = sb("rt", [128, 256], F32, C0); C0 += 1024
    assert C0 <= 205 * KB
    r_const = Res()
    r_small = Res()

    kT_all = sb("kT_all", [128, 4, SEQ], BF16, 0)
    V_all = sb("V_all", [128, 32, 512], BF16, 32 * KB)
    qT_all = sb("qT_all", [128, 4, NOWN], BF16, 64 * KB)
    hTo = sb("hTo", [128, 8, NOWN], BF16, 80 * KB)
    convT = sb("convT", [128, 4, NOWN], BF16, 112 * KB)
    attnT = sb("attnT", [128, 4, NOWN], BF16, 128 * KB)
    r_kT = [Res() for _ in range(32)]
    r_V = [Res() for _ in range(32)]
    r_qT = [Res() for _ in range(16)]
    r_hTo = [Res() for _ in range(16)]
    r_convT = [Res() for _ in range(4)]
    r_attnT = [Res() for _ in range(4)]
    W0 = 144 * KB
    X0 = 176 * KB

    negU = cst[:, 0:128]
    negOnes = cst[:, 128:256]

    P.dma("pool", ident[:], ident_d[:, :], writes=[r_const])
    P.dma("pool", cst[:], cst_d[:, :], writes=[r_const])
    P.dma("sp", g1t[:], g1_d.partition_broadcast(128), writes=[r_const])
    P.dma("sp", g2t[:], g2_d.partition_broadcast(128), writes=[r_const])
    P.dma("sp", gqk[:], gq_d.partition_broadcast(128), writes=[r_const])
    P.dma("sp", gkt[:], gk_d.partition_broadcast(128), writes=[r_const])
    for jj in range(3):
        P.dma("sp", cw[:, jj, :], convw_d[jj:jj + 1, :].rearrange("o (c p) -> p (o c)", p=128), writes=[r_const],
              allow_slow_non_contiguous=True)
    P.dma("sp", kpos[:], kpos_d[:, :], writes=[r_const])
    P.dma("sp", tflag[0:1, :], tflag_d[:, :], writes=[r_tflag])
    P.dma("sp", brt[:], br_d.partition_broadcast(128), writes=[r_const])
    P.op("dve", lambda e: e.memset(epsb[:], EPS), writes=[r_const])
    P.op("dve", lambda e: e.scalar_tensor_tensor(out=gqk[:], in0=gqk[:], scalar=0.125, in1=gkt[:],
                                                 op0=ALU.mult, op1=ALU.mult),
         reads=[r_const], writes=[r_const])

    sq_p = [sb("sq0", [128, 512], F32, X0 + 12 * KB), sb("sq1", [128, 512], F32, X0 + 14 * KB)]
    r_sq_p = [Res(), Res()]
    r_small_p = [Res(), Res()]

    def rms_stats(src_ap, r_src, col, n_feat, junk_ap, r_junk, par):
        r_sm = r_small_p[par]
        P.op("act", lambda e: e.activation(out=junk_ap, in_=src_ap, func=AF.Square,
                                           accum_out=small[:, col:col + 1]),
             reads=[r_src], writes=[r_sm, r_junk])
        P.op("act", lambda e: e.activation(out=small[:, col + 1:col + 2], in_=small[:, col:col + 1], func=AF.Ln,
                                           scale=1.0 / n_feat, bias=epsb[:, 0:1]),
             reads=[r_const], writes=[r_sm])
        P.op("act", lambda e: e.activation(out=small[:, col + 2:col + 3], in_=small[:, col + 1:col + 2], func=AF.Exp,
                                           scale=-0.5),
             writes=[r_sm])
        return small[:, col + 2:col + 3]

    def norm_transpose(x_ap, r_x, g_ap, hn_ap, r_hn, pbank, dest_ap, dest_res, par, evac="act"):
        rstd = rms_stats(x_ap, r_x, par * 32, D, hn_ap, r_hn, par)
        P.op("dve", lambda e: e.scalar_tensor_tensor(out=hn_ap, in0=x_ap, scalar=rstd, in1=g_ap,
                                                     op0=ALU.mult, op1=ALU.mult),
             reads=[r_x, r_small_p[par], r_const], writes=[r_hn])
        pT = bank_bf(pbank)

        def tr(e):
            for k in range(8):
                ins = e.transpose(out=pT[:, k * 128:(k + 1) * 128], in_=hn_ap[:, k * 128:(k + 1) * 128],
                                  identity=ident[:])
            return ins
        P.op("pe", tr, reads=[r_hn, r_const], writes=[r_bank[pbank]])
        if evac == "act":
            P.op("act", lambda e: e.copy(out=dest_ap, in_=pT.rearrange("p (k t) -> p k t", k=8)),
                 reads=[r_bank[pbank]], writes=list(dest_res))
        else:
            P.op("dve", lambda e: e.tensor_copy(out=dest_ap, in_=pT.rearrange("p (k t) -> p k t", k=8)),
                 reads=[r_bank[pbank]], writes=list(dest_res))

    def head_norm_a(pb, r_pb, par):
        sq, r_sq, r_sm = sq_p[par], r_sq_p[par], r_small_p[par]
        P.op("act", lambda e: e.activation(out=sq[:], in_=pb, func=AF.Square), reads=[r_pb], writes=[r_sq, r_sm])

    def head_norm_b(par):
        col = par * 32 + 8
        sq, r_sq, r_sm = sq_p[par], r_sq_p[par], r_small_p[par]
        P.op("dve", lambda e: e.tensor_reduce(out=small[:, col:col + 8], in_=sq[:].rearrange("p (h d) -> p h d", h=8),
                                              axis=AX.X, op=ALU.add),
             reads=[r_sq], writes=[r_sm])
        P.op("act", lambda e: e.activation(out=small[:, col:col + 8], in_=small[:, col:col + 8], func=AF.Ln,
                                           scale=1.0 / 64, bias=epsb[:, 0:1]),
             reads=[r_const], writes=[r_sm, r_sq])
        P.op("act", lambda e: e.activation(out=small[:, col + 8:col + 16], in_=small[:, col:col + 8], func=AF.Exp,
                                           scale=-0.5),
             writes=[r_sm])
        return small[:, col + 8:col + 16]

    wkv = sb("wkv", [128, 8, 1024], BF16, 64 * KB)
    r_wkv = Res()
    P.dma("pool", wkv[:], w_in[:, 512:1536].rearrange("(k p) n -> p k n", p=128), writes=[r_wkv])
    wq = sb("wq", [128, 8, 512], BF16, W0)
    wcv = sb("wcv", [128, 8, 1536], BF16, W0 + 8 * KB)
    r_wq = Res()
    r_wcv = Res()
    P.dma("pool", wq[:], w_in[:, 0:512].rearrange("(k p) n -> p k n", p=128), writes=[r_wq])
    P.dma("pool", wcv[:], w_in[:, 1536:3072].rearrange("(k p) n -> p k n", p=128), writes=[r_wcv])
    xb = [sb(f"xb{i}", [128, D], F32, X0 + i * 4 * KB) for i in range(2)]
    r_xb = [Res(), Res()]
    hn_p = [sb("hn0", [128, D], BF16, X0 + 8 * KB), sb("hn1", [128, D], BF16, 80 * KB)]
    r_hn_p = [Res(), Res()]
    hT_p = [sb("hT0", [128, 8, 128], BF16, X0 + 10 * KB), sb("hT1", [128, 8, 128], BF16, 82 * KB)]
    r_hT_p = [Res(), Res()]
    kn_p = [sb("kn0", [128, 512], BF16, 84 * KB), sb("kn1", [128, 512], BF16, 85 * KB)]
    r_kn_p = [Res(), Res()]
    hn, r_hn = hn_p[0], r_hn_p[0]

    def b_s1a(kt):
        par = kt % 2
        P.dma("sp", xb[par][:], xf[kt * 128:(kt + 1) * 128, :], writes=[r_xb[par]])
        rstd = rms_stats(xb[par][:], r_xb[par], par * 32, D, hn_p[par][:], r_hn_p[par], par)
        P.op("dve", lambda e: e.scalar_tensor_tensor(out=hn_p[par][:], in0=xb[par][:], scalar=rstd, in1=g1t[:],
                                                     op0=ALU.mult, op1=ALU.mult),
             reads=[r_xb[par], r_small_p[par], r_const], writes=[r_hn_p[par]])

    def tr8(src, r_src, pbank, dest_ap, dest_res, evac):
        pT = bank_bf(pbank)

        def tr(e):
            for k in range(8):
                ins = e.transpose(out=pT[:, k * 128:(k + 1) * 128], in_=src[:, k * 128:(k + 1) * 128], identity=ident[:])
            return ins
        P.op("pe", tr, reads=[r_src, r_const], writes=[r_bank[pbank]])
        if evac == "act":
            P.op("act", lambda e: e.copy(out=dest_ap, in_=pT.rearrange("p (k t) -> p k t", k=8)),
                 reads=[r_bank[pbank]], writes=list(dest_res))
        else:
            P.op("dve", lambda e: e.tensor_copy(out=dest_ap, in_=pT.rearrange("p (k t) -> p k t", k=8)),
                 reads=[r_bank[pbank]], writes=list(dest_res))

    def b_s1b(kt):
        par = kt % 2
        tr8(hn_p[par], r_hn_p[par], par * 4, hT_p[par][:], [r_hT_p[par]], "act")

    def b_s2a(kt):
        par = kt % 2
        pb = par * 4
        hT, r_hT = hT_p[par], r_hT_p[par]

        def mm_kv(e):
            for half in range(2):
                for k in range(8):
                    ins = e.matmul(bank[pb + 1 + half][:], lhsT=hT[:, k, :], rhs=wkv[:, k, half * 512:(half + 1) * 512],
                                   start=(k == 0), stop=(k == 7))
            return ins
        P.op("pe", mm_kv, reads=[r_hT, r_wkv], writes=[r_bank[pb + 1], r_bank[pb + 2]])
        P.op("dve", lambda e: e.tensor_copy(out=V_all[:, kt, :], in_=bank[pb + 2][:]),
             reads=[r_bank[pb + 2]], writes=[r_V[kt]])
        head_norm_a(bank[pb + 1][:], r_bank[pb + 1], par)

    def b_s2b(kt):
        par = kt % 2
        pb = par * 4
        kn, r_kn = kn_p[par], r_kn_p[par]
        krs = head_norm_b(par)
        P.op("dve", lambda e: e.tensor_tensor(
            out=kn[:].rearrange("p (h d) -> p h d", h=8), in0=bank[pb + 1][:].rearrange("p (h d) -> p h d", h=8),
            in1=krs.unsqueeze(2).to_broadcast([128, 8, 64]), op=ALU.mult),
            reads=[r_bank[pb + 1], r_small_p[par]], writes=[r_kn])

    def b_s2c(kt):
        par = kt % 2
        pb = par * 4
        kn, r_kn = kn_p[par], r_kn_p[par]
        pkT = bank_bf(pb + 3)

        def tr_k(e):
            for a in range(4):
                ins = e.transpose(out=pkT[:, a * 128:(a + 1) * 128], in_=kn[:, a * 128:(a + 1) * 128], identity=ident[:])
            return ins
        P.op("pe", tr_k, reads=[r_kn, r_const], writes=[r_bank[pb + 3]])
        P.op("dve", lambda e: e.tensor_copy(out=kT_all[:, :, kt * 128:(kt + 1) * 128],
                                            in_=pkT[:, 0:512].rearrange("p (a t) -> p a t", a=4)),
             reads=[r_bank[pb + 3]], writes=[r_kT[kt]])

    b_s1a(0)
    b_s1b(0)
    for kt in range(32):
        if kt + 1 < 32:
            b_s1a(kt + 1)
        b_s2a(kt)
        if kt + 1 < 32:
            b_s1b(kt + 1)
        b_s2b(kt)
        if kt >= 1:
            b_s2c(kt - 1)
    b_s2c(31)

    P.barrier()
    qn32 = sb("qn32", [128, 512], F32, X0 + 10 * KB)
    r_qn32 = Res()
    A0 = 128 * KB
    qn = sb("qn", [128, 512], BF16, A0)
    r_qn = Res()
    hTh = sb("hTh", [128, 8, 128], BF16, A0 + 1 * KB)
    r_hTh = Res()
    ccS = sb("ccS", [128, 512], F32, A0 + 3 * KB)
    r_ccS = Res()
    cub = sb("cub", [128, 768], F32, A0 + 5 * KB)
    r_cub = Res()
    y1 = sb("y1", [128, 512], F32, A0 + 8 * KB)
    y2 = sb("y2", [128, 512], F32, A0 + 10 * KB)
    r_y1 = Res()
    r_y2 = Res()
    cuh = sb("cuh", [128, 4, 8], F32, A0 + 12 * KB)
    r_cuh = Res()

    P.dma("sp", xb[0][:], xh[:, :], writes=[r_xb[0]])
    hn_pc = [hn_p[0], sb("hn1c", [128, D], BF16, A0 + 12 * KB + 512)]
    r_hn_pc = [r_hn_p[0], Res()]
    qn_p = [qn, sb("qn1", [128, 512], BF16, A0 + 14 * KB + 512)]
    r_qn_p = [r_qn, Res()]
    norm_transpose(xb[0][:], r_xb[0], g1t[:], hn_pc[0][:], r_hn_pc[0], 0, hTh[:], [r_hTh], 0)
    for c in range(4):
        def mm_h(e, c=c):
            for which in range(2):
                for k in range(8):
                    m = 4 + which * 4 + c
                    ins = e.matmul(bank[1 + which][:, 0:128], lhsT=wcv[:, k, m * 128:(m + 1) * 128], rhs=hTh[:, k, :],
                                   start=(k == 0), stop=(k == 7))
            return ins
        P.op("pe", mm_h, reads=[r_hTh, r_wcv], writes=[r_bank[1], r_bank[2]])
        P.op("act", lambda e: e.copy(out=qn32[:, 0:8], in_=bank[1][:, 0:8]), reads=[r_bank[1]], writes=[r_qn32])
        P.op("dve", lambda e, c=c: e.tensor_tensor(out=cuh[:, c, :], in0=qn32[:, 0:8], in1=bank[2][:, 0:8], op=ALU.mult),
             reads=[r_qn32, r_bank[2]], writes=[r_cuh])

    def c_s1a(ot):
        par = ot % 2
        P.dma("sp", xb[par][:], xo[ot * 128:(ot + 1) * 128, :], writes=[r_xb[par]])
        rstd = rms_stats(xb[par][:], r_xb[par], par * 32, D, hn_pc[par][:], r_hn_pc[par], par)
        P.op("dve", lambda e: e.scalar_tensor_tensor(out=hn_pc[par][:], in0=xb[par][:], scalar=rstd, in1=g1t[:],
                                                     op0=ALU.mult, op1=ALU.mult),
             reads=[r_xb[par], r_small_p[par], r_const], writes=[r_hn_pc[par]])

    def c_s1b(ot):
        par = ot % 2
        tr8(hn_pc[par], r_hn_pc[par], par * 4, hTo[:, :, ot * 128:(ot + 1) * 128], [r_hTo[ot]], "act")

    def c_s2a(ot):
        par = ot % 2
        pb = par * 4

        def mm_q(e):
            for k in range(8):
                ins = e.matmul(bank[pb + 1][:], lhsT=hTo[:, k, ot * 128:(ot + 1) * 128], rhs=wq[:, k, :],
                               start=(k == 0), stop=(k == 7))
            return ins
        P.op("pe", mm_q, reads=[r_hTo[ot], r_wq], writes=[r_bank[pb + 1]])
        head_norm_a(bank[pb + 1][:], r_bank[pb + 1], par)

    def c_s2b(ot):
        par = ot % 2
        pb = par * 4
        qrs = head_norm_b(par)
        qn, r_qn = qn_p[par], r_qn_p[par]
        P.op("dve", lambda e: e.tensor_tensor(
            out=qn32[:].rearrange("p (h d) -> p h d", h=8), in0=bank[pb + 1][:].rearrange("p (h d) -> p h d", h=8),
            in1=qrs.unsqueeze(2).to_broadcast([128, 8, 64]), op=ALU.mult),
            reads=[r_bank[pb + 1], r_small_p[par]], writes=[r_qn32])
        P.op("dve", lambda e: e.tensor_tensor(
            out=qn[:].rearrange("p (h d) -> p h d", h=8), in0=qn32[:].rearrange("p (h d) -> p h d", h=8),
            in1=gqk[:].unsqueeze(1).to_broadcast([128, 8, 64]), op=ALU.mult),
            reads=[r_qn32, r_const], writes=[r_qn])

    def c_s2c(ot):
        par = ot % 2
        pb = par * 4
        qn, r_qn = qn_p[par], r_qn_p[par]
        pqT = bank_bf(pb + 3)

        def tr_q(e):
            for a in range(4):
                ins = e.transpose(out=pqT[:, a * 128:(a + 1) * 128], in_=qn[:, a * 128:(a + 1) * 128], identity=ident[:])
            return ins
        P.op("pe", tr_q, reads=[r_qn, r_const], writes=[r_bank[pb + 3]])
        P.op("dve", lambda e: e.tensor_copy(out=qT_all[:, :, ot * 128:(ot + 1) * 128],
                                            in_=pqT[:, 0:512].rearrange("p (a t) -> p a t", a=4)),
             reads=[r_bank[pb + 3]], writes=[r_qT[ot]])

    def c_conv(j):
        if True:
            cols = slice(j * 512, (j + 1) * 512)
            for c in range(4):
                pbb = 1 + (c % 2) * 4

                def mm_c(e, c=c, pbb=pbb, cols=cols):
                    for which in range(3):
                        m = which * 4 + c
                        for k in range(8):
                            ins = e.matmul(bank[pbb + which][:], lhsT=wcv[:, k, m * 128:(m + 1) * 128], rhs=hTo[:, k, cols],
                                           start=(k == 0), stop=(k == 7))
                    return ins
                P.op("pe", mm_c, reads=[r_hTo[4 * j + i] for i in range(4)] + [r_wcv],
                     writes=[r_bank[pbb], r_bank[pbb + 1], r_bank[pbb + 2]])
                P.op("act", lambda e, pbb=pbb: e.copy(out=ccS[:], in_=bank[pbb + 1][:]), reads=[r_bank[pbb + 1]], writes=[r_ccS])
                P.op("dve", lambda e, pbb=pbb: e.tensor_tensor(out=cub[:, 2:514], in0=ccS[:], in1=bank[pbb + 2][:], op=ALU.mult),
                     reads=[r_ccS, r_bank[pbb + 2]], writes=[r_cub])
                P.op("dve", lambda e, c=c, j=j: e.tensor_copy(out=cub[:, 0:2], in_=cuh[:, c, 2 * j:2 * j + 2]),
                     reads=[r_cuh], writes=[r_cub])
                P.op("dve", lambda e, c=c: e.tensor_scalar(out=y1[:], in0=cub[:, 0:512], scalar1=cw[:, 0, c:c + 1], scalar2=None,
                                                            op0=ALU.mult),
                     reads=[r_cub, r_const], writes=[r_y1])
                P.op("dve", lambda e, c=c: e.scalar_tensor_tensor(out=y2[:], in0=cub[:, 1:513], scalar=cw[:, 1, c:c + 1], in1=y1[:],
                                                                   op0=ALU.mult, op1=ALU.add),
                     reads=[r_cub, r_y1, r_const], writes=[r_y2])
                P.op("dve", lambda e, c=c: e.scalar_tensor_tensor(out=y1[:], in0=cub[:, 2:514], scalar=cw[:, 2, c:c + 1], in1=y2[:],
                                                                   op0=ALU.mult, op1=ALU.add),
                     reads=[r_cub, r_y2, r_const], writes=[r_y1])
                P.op("dve", lambda e, c=c, pbb=pbb, cols=cols: e.tensor_tensor(out=convT[:, c, cols], in0=y1[:], in1=bank[pbb][:],
                                                                               op=ALU.mult),
                     reads=[r_y1, r_bank[pbb]], writes=[r_convT[j]])

    c_s1a(0)
    c_s1b(0)
    for ot in range(16):
        if ot + 1 < 16:
            c_s1a(ot + 1)
        c_s2a(ot)
        if ot + 1 < 16:
            c_s1b(ot + 1)
        c_s2b(ot)
        if ot >= 1:
            c_s2c(ot - 1)
        if ot % 4 == 3:
            c_conv(ot // 4)
    c_s2c(15)

    P.barrier()
    spb = sb("spb", [128, 32, 512], BF16, W0)
    r_sp = [Res() for _ in range(32)]
    masks = sb("masks", [128, 8, 512], BF16, X0)
    r_mask = Res()
    qposb = sb("qposb", [128, 512], F32, X0 + 4 * KB)
    r_qpos = Res()
    NB = 4
    ab = [sb(f"ab{i}", [128, 512], BF16, X0 + 9 * KB + i * KB) for i in range(NB)]
    r_a = [Res() for _ in range(NB)]
    NRB = 4
    Rb = [sb(f"Rb{i}", [128, 512], BF16, X0 + (6 + i) * KB if i < 3 else X0 + 13 * KB) for i in range(NRB)]
    r_R = [Res() for _ in range(NRB)]
    WARM = 1
    r_junk = Res()
    WARM2 = 0

    def gen_attention(Ls):
      for j in range(4):
          L = Ls[j]
          cols = slice(j * 512, (j + 1) * 512)
          P.dma("sp", qposb[:], qpos_d[:, cols].partition_broadcast(128), writes=[r_qpos])
          for i in range(4):
              n = L - 4 + i
              P.op("dve", lambda e, i=i, n=n: e.tensor_scalar(out=masks[:, i, :], in0=qposb[:], scalar1=kpos[:, n:n + 1],
                                                              scalar2=-30000.0, op0=ALU.is_le, op1=ALU.mult),
                   reads=[r_qpos, r_const], writes=[r_mask])
          qres = [r_qT[4 * j + i] for i in range(4)]
          for h in range(8):
              pr = h // 2
              hs = slice((h % 2) * 64, (h % 2) * 64 + 64)
              pO = 4 + (h % 2)
              for it, n in enumerate(range(L - 1, -1, -1)):
                  b = it % NB
                  masked = n >= L - 4

                  def zmm(e, n=n, b=b, hs=hs, pr=pr, cols=cols, masked=masked, i=n - (L - 4)):
                      ins = e.matmul(bank[b][:], lhsT=kT_all[hs, pr, n * 128:(n + 1) * 128], rhs=qT_all[hs, pr, cols],
                                     start=True, stop=not masked)
                      if masked:
                          ins = e.matmul(bank[b][:], lhsT=ident[:], rhs=masks[:, i, :], start=False, stop=True)
                      return ins
                  P.op("pe", zmm, reads=[r_kT[n], r_const] + qres + ([r_mask] if masked else []), writes=[r_bank[b]])
                  if WARM:
                      def warm(e, hs=hs, pr=pr, cols=cols, n=n):
                          for _ in range(WARM):
                              ins = e.matmul(bank[7][:], lhsT=kT_all[hs, pr, n * 128:(n + 1) * 128], rhs=qT_all[hs, pr, cols],
                                             start=True, stop=True)
                          return ins
                      P.op("pe", warm, writes=[r_junk])
                  P.op("act", lambda e, n=n, b=b: e.activation(out=spb[:, n, :], in_=bank[b][:], func=AF.Softplus),
                       reads=[r_bank[b]], writes=[r_sp[n]])
              pend = None
              for it, n in enumerate(range(L - 1, -1, -1)):
                  b = it % NB
                  first = (it == 0)
                  masked = n >= L - 4

                  def cmm(e, n=n, b=b, first=first, rb=it % NRB, hs=hs, pr=pr, cols=cols, masked=masked, i=n - (L - 4)):
                      e.matmul(bank[b][:], lhsT=negU, rhs=spb[:, n, :], start=True, stop=False)
                      if not first:
                          e.matmul(bank[b][:], lhsT=negOnes, rhs=Rb[rb][:], start=False, stop=False)
                      if masked:
                          e.matmul(bank[b][:], lhsT=ident[:], rhs=masks[:, i, :], start=False, stop=False)
                      return e.matmul(bank[b][:], lhsT=kT_all[hs, pr, n * 128:(n + 1) * 128], rhs=qT_all[hs, pr, cols],
                                      start=False, stop=True)
                  P.op("pe", cmm, reads=[r_sp[n], r_const, r_kT[n]] + qres + ([] if first else [r_R[it % NRB]]) +
                       ([r_mask] if masked else []), writes=[r_bank[b]])
                  if pend is not None:
                      P.op("pe", pend[0], reads=pend[1], writes=[r_bank[pO]])
                  if WARM2:
                      def warm2(e, hs=hs, pr=pr, cols=cols, n=n):
                          for _ in range(WARM2):
                              ins = e.matmul(bank[7][:], lhsT=kT_all[hs, pr, n * 128:(n + 1) * 128], rhs=qT_all[hs, pr, cols],
                                             start=True, stop=True)
                          return ins
                      P.op("pe", warm2, writes=[r_junk])
                  ai = it % NB
                  P.op("act", lambda e, b=b, ai=ai: e.activation(out=ab[ai][:], in_=bank[b][:], func=AF.Exp),
                       reads=[r_bank[b]], writes=[r_a[ai]])
                  if n > 0:
                      if first:
                          P.op("dve", lambda e, n=n: e.tensor_copy(out=Rb[1][:], in_=spb[:, n, :]),
                               reads=[r_sp[n]], writes=[r_R[1]])
                      else:
                          P.op("dve", lambda e, n=n, it=it: e.tensor_tensor(out=Rb[(it + 1) % NRB][:], in0=Rb[it % NRB][:],
                                                                            in1=spb[:, n, :], op=ALU.add),
                               reads=[r_sp[n], r_R[it % NRB]], writes=[r_R[(it + 1) % NRB]])
                  kw = {} if h % 2 == 0 else {"tile_position": (0, 64)}
                  pend = ((lambda e, n=n, ai=ai, first=first, last=(n == 0), h=h, hs=hs, pO=pO, kw=kw: e.matmul(
                      bank[pO][hs, :], lhsT=V_all[:, n, h * 64:(h + 1) * 64], rhs=ab[ai][:], start=first, stop=last, **kw)),
                      [r_V[n], r_a[ai]])
              P.op("pe", pend[0], reads=pend[1], writes=[r_bank[pO]])
              P.op("dve", lambda e, hs=hs, pr=pr, cols=cols, pO=pO: e.tensor_copy(out=attnT[hs, pr, cols], in_=bank[pO][hs, :]),
                   reads=[r_bank[pO]], writes=[r_attnT[j]])


    tf_deps = [r_tflag.w]
    P.begin_group(tflag[0:1, 0:1], tf_deps, 1)
    gen_attention((4, 16, 20, 32))
    P.end_group()
    P.begin_group(tflag[0:1, 1:2], tf_deps, 1)
    gen_attention((8, 12, 24, 28))
    P.end_group()

    last = []
    if DEBUG:
        P.barrier()
        stage = sb("stage", [128, 4 * NOWN], F32, 0)
        r_stage = Res()
        P.op("dve", lambda e: e.tensor_copy(out=stage[:], in_=attnT[:].rearrange("p a t -> p (a t)")), writes=[r_stage])
        last.append(P.dma("sp", dbg["attnT"][:, :], stage[:], reads=[r_stage]))
        stage2 = sb("stage2", [128, 4 * NOWN], F32, 32 * KB)
        r_stage2 = Res()
        P.op("dve", lambda e: e.tensor_copy(out=stage2[:], in_=convT[:].rearrange("p a t -> p (a t)")), writes=[r_stage2])
        last.append(P.dma("sp", dbg["convT"][:, :], stage2[:], reads=[r_stage2]))
    if stop_after == "D":
        P.barrier()
        P.build(last + P.gdeps)
        return nc

    P.barrier()
    mT = sb("mT", [128, 8, NOWN], BF16, 0)
    r_mT = [Res() for _ in range(16)]
    wg = sb("wg", [128, 8, 2048], BF16, 32 * KB)
    wsb = sb("wsb", [128, 4, D], BF16, W0)
    wcb = sb("wcb", [128, 4, D], BF16, W0 + 8 * KB)
    r_wg, r_wsb, r_wcb = Res(), Res(), Res()
    P.dma("pool", wsb[:], wsb_d.rearrange("(a p) n -> p a n", p=128), writes=[r_wsb])
    P.dma("pool", wcb[:], wcb_d.rearrange("(a p) n -> p a n", p=128), writes=[r_wcb])
    for half in range(2):
        P.dma("pool", wg[:, :, half * 1024:(half + 1) * 1024],
              w_in[:, 3072 + half * 1024:3072 + (half + 1) * 1024].rearrange("(k p) n -> p k n", p=128), writes=[r_wg])
    wout = sb("wout", [128, 8, D], BF16, 64 * KB)
    r_wout = Res()
    P.dma("pool", wout[:], wout_d.rearrange("(k p) n -> p k n", p=128), writes=[r_wout])
    sgA = [sb(f"sgA{i}", [128, 512], F32, X0 + i * 2 * KB) for i in range(2)]
    sgB = [sb(f"sgB{i}", [128, 512], F32, X0 + 4 * KB + i * 2 * KB) for i in range(2)]
    tA = [sb(f"tA{i}", [128, 512], F32, X0 + 8 * KB + i * 2 * KB) for i in range(2)]
    tB = [sb(f"tB{i}", [128, 512], F32, X0 + 12 * KB + i * 2 * KB) for i in range(2)]
    r_sgA, r_sgB, r_tA, r_tB = [Res(), Res()], [Res(), Res()], [Res(), Res()], [Res(), Res()]
    it = 0
    for dt in range(8):
        dcols = slice(dt * 128, (dt + 1) * 128)
        gacols = slice(dt * 128, (dt + 1) * 128)
        gbcols = slice(1024 + dt * 128, 1024 + (dt + 1) * 128)
        for j in range(4):
            cols = slice(j * 512, (j + 1) * 512)
            s_ = it % 2
            it += 1
            pb = 4 * s_

            def mm_e1(e, pb=pb, dcols=dcols, gacols=gacols, gbcols=gbcols, cols=cols):
                for a in range(4):
                    e.matmul(bank[pb][:], lhsT=wsb[:, a, dcols], rhs=attnT[:, a, cols], start=(a == 0), stop=(a == 3))
                for a in range(4):
                    e.matmul(bank[pb + 1][:], lhsT=wcb[:, a, dcols], rhs=convT[:, a, cols], start=(a == 0), stop=(a == 3))
                for k in range(8):
                    e.matmul(bank[pb + 2][:], lhsT=wg[:, k, gacols], rhs=hTo[:, k, cols], start=(k == 0), stop=(k == 7))
                for k in range(8):
                    ins = e.matmul(bank[pb + 3][:], lhsT=wg[:, k, gbcols], rhs=hTo[:, k, cols], start=(k == 0), stop=(k == 7))
                return ins
            P.op("pe", mm_e1, reads=[r_wsb, r_wcb, r_wg, r_attnT[j], r_convT[j]] + [r_hTo[4 * j + i] for i in range(4)],
                 writes=[r_bank[pb + i] for i in range(4)])
            P.op("act", lambda e, pb=pb, s_=s_: e.activation(out=sgA[s_][:], in_=bank[pb + 2][:], func=AF.Sigmoid),
                 reads=[r_bank[pb + 2]], writes=[r_sgA[s_]])
            P.op("act", lambda e, pb=pb, s_=s_: e.activation(out=sgB[s_][:], in_=bank[pb + 3][:], func=AF.Sigmoid),
                 reads=[r_bank[pb + 3]], writes=[r_sgB[s_]])
            P.op("dve", lambda e, pb=pb, s_=s_: e.tensor_tensor(out=tA[s_][:], in0=sgA[s_][:], in1=bank[pb][:], op=ALU.mult),
                 reads=[r_sgA[s_], r_bank[pb]], writes=[r_tA[s_]])
            P.op("dve", lambda e, pb=pb, s_=s_: e.tensor_tensor(out=tB[s_][:], in0=sgB[s_][:], in1=bank[pb + 1][:], op=ALU.mult),
                 reads=[r_sgB[s_], r_bank[pb + 1]], writes=[r_tB[s_]])
            P.op("pool", lambda e, s_=s_, dt=dt, cols=cols: e.tensor_tensor(out=mT[:, dt, cols], in0=tA[s_][:], in1=tB[s_][:], op=ALU.add),
                 reads=[r_tA[s_], r_tB[s_]], writes=[r_mT[4 * j + i] for i in range(4)])

    P.barrier()
    h2rows = sb("h2rows", [128, 16, D], BF16, 32 * KB)
    r_h2r = [Res() for _ in range(16)]
    x1 = sb("x1", [128, 16, D], F32, 80 * KB)
    r_x1 = [Res() for _ in range(16)]
    wrt = sb("wrt", [128, 8, 36], BF16, W0 + 4 * KB)
    r_wrt = Res()
    P.dma("pool", wrt[:], wr_d.rearrange("(k p) n -> p k n", p=128), writes=[r_wrt])
    lg = sb("lg", [128, 16, 36], F32, W0 + 5 * KB)
    r_lg = Res()
    RT0 = W0 + 8 * KB
    xb2 = [sb(f"xb2_{i}", [128, D], F32, X0 + i * 4 * KB) for i in range(2)]
    r_xb2 = [Res(), Res()]
    h2Tt = [sb(f"h2Tt{i}", [128, 8, 128], BF16, X0 + 8 * KB + i * 2 * KB) for i in range(2)]
    r_h2Tt = [Res(), Res()]
    def e_a(ot):
        par = ot % 2
        x_t, r_x = xb2[par], r_xb2[par]
        P.dma("sp", x_t[:], xo[ot * 128:(ot + 1) * 128, :], writes=[r_x])
        pb = par * 4

        def mm_o(e):
            for half in range(2):
                for dt in range(8):
                    ins = e.matmul(bank[pb + half][:], lhsT=mT[:, dt, ot * 128:(ot + 1) * 128],
                                   rhs=wout[:, dt, half * 512:(half + 1) * 512], start=(dt == 0), stop=(dt == 7))
            return ins
        P.op("pe", mm_o, reads=[r_mT[ot], r_wout], writes=[r_bank[pb], r_bank[pb + 1]])
        for half in range(2):
            P.op("dve", lambda e, half=half: e.tensor_tensor(
                out=x1[:, ot, half * 512:(half + 1) * 512], in0=x_t[:, half * 512:(half + 1) * 512], in1=bank[pb + half][:],
                op=ALU.add), reads=[r_x, r_bank[pb + half]], writes=[r_x1[ot]])

    def e_b(ot):
        par = ot % 2
        rstd = rms_stats(x1[:, ot, :], r_x1[ot], par * 32, D, h2rows[:, ot, :], r_h2r[ot], par)
        P.op("dve", lambda e: e.scalar_tensor_tensor(out=h2rows[:, ot, :], in0=x1[:, ot, :], scalar=rstd, in1=g2t[:],
                                                     op0=ALU.mult, op1=ALU.mult),
             reads=[r_x1[ot], r_small_p[par], r_const], writes=[r_h2r[ot]])

    def e_c(ot):
        par = ot % 2
        pb = par * 4
        hT_t = h2Tt[par]
        tr8(h2rows[:, ot, :], r_h2r[ot], pb + 2, hT_t[:], [r_h2Tt[par]], "act")

        def mm_r(e):
            for k in range(8):
                ins = e.matmul(bank[pb + 3][:, 0:36], lhsT=hT_t[:, k, :], rhs=wrt[:, k, :],
                               start=(k == 0), stop=(k == 7))
            return ins
        P.op("pe", mm_r, reads=[r_h2Tt[par], r_wrt], writes=[r_bank[pb + 3]])
        P.op("dve", lambda e: e.tensor_tensor(out=lg[:, ot, :], in0=bank[pb + 3][:, 0:36], in1=brt[:], op=ALU.add),
             reads=[r_bank[pb + 3], r_const], writes=[r_lg])

    e_a(0)
    e_a(1)
    e_b(0)
    for ot in range(16):
        if ot + 2 < 16:
            e_a(ot + 2)
        if ot + 1 < 16:
            e_b(ot + 1)
        e_c(ot)

    NR = 4
    wgu = [sb(f"wgu{i}", [128, 8, 512], BF16, i * 8 * KB) for i in range(NR)]
    wde = [sb(f"wde{i}", [128, 2, D], BF16, 64 * KB + i * 4 * KB) for i in range(NR)]
    r_wgu = [Res() for _ in range(NR)]
    r_wde = [Res() for _ in range(NR)]
    def load_gu(ex):
        s_ = ex % NR
        P.dma("pool", wgu[s_][:, :, 0:DE], wge_d[ex].rearrange("(k p) f -> p k f", p=128), writes=[r_wgu[s_]])
        P.dma("pool", wgu[s_][:, :, DE:2 * DE], wue_d[ex].rearrange("(k p) f -> p k f", p=128), writes=[r_wgu[s_]])

    def load_d(ex):
        s_ = ex % NR
        P.dma("pool", wde[s_][:], wde_d[ex].rearrange("(f p) d -> p f d", p=128), writes=[r_wde[s_]])

    k_pe = P.cnt["pe"]
    si_pe = (k_pe - 1) // P.SEM_SPLIT
    pe_done = Tok(P.sems["pe"][si_pe], k_pe - si_pe * P.SEM_SPLIT)
    for ex in range(NR):
        P.dma("pool", wgu[ex][:, :, 0:DE], wge_d[ex].rearrange("(k p) f -> p k f", p=128), writes=[r_wgu[ex]], deps=[pe_done])
        P.dma("pool", wgu[ex][:, :, DE:2 * DE], wue_d[ex].rearrange("(k p) f -> p k f", p=128), writes=[r_wgu[ex]], deps=[pe_done])
        P.dma("pool", wde[ex][:], wde_d[ex].rearrange("(f p) d -> p f d", p=128), writes=[r_wde[ex]], deps=[pe_done])

    NT = 16
    off = [RT0]

    def rtile(name, shape, dt=F32):
        n = int(np.prod(shape)) * (2 if dt == BF16 else 4)
        t = sb("rt_" + name, [128] + list(shape), dt, off[0])
        off[0] += (n + 31) // 32 * 32
        return t
    r_rt = Res()

    def dv(fn, reads=(), writes=()):
        P.op("dve", fn, reads=[r_rt] + list(reads), writes=[r_rt] + list(writes))
    gl = lg[:, :, 0:4]
    el = lg[:, :, 4:36]
    gmax = rtile("gmax", [NT, 1])
    gone = rtile("gone", [NT, 4])
    gsh = rtile("gsh", [NT, 4])
    gsum = rtile("gsum", [NT, 1])
    gw = rtile("gw", [NT, 1])
    tmp = rtile("tmp", [NT, 32])
    selx = rtile("selx", [NT, 8])
    m1 = rtile("m1", [NT, 1])
    oh1 = rtile("oh1", [NT, 8])
    sel2 = rtile("sel2", [NT, 8])
    m2 = rtile("m2", [NT, 1])
    oh2 = rtile("oh2", [NT, 8])
    d21 = rtile("d21", [NT, 1])
    e21 = rtile("e21", [NT, 1])
    w1 = rtile("w1", [NT, 1])
    w2 = rtile("w2", [NT, 1])
    ohA = rtile("ohA", [NT, 32])
    ohB = rtile("ohB", [NT, 32])
    ohf = rtile("ohf", [NT, 32], BF16)
    pos = rtile("pos", [NT, 32])
    csum = rtile("csum", [NT, 32])
    carry = rtile("carry", [NT + 1, 32])
    dstA = rtile("dstA", [NT])
    dstB = rtile("dstB", [NT])
    dst_i = rtile("dst_i", [2, NT], mybir.dt.int32)
    cnt_i = rtile("cnt_i", [32], mybir.dt.int32)
    gmax_f = rtile("gmax_f", [4])
    gmax_i = rtile("gmax_i", [4], mybir.dt.int32)
    ebase = rtile("ebase", [32])
    tris = rtile("tris", [128], BF16)
    ones_b = rtile("ones_b", [128], BF16)
    comb = rtile("comb", [NT, 32])
    assert off[0] <= X0, off[0]
    P.dma("sp", ebase[:], ebase_d.partition_broadcast(128), writes=[r_rt])
    P.dma("pool", tris[:], tris_d[:, :], writes=[r_rt])
    P.op("dve", lambda e: e.memset(ones_b[:], 1.0), writes=[r_rt])

    def bc(ap, shape):
        return ap.to_broadcast(shape)
    P.op("dve", lambda e: e.tensor_reduce(out=gmax[:], in_=gl, axis=AX.X, op=ALU.max), reads=[r_lg], writes=[r_rt])
    dv(lambda e: e.tensor_tensor(out=gone[:], in0=gl, in1=bc(gmax[:], [128, NT, 4]), op=ALU.is_equal), reads=[r_lg])
    dv(lambda e: e.tensor_tensor(out=gsh[:], in0=gl, in1=bc(gmax[:], [128, NT, 4]), op=ALU.subtract), reads=[r_lg])
    P.op("act", lambda e: e.activation(out=gsh[:], in_=gsh[:], func=AF.Exp), reads=[r_rt], writes=[r_rt])
    dv(lambda e: e.tensor_reduce(out=gsum[:], in_=gsh[:], axis=AX.X, op=ALU.add))
    dv(lambda e: e.reciprocal(out=gw[:], in_=gsum[:]))
    dv(lambda e: e.tensor_tensor(out=tmp[:].rearrange("p t (g x) -> p t g x", g=4), in0=el.rearrange("p t (g x) -> p t g x", g=4),
                                 in1=bc(gone[:].unsqueeze(3), [128, NT, 4, 8]), op=ALU.mult), reads=[r_lg])
    dv(lambda e: e.tensor_reduce(out=selx[:], in_=tmp[:].rearrange("p t (g x) -> p t x g", g=4), axis=AX.X, op=ALU.add))
    dv(lambda e: e.tensor_reduce(out=m1[:], in_=selx[:], axis=AX.X, op=ALU.max))
    dv(lambda e: e.tensor_tensor(out=oh1[:], in0=selx[:], in1=bc(m1[:], [128, NT, 8]), op=ALU.is_equal))
    dv(lambda e: e.scalar_tensor_tensor(out=sel2[:], in0=oh1[:], scalar=-1e30, in1=selx[:], op0=ALU.mult, op1=ALU.add))
    dv(lambda e: e.tensor_reduce(out=m2[:], in_=sel2[:], axis=AX.X, op=ALU.max))
    dv(lambda e: e.tensor_tensor(out=oh2[:], in0=sel2[:], in1=bc(m2[:], [128, NT, 8]), op=ALU.is_equal))
    dv(lambda e: e.tensor_tensor(out=d21[:], in0=m2[:], in1=m1[:], op=ALU.subtract))
    P.op("act", lambda e: e.activation(out=e21[:], in_=d21[:], func=AF.Exp), reads=[r_rt], writes=[r_rt])
    dv(lambda e: e.tensor_scalar(out=w1[:], in0=e21[:], scalar1=1.0, scalar2=None, op0=ALU.add))
    dv(lambda e: e.reciprocal(out=w1[:], in_=w1[:]))
    dv(lambda e: e.tensor_tensor(out=w2[:], in0=e21[:], in1=w1[:], op=ALU.mult))
    dv(lambda e: e.tensor_tensor(out=w1[:], in0=w1[:], in1=gw[:], op=ALU.mult))
    dv(lambda e: e.tensor_tensor(out=w2[:], in0=w2[:], in1=gw[:], op=ALU.mult))
    dv(lambda e: e.tensor_tensor(out=ohA[:].rearrange("p t (g x) -> p t g x", g=4),
                                 in0=bc(gone[:].unsqueeze(3), [128, NT, 4, 8]),
                                 in1=bc(oh1[:].unsqueeze(2), [128, NT, 4, 8]), op=ALU.mult))
    dv(lambda e: e.tensor_tensor(out=ohB[:].rearrange("p t (g x) -> p t g x", g=4),
                                 in0=bc(gone[:].unsqueeze(3), [128, NT, 4, 8]),
                                 in1=bc(oh2[:].unsqueeze(2), [128, NT, 4, 8]), op=ALU.mult))
    dv(lambda e: e.tensor_tensor(out=ohf[:], in0=ohA[:], in1=ohB[:], op=ALU.add))
    ohf2 = ohf[:].rearrange("p t x -> p (t x)")

    def mm_pos(e):
        e.matmul(bank[0][:], lhsT=tris[:], rhs=ohf2, start=True, stop=True)
        return e.matmul(bank[1][:], lhsT=ones_b[:], rhs=ohf2, start=True, stop=True)
    P.op("pe", mm_pos, reads=[r_rt], writes=[r_bank[0], r_bank[1]])
    P.op("dve", lambda e: e.tensor_copy(out=csum[:].rearrange("p t x -> p (t x)"), in_=bank[1][:]),
         reads=[r_bank[1]], writes=[r_rt])
    dv(lambda e: e.memset(carry[:, 0, :], 0.0))
    for t_ in range(NT):
        dv(lambda e, t_=t_: e.tensor_tensor(out=carry[:, t_ + 1, :], in0=carry[:, t_, :], in1=csum[:, t_, :], op=ALU.add))
    P.op("dve", lambda e: e.tensor_tensor(out=pos[:].rearrange("p t x -> p (t x)"), in0=bank[0][:],
                                          in1=carry[:, 0:NT, :].rearrange("p t x -> p (t x)"), op=ALU.add),
         reads=[r_bank[0], r_rt], writes=[r_rt])
    dv(lambda e: e.tensor_tensor(out=pos[:], in0=pos[:], in1=bc(ebase[:].unsqueeze(1), [128, NT, 32]), op=ALU.add))
    dv(lambda e: e.tensor_tensor(out=tmp[:], in0=pos[:], in1=ohA[:], op=ALU.mult))
    dv(lambda e: e.tensor_reduce(out=dstA[:], in_=tmp[:], axis=AX.X, op=ALU.add))
    dv(lambda e: e.tensor_tensor(out=tmp[:], in0=pos[:], in1=ohB[:], op=ALU.mult))
    dv(lambda e: e.tensor_reduce(out=dstB[:], in_=tmp[:], axis=AX.X, op=ALU.add))
    dv(lambda e: e.tensor_copy(out=dst_i[:, 0, :], in_=dstA[:]))
    dv(lambda e: e.tensor_copy(out=dst_i[:, 1, :], in_=dstB[:]))
    dv(lambda e: e.tensor_copy(out=cnt_i[:], in_=carry[:, NT, :]))
    dv(lambda e: e.tensor_reduce(out=gmax_f[:, 0:1], in_=carry[:, NT, :], axis=AX.X, op=ALU.max))
    dv(lambda e: e.tensor_copy(out=gmax_i[:, 0:1], in_=gmax_f[:, 0:1]))
    if DEBUG:
        dv(lambda e: e.tensor_tensor(out=comb[:], in0=ohA[:], in1=bc(w1[:], [128, NT, 32]), op=ALU.mult))
        dv(lambda e: e.scalar_tensor_tensor(out=tmp[:], in0=ohB[:], scalar=1.0, in1=bc(w2[:], [128, NT, 32]), op0=ALU.mult, op1=ALU.mult))
        dv(lambda e: e.tensor_tensor(out=comb[:], in0=comb[:], in1=tmp[:], op=ALU.add))
        last.append(P.dma("sp", dbg["x1"].rearrange("(t p) d -> p t d", p=128), x1[:], reads=r_x1))
        last.append(P.dma("sp", dbg["comb"].rearrange("(t p) c -> p t c", p=128), comb[:], reads=[r_rt]))
    if stop_after == "E":
        P.barrier()
        P.build(last + P.gdeps)
        return nc

    CAP = NOWN
    Gx = nc.dram_tensor("Gx", [NE * CAP, D], BF16).ap()
    Yg = nc.dram_tensor("Yg", [NE * CAP, D], F32).ap()
    r_Gx, r_Yg = Res(), Res()
    for ot in range(16):
        for s_ in range(2):
            P.idma(Gx[:, :], dst_i[:, s_, ot:ot + 1], h2rows[:, ot, :], None, reads=[r_rt, r_h2r[ot]])
    P.barrier()
    if stop_after == "F0":
        P.build(last + P.gdeps)
        return nc
    F0 = 32 * KB
    Xg = [sb(f"Xg{i}", [128, D], BF16, F0 + i * 2 * KB) for i in range(3)]
    XgT = [sb(f"XgT{i}", [128, 8, 128], BF16, F0 + 6 * KB + i * 2 * KB) for i in range(2)]
    sgs = [sb(f"sgs{i}", [128, DE], F32, F0 + 10 * KB + i * KB) for i in range(2)]
    actb = [sb(f"actb{i}", [128, DE], BF16, F0 + 12 * KB + i * 512) for i in range(2)]
    actT = [sb(f"actT{i}", [128, 2, 128], BF16, F0 + 13 * KB + i * 512) for i in range(2)]
    Ys = [sb(f"Ys{i}", [128, D], F32, 144 * KB + i * 4 * KB) for i in range(2)]
    E0 = 48 * KB
    XgX = sb("XgX", [128, D], BF16, E0)
    XgTX = sb("XgTX", [128, 8, 128], BF16, E0 + 2 * KB)
    sgsX = sb("sgsX", [128, DE], F32, E0 + 4 * KB)
    actbX = sb("actbX", [128, DE], BF16, E0 + 5 * KB)
    actTX = sb("actTX", [128, 2, 128], BF16, E0 + 5 * KB + 512)
    r_Xg = [Res() for _ in range(3)]
    r_XgT, r_sgs, r_actb, r_actT, r_Ys = ([Res(), Res()] for _ in range(5))
    r_XgX, r_XgTX, r_sgsX, r_actbX, r_actTX = (Res() for _ in range(5))
    ys_i = [0]

    def stage_x(xg, r_xg, xgt, r_xgt, pbank):
        pT = bank_bf(pbank)

        def tr_x(e):
            for k in range(8):
                ins = e.transpose(out=pT[:, k * 128:(k + 1) * 128], in_=xg[:, k * 128:(k + 1) * 128], identity=ident[:])
            return ins
        P.op("pe", tr_x, reads=[r_xg, r_const], writes=[r_bank[pbank]])
        P.op("dve", lambda e: e.tensor_copy(out=xgt[:], in_=pT.rearrange("p (k t) -> p k t", k=8)),
             reads=[r_bank[pbank]], writes=[r_xgt])

    def stage_gu(ex, xgt, r_xgt, sg_, r_sg, ab_, r_ab, pbank):
        s_ = ex % NR

        def mm_gu(e):
            for k in range(8):
                ins = e.matmul(bank[pbank][:], lhsT=xgt[:, k, :], rhs=wgu[s_][:, k, :], start=(k == 0), stop=(k == 7))
            return ins
        P.op("pe", mm_gu, reads=[r_xgt, r_wgu[s_]], writes=[r_bank[pbank]])
        P.op("act", lambda e: e.activation(out=sg_[:], in_=bank[pbank][:, 0:DE], func=AF.Silu),
             reads=[r_bank[pbank]], writes=[r_sg])
        P.op("dve", lambda e: e.tensor_tensor(out=ab_[:], in0=sg_[:], in1=bank[pbank][:, DE:2 * DE], op=ALU.mult),
             reads=[r_sg, r_bank[pbank]], writes=[r_ab])

    def stage_d(ex, row0, ab_, r_ab, at_, r_at):
        s_ = ex % NR
        pA = bank_bf(4)

        def tr_a(e):
            for f in range(2):
                ins = e.transpose(out=pA[:, f * 128:(f + 1) * 128], in_=ab_[:, f * 128:(f + 1) * 128], identity=ident[:])
            return ins
        P.op("pe", tr_a, reads=[r_ab, r_const], writes=[r_bank[4]])
        P.op("dve", lambda e: e.tensor_copy(out=at_[:], in_=pA[:, 0:256].rearrange("p (f t) -> p f t", f=2)),
             reads=[r_bank[4]], writes=[r_at])

        def mm_d(e):
            for half in range(2):
                for f in range(2):
                    ins = e.matmul(bank[5 + half][:], lhsT=at_[:, f, :], rhs=wde[s_][:, f, half * 512:(half + 1) * 512],
                                   start=(f == 0), stop=(f == 1))
            return ins
        P.op("pe", mm_d, reads=[r_at, r_wde[s_]], writes=[r_bank[5], r_bank[6]])
        yb = ys_i[0] % 2
        ys_i[0] += 1
        P.op("dve", lambda e: e.tensor_copy(out=Ys[yb][:, 0:512], in_=bank[5][:]), reads=[r_bank[5]], writes=[r_Ys[yb]])
        P.op("dve", lambda e: e.tensor_copy(out=Ys[yb][:, 512:1024], in_=bank[6][:]), reads=[r_bank[6]], writes=[r_Ys[yb]])
        P.dma("sp", Yg[row0:row0 + 128, :], Ys[yb][:], reads=[r_Ys[yb]])

    ST = STATIC_TILES

    def load_x(u):
        ex, half = u // ST, u % ST
        r0 = ex * CAP + half * 128
        P.dma("act", Xg[u % 3][:], Gx[r0:r0 + 128, :], reads=[r_Gx], writes=[r_Xg[u % 3]])

    NU = ST * NE
    load_x(0)
    for it in range(NU + 2):
        if it + 1 < NU:
            load_x(it + 1)
        if it < NU:
            u = it
            stage_x(Xg[u % 3], r_Xg[u % 3], XgT[u % 2], r_XgT[u % 2], u % 2)
        if 0 <= it - 2 < NU:
            u = it - 2
            stage_d(u // ST, (u // ST) * CAP + (u % ST) * 128, actb[u % 2], r_actb[u % 2], actT[u % 2], r_actT[u % 2])
        if 0 <= it - 1 < NU:
            u = it - 1
            stage_gu(u // ST, XgT[u % 2], r_XgT[u % 2], sgs[u % 2], r_sgs[u % 2], actb[u % 2], r_actb[u % 2], 2 + u % 2)
        if it >= ST + 1 and (it - ST - 1) % ST == 0:
            exn = (it - ST - 1) // ST + NR
            if exn < NE:
                load_gu(exn)
                load_d(exn)

    cdeps = [r_rt.w]
    for g in range(1):
        P.begin_group(gmax_i[0:1, 0:1], cdeps, 128 * ST + 1)
        for ex in range(NE):
            P.begin_expert(cnt_i[0:1, ex:ex + 1], cdeps, k0=ST)
            for kt in range(ST, CAP // 128):
                P.begin_tile()
                if kt == ST:
                    load_gu(ex)
                    load_d(ex)
                row0 = ex * CAP + kt * 128
                P.dma("sp", XgX[:], Gx[row0:row0 + 128, :], reads=[r_Gx], writes=[r_XgX])
                stage_x(XgX, r_XgX, XgTX, r_XgTX, 7)
                stage_gu(ex, XgTX, r_XgTX, sgsX, r_sgsX, actbX, r_actbX, 7)
                stage_d(ex, row0, actbX, r_actbX, actTX, r_actTX)
            P.end_expert()
        P.end_group()

    P.barrier()
    if stop_after == "F1":
        P.build(last + P.gdeps)
        return nc
    NG = 8
    gbuf = [sb(f"gbuf{i}", [128, D], F32, i * 4 * KB) for i in range(NG)]
    r_gb = [Res() for _ in range(NG)]
    gi = 0
    for ot in range(16):
        for s_ in range(2):
            g_ = gbuf[gi % NG]
            r_g = r_gb[gi % NG]
            gi += 1
            P.idma(g_[:], None, Yg[:, :], dst_i[:, s_, ot:ot + 1], reads=[r_Yg, r_rt], writes=[r_g])
            wsc = (w1 if s_ == 0 else w2)
            P.op("dve", lambda e, g_=g_, ot=ot, wsc=wsc: e.scalar_tensor_tensor(
                out=x1[:, ot, :], in0=g_[:], scalar=wsc[:, ot, :], in1=x1[:, ot, :], op0=ALU.mult, op1=ALU.add),
                reads=[r_g, r_rt], writes=[r_x1[ot]])
        last.append(P.dma("sp", out_d[ot * 128:(ot + 1) * 128, :], x1[:, ot, :], reads=[r_x1[ot]]))
    P.barrier()
    P.build(last + P.gdeps)
    return nc


def make_inputs(core, inputs):
    b = core // 2
    chunks = CHUNKS[core % 2]
    x = np.asarray(inputs["x"], dtype=np.float32)
    xb = x[b]
    xo = np.concatenate([xb[c * 512:(c + 1) * 512] for c in chunks], axis=0)
    xh = np.zeros((128, D), np.float32)
    for j, c in enumerate(chunks):
        if c > 0:
            xh[2 * j:2 * j + 2] = xb[c * 512 - 2:c * 512]
    qpos = np.concatenate([np.arange(c * 512, (c + 1) * 512) for c in chunks]).astype(np.float32)[None, :]
    kpos = (np.arange(32)[None, :] * 128 + np.arange(128)[:, None]).astype(np.float32)
    negU = -(np.arange(128)[:, None] >= np.arange(128)[None, :]).astype(np.float32)
    cst = np.concatenate([negU, -np.ones((128, 128), np.float32)], axis=1)
    ebase = (np.arange(NE) * NOWN).astype(np.float32)[None, :]
    tris = (np.arange(128)[:, None] < np.arange(128)[None, :]).astype(np.float32)
    f = lambda k: np.ascontiguousarray(np.asarray(inputs[k], dtype=np.float32)[0])
    wr = np.concatenate([f("w_router_group"), f("w_router_expert")], axis=1)
    br = np.concatenate([f("b_router_group"), f("b_router_expert")])[None, :]
    return {
        "tflag": np.array([[1 - core % 2, core % 2]], dtype=np.int32),
        "xf": np.ascontiguousarray(xb), "xo": np.ascontiguousarray(xo), "xh": xh, "qpos": qpos, "kpos": kpos,
        "w_in": f("w_in"), "g1": f("norm_mix_g")[None, :], "gq": f("q_norm_g")[None, :], "gk": f("k_norm_g")[None, :],
        "convw": f("conv_w"), "wsb": f("w_sb_branch"), "wcb": f("w_conv_branch"), "wout": f("w_out"),
        "g2": f("norm_ffn_g")[None, :], "wr": np.ascontiguousarray(wr), "br": np.ascontiguousarray(br),
        "wge": f("w_gate_e"), "wue": f("w_up_e"), "wde": f("w_down_e"),
        "ident": np.eye(128, dtype=np.float32), "cst": cst, "ebase": ebase, "tris": tris,
    }


def kernel(**inputs):
    nc = build_program()
    in_maps = [make_inputs(c, inputs) for c in range(8)]
    res = run_bass_kernel_spmd(nc, in_maps, core_ids=list(range(8)))
    out = np.zeros((4, SEQ, D), np.float32)
    for c in range(8):
        b = c // 2
        o = res.results[c]["out"]
        for j, ch in enumerate(CHUNKS[c % 2]):
            out[b, ch * 512:(ch + 1) * 512] = o[j * 512:(j + 1) * 512]
    return out
```

```python
import contextlib
import numpy as np
import concourse.bass as bass
import concourse.mybir as mybir
from concourse.bass_utils import run_bass_kernel_spmd

F32 = mybir.dt.float32
BF16 = mybir.dt.bfloat16
AF = mybir.ActivationFunctionType
ALU = mybir.AluOpType
AX = mybir.AxisListType

D = 1024
SEQ = 4096
NOWN = 2048
NE = 32
DE = 256
EPS = 1e-6
CHUNKS = {0: (0, 3, 4, 7), 1: (1, 2, 5, 6)}


class Tok:
    __slots__ = ("sem", "val")

    def __init__(self, sem, val):
        self.sem = sem
        self.val = val


class Res:
    __slots__ = ("w", "r")

    def __init__(self):
        self.w = None
        self.r = []


class Prog:
    ENGS = ("pe", "act", "dve", "pool", "sp")
    SEM_SPLIT = 12000

    def __init__(self, nc):
        self.nc = nc
        self.es = contextlib.ExitStack()
        self.ops = {e: [] for e in self.ENGS}
        self.cnt = {e: 0 for e in self.ENGS}
        self.sems = {e: [] for e in self.ENGS}
        self.dma_sems = {}
        self.dma_cnt = {}
        self.dma_rr = {}
        self.dma_all = []
        self.nsem = 0
        self.gdeps = []

    def new_sem(self, name):
        self.nsem += 1
        return self.es.enter_context(self.nc.semaphore(name))

    def sbuf_at(self, name, shape, dt, off):
        return self.nc.alloc_sbuf_tensor_at(name, list(shape), dt, offset=off)

    def psum(self, name, shape, dt):
        return self.es.enter_context(self.nc.psum_tensor(name, list(shape), dt))

    def _deps(self, reads, writes, extra, nobar=False):
        deps = list(extra)
        if not nobar:
            deps.extend(self.gdeps)
        for r in reads:
            if r.w is not None:
                deps.append(r.w)
        for w in writes:
            if w.w is not None:
                deps.append(w.w)
            deps.extend(w.r)
        return deps

    def _note(self, tok, reads, writes):
        for r in reads:
            r.r.append(tok)
            if len(r.r) > 32:
                r.r = r.r[-32:]
        for w in writes:
            w.w = tok
            w.r = []

    def op(self, eng, fn, reads=(), writes=(), deps=()):
        d = self._deps(reads, writes, deps)
        k = self.cnt[eng]
        si = k // self.SEM_SPLIT
        while len(self.sems[eng]) <= si:
            self.sems[eng].append(self.new_sem(f"c_{eng}_{len(self.sems[eng])}"))
        sem = self.sems[eng][si]
        self.cnt[eng] = k + 1
        tok = Tok(sem, k - si * self.SEM_SPLIT + 1)
        self.ops[eng].append((fn, d, sem, 1, tok.val))
        self._note(tok, reads, writes)
        return tok

    def dma(self, q, out, in_, reads=(), writes=(), deps=(), nsem=6, nobar=False, **kw):
        d = self._deps(reads, writes, deps, nobar)
        if q not in self.dma_sems:
            self.dma_sems[q] = [self.new_sem(f"d_{q}_{i}") for i in range(nsem)]
            self.dma_cnt[q] = [0] * nsem
            self.dma_rr[q] = 0
        i = self.dma_rr[q]
        self.dma_rr[q] = (i + 1) % len(self.dma_sems[q])
        if self.dma_cnt[q][i] >= 3500:
            self.dma_sems[q][i] = self.new_sem(f"d_{q}_{i}_x{self.nsem}")
            self.dma_cnt[q][i] = 0
        self.dma_cnt[q][i] += 1
        sem = self.dma_sems[q][i]
        tok = Tok(sem, 16 * self.dma_cnt[q][i])
        self.dma_all.append(tok)

        def fn(e, out=out, in_=in_, kw=kw):
            return e.dma_start(out=out, in_=in_, **kw)

        self.ops[q].append((fn, d, sem, 16, tok.val))
        self._note(tok, reads, writes)
        return tok

    def begin_expert(self, cnt_ap, deps, k0=0):
        for e in self.ENGS:
            self.ops[e].append(("BEGIN_E", cnt_ap, list(deps), k0))

    def begin_group(self, cnt_ap, deps, thr):
        for e in self.ENGS:
            self.ops[e].append(("BEGIN_G", cnt_ap, list(deps), thr))

    def end_group(self):
        for e in self.ENGS:
            self.ops[e].append(("END_G",))

    def begin_tile(self):
        for e in self.ENGS:
            self.ops[e].append(("TILE",))

    def end_expert(self):
        for e in self.ENGS:
            self.ops[e].append(("END_E",))

    def idma(self, out, out_off, in_, in_off, reads=(), writes=(), deps=()):
        q = "pool"
        d = self._deps(reads, writes, deps)
        if q not in self.dma_sems:
            self.dma_sems[q] = [self.new_sem(f"d_{q}_{i}") for i in range(6)]
            self.dma_cnt[q] = [0] * 6
            self.dma_rr[q] = 0
        i = self.dma_rr[q]
        self.dma_rr[q] = (i + 1) % len(self.dma_sems[q])
        self.dma_cnt[q][i] += 1
        sem = self.dma_sems[q][i]
        tok = Tok(sem, 16 * self.dma_cnt[q][i])

        def fn(e):
            oo = bass.IndirectOffsetOnAxis(ap=out_off, axis=0) if out_off is not None else None
            io = bass.IndirectOffsetOnAxis(ap=in_off, axis=0) if in_off is not None else None
            return e.indirect_dma_start(out=out, out_offset=oo, in_=in_, in_offset=io)
        self.ops[q].append((fn, d, sem, 16, tok.val))
        self._note(tok, reads, writes)
        return tok

    def barrier(self):
        toks = []
        for e in self.ENGS:
            k = self.cnt[e]
            if k > 0:
                si = (k - 1) // self.SEM_SPLIT
                toks.append(Tok(self.sems[e][si], k - si * self.SEM_SPLIT))
        for q in self.dma_sems:
            for s, c in zip(self.dma_sems[q], self.dma_cnt[q]):
                if c > 0:
                    toks.append(Tok(s, 16 * c))
        self.gdeps = toks

    def build(self, final_toks):
        nc = self.nc
        prog = self
        with nc.Block() as block:
            def emit(eng_name, e):
                reg = [None, None]

                def wait_deps(deps, waited):
                    for t in deps:
                        key = id(t.sem)
                        if waited.get(key, 0) < t.val:
                            e.wait_ge(t.sem, t.val)
                            waited[key] = t.val

                def emit_op(item, waited):
                    fn, deps, sem, inc, _val = item
                    wait_deps(deps, waited)
                    ins = fn(e)
                    ins.then_inc(sem, inc)

                def flat_ops(items):
                    return [it for it in items if callable(it[0])]

                def replay(ops_):
                    incs = {}
                    order = []
                    for (_f, _d, sem, inc, val) in ops_:
                        if id(sem) not in incs:
                            incs[id(sem)] = [sem, 0, val - inc]
                            order.append(id(sem))
                        incs[id(sem)][1] += inc
                    for key in order:
                        if incs[key][2] > 0:
                            e.wait_ge(incs[key][0], incs[key][2])
                    for key in order:
                        left = incs[key][1]
                        while left > 0:
                            step = min(left, 400)
                            e.sem_inc(incs[key][0], step)
                            left -= step

                def run(items, waited):
                    i = 0
                    while i < len(items):
                        it = items[i]
                        if it[0] == "BEGIN_G":
                            _, cnt_ap, deps, thr = it
                            depth = 1
                            j = i + 1
                            while True:
                                if items[j][0] == "BEGIN_G":
                                    depth += 1
                                elif items[j][0] == "END_G":
                                    depth -= 1
                                    if depth == 0:
                                        break
                                j += 1
                            inner = items[i + 1:j]
                            i = j + 1
                            ops_ = flat_ops(inner)
                            if not ops_:
                                continue
                            wait_deps(deps, waited)
                            if reg[1] is None:
                                reg[1] = e.alloc_register("gcnt_" + eng_name)
                            e.reg_load(reg[1], cnt_ap)
                            with e.If_lt(reg[1], thr):
                                replay(ops_)
                            with e.Else():
                                run(inner, dict(waited))
                        elif it[0] == "BEGIN_E":
                            _, cnt_ap, deps, k0 = it
                            tiles = []
                            i += 1
                            while items[i][0] != "END_E":
                                if items[i][0] == "TILE":
                                    tiles.append([])
                                else:
                                    tiles[-1].append(items[i])
                                i += 1
                            i += 1
                            if sum(len(t) for t in tiles) == 0:
                                continue
                            wait_deps(deps, waited)
                            if reg[0] is None:
                                reg[0] = e.alloc_register("cnt_" + eng_name)
                            e.reg_load(reg[0], cnt_ap)

                            def rec(k, w_outer):
                                if k == len(tiles):
                                    return
                                with e.If_lt(reg[0], 128 * (k + k0) + 1):
                                    replay([op_ for tl in tiles[k:] for op_ in tl])
                                with e.Else():
                                    w = dict(w_outer)
                                    for op_ in tiles[k]:
                                        emit_op(op_, w)
                                    rec(k + 1, w)
                            rec(0, waited)
                        else:
                            emit_op(it, waited)
                            i += 1

                run(prog.ops[eng_name], {})
                if eng_name == "sp":
                    for t in final_toks:
                        e.wait_ge(t.sem, t.val)

            @block.tensor
            def _(e):
                emit("pe", e)

            @block.scalar
            def _(e):
                emit("act", e)

            @block.vector
            def _(e):
                emit("dve", e)

            @block.gpsimd
            def _(e):
                emit("pool", e)

            @block.sync
            def _(e):
                emit("sp", e)
        self.es.close()


KB = 1024


STATIC_TILES = 2


def build_program(stop_after=None, DEBUG=False):
    nc = bass.Bass("TRN2", target_bir_lowering=False)
    P = Prog(nc)

    def din(name, shape):
        return nc.dram_tensor(name, list(shape), F32, kind="ExternalInput").ap()

    xf = din("xf", [SEQ, D])
    xo = din("xo", [NOWN, D])
    xh = din("xh", [128, D])
    qpos_d = din("qpos", [1, NOWN])
    kpos_d = din("kpos", [128, 32])
    tflag_d = nc.dram_tensor("tflag", [1, 2], mybir.dt.int32, kind="ExternalInput").ap()
    w_in = din("w_in", [D, 5120])
    g1_d = din("g1", [1, D])
    gq_d = din("gq", [1, 64])
    gk_d = din("gk", [1, 64])
    convw_d = din("convw", [3, 512])
    wsb_d = din("wsb", [512, D])
    wcb_d = din("wcb", [512, D])
    wout_d = din("wout", [D, D])
    g2_d = din("g2", [1, D])
    wr_d = din("wr", [D, 36])
    br_d = din("br", [1, 36])
    wge_d = din("wge", [NE, D, DE])
    wue_d = din("wue", [NE, D, DE])
    wde_d = din("wde", [NE, DE, D])
    ident_d = din("ident", [128, 128])
    cst_d = din("cst", [128, 256])
    ebase_d = din("ebase", [1, NE])
    tris_d = din("tris", [128, 128])
    out_d = nc.dram_tensor("out", [NOWN, D], F32, kind="ExternalOutput").ap()
    dbg = {}
    if DEBUG:
        dbg["attnT"] = nc.dram_tensor("dbg_attnT", [128, 4 * NOWN], F32, kind="ExternalOutput").ap()
        dbg["convT"] = nc.dram_tensor("dbg_convT", [128, 4 * NOWN], F32, kind="ExternalOutput").ap()
        dbg["x1"] = nc.dram_tensor("dbg_x1", [NOWN, D], F32, kind="ExternalOutput").ap()
        dbg["comb"] = nc.dram_tensor("dbg_comb", [NOWN, 32], F32, kind="ExternalOutput").ap()

    bank = [P.psum(f"bank{i}", [128, 512], F32) for i in range(8)]
    r_bank = [Res() for _ in range(8)]

    def bank_bf(i):
        return bank[i][:].bitcast(BF16)

    BASE = 16896

    def sb(name, shape, dt, off):
        return P.sbuf_at(name, shape, dt, BASE + off)

    C0 = 192 * KB
    ident = sb("ident", [128, 128], BF16, C0); C0 += 256
    cst = sb("cst", [128, 256], BF16, C0); C0 += 512
    g1t = sb("g1t", [128, D], F32, C0); C0 += 4096
    g2t = sb("g2t", [128, D], F32, C0); C0 += 4096
    gqk = sb("gqk", [128, 64], F32, C0); C0 += 256
    gkt = sb("gkt", [128, 64], F32, C0); C0 += 256
    cw = sb("cw", [128, 3, 4], F32, C0); C0 += 64
    kpos = sb("kpos", [128, 32], F32, C0); C0 += 128
    tflag = sb("tflag", [128, 2], mybir.dt.int32, C0); C0 += 32
    r_tflag = Res()
    epsb = sb("epsb", [128, 2], F32, C0); C0 += 32
    brt = sb("brt", [128, 36], F32, C0); C0 += 160
    small = sb("small", [128, 64], F32, C0); C0 += 256
    rt = sb("rt", [128, 256], F32, C0); C0 += 1024
    assert C0 <= 205 * KB
    r_const = Res()
    r_small = Res()

    kT_all = sb("kT_all", [128, 4, SEQ], BF16, 0)
    V_all = sb("V_all", [128, 32, 512], BF16, 32 * KB)
    qT_all = sb("qT_all", [128, 4, NOWN], BF16, 64 * KB)
    hTo = sb("hTo", [128, 8, NOWN], BF16, 80 * KB)
    convT = sb("convT", [128, 4, NOWN], BF16, 112 * KB)
    attnT = sb("attnT", [128, 4, NOWN], BF16, 128 * KB)
    r_kT = [Res() for _ in range(32)]
    r_V = [Res() for _ in range(32)]
    r_qT = [Res() for _ in range(16)]
    r_hTo = [Res() for _ in range(16)]
    r_convT = [Res() for _ in range(4)]
    r_attnT = [Res() for _ in range(4)]
    W0 = 144 * KB
    X0 = 176 * KB

    negU = cst[:, 0:128]
    negOnes = cst[:, 128:256]

    P.dma("pool", ident[:], ident_d[:, :], writes=[r_const])
    P.dma("pool", cst[:], cst_d[:, :], writes=[r_const])
    P.dma("sp", g1t[:], g1_d.partition_broadcast(128), writes=[r_const])
    P.dma("sp", g2t[:], g2_d.partition_broadcast(128), writes=[r_const])
    P.dma("sp", gqk[:], gq_d.partition_broadcast(128), writes=[r_const])
    P.dma("sp", gkt[:], gk_d.partition_broadcast(128), writes=[r_const])
    for jj in range(3):
        P.dma("sp", cw[:, jj, :], convw_d[jj:jj + 1, :].rearrange("o (c p) -> p (o c)", p=128), writes=[r_const],
              allow_slow_non_contiguous=True)
    P.dma("sp", kpos[:], kpos_d[:, :], writes=[r_const])
    P.dma("sp", tflag[0:1, :], tflag_d[:, :], writes=[r_tflag])
    P.dma("sp", brt[:], br_d.partition_broadcast(128), writes=[r_const])
    P.op("dve", lambda e: e.memset(epsb[:], EPS), writes=[r_const])
    P.op("dve", lambda e: e.scalar_tensor_tensor(out=gqk[:], in0=gqk[:], scalar=0.125, in1=gkt[:],
                                                 op0=ALU.mult, op1=ALU.mult),
         reads=[r_const], writes=[r_const])

    sq_p = [sb("sq0", [128, 512], F32, X0 + 12 * KB), sb("sq1", [128, 512], F32, X0 + 14 * KB)]
    r_sq_p = [Res(), Res()]
    r_small_p = [Res(), Res()]

    def rms_stats(src_ap, r_src, col, n_feat, junk_ap, r_junk, par):
        r_sm = r_small_p[par]
        P.op("act", lambda e: e.activation(out=junk_ap, in_=src_ap, func=AF.Square,
                                           accum_out=small[:, col:col + 1]),
             reads=[r_src], writes=[r_sm, r_junk])
        P.op("act", lambda e: e.activation(out=small[:, col + 1:col + 2], in_=small[:, col:col + 1], func=AF.Ln,
                                           scale=1.0 / n_feat, bias=epsb[:, 0:1]),
             reads=[r_const], writes=[r_sm])
        P.op("act", lambda e: e.activation(out=small[:, col + 2:col + 3], in_=small[:, col + 1:col + 2], func=AF.Exp,
                                           scale=-0.5),
             writes=[r_sm])
        return small[:, col + 2:col + 3]

    def norm_transpose(x_ap, r_x, g_ap, hn_ap, r_hn, pbank, dest_ap, dest_res, par, evac="act"):
        rstd = rms_stats(x_ap, r_x, par * 32, D, hn_ap, r_hn, par)
        P.op("dve", lambda e: e.scalar_tensor_tensor(out=hn_ap, in0=x_ap, scalar=rstd, in1=g_ap,
                                                     op0=ALU.mult, op1=ALU.mult),
             reads=[r_x, r_small_p[par], r_const], writes=[r_hn])
        pT = bank_bf(pbank)

        def tr(e):
            for k in range(8):
                ins = e.transpose(out=pT[:, k * 128:(k + 1) * 128], in_=hn_ap[:, k * 128:(k + 1) * 128],
                                  identity=ident[:])
            return ins
        P.op("pe", tr, reads=[r_hn, r_const], writes=[r_bank[pbank]])
        if evac == "act":
            P.op("act", lambda e: e.copy(out=dest_ap, in_=pT.rearrange("p (k t) -> p k t", k=8)),
                 reads=[r_bank[pbank]], writes=list(dest_res))
        else:
            P.op("dve", lambda e: e.tensor_copy(out=dest_ap, in_=pT.rearrange("p (k t) -> p k t", k=8)),
                 reads=[r_bank[pbank]], writes=list(dest_res))

    def head_norm_a(pb, r_pb, par):
        sq, r_sq, r_sm = sq_p[par], r_sq_p[par], r_small_p[par]
        P.op("act", lambda e: e.activation(out=sq[:], in_=pb, func=AF.Square), reads=[r_pb], writes=[r_sq, r_sm])

    def head_norm_b(par):
        col = par * 32 + 8
        sq, r_sq, r_sm = sq_p[par], r_sq_p[par], r_small_p[par]
        P.op("dve", lambda e: e.tensor_reduce(out=small[:, col:col + 8], in_=sq[:].rearrange("p (h d) -> p h d", h=8),
                                              axis=AX.X, op=ALU.add),
             reads=[r_sq], writes=[r_sm])
        P.op("act", lambda e: e.activation(out=small[:, col:col + 8], in_=small[:, col:col + 8], func=AF.Ln,
                                           scale=1.0 / 64, bias=epsb[:, 0:1]),
             reads=[r_const], writes=[r_sm, r_sq])
        P.op("act", lambda e: e.activation(out=small[:, col + 8:col + 16], in_=small[:, col:col + 8], func=AF.Exp,
                                           scale=-0.5),
             writes=[r_sm])
        return small[:, col + 8:col + 16]

    wkv = sb("wkv", [128, 8, 1024], BF16, 64 * KB)
    r_wkv = Res()
    P.dma("pool", wkv[:], w_in[:, 512:1536].rearrange("(k p) n -> p k n", p=128), writes=[r_wkv])
    wq = sb("wq", [128, 8, 512], BF16, W0)
    wcv = sb("wcv", [128, 8, 1536], BF16, W0 + 8 * KB)
    r_wq = Res()
    r_wcv = Res()
    P.dma("pool", wq[:], w_in[:, 0:512].rearrange("(k p) n -> p k n", p=128), writes=[r_wq])
    P.dma("pool", wcv[:], w_in[:, 1536:3072].rearrange("(k p) n -> p k n", p=128), writes=[r_wcv])
    xb = [sb(f"xb{i}", [128, D], F32, X0 + i * 4 * KB) for i in range(2)]
    r_xb = [Res(), Res()]
    hn_p = [sb("hn0", [128, D], BF16, X0 + 8 * KB), sb("hn1", [128, D], BF16, 80 * KB)]
    r_hn_p = [Res(), Res()]
    hT_p = [sb("hT0", [128, 8, 128], BF16, X0 + 10 * KB), sb("hT1", [128, 8, 128], BF16, 82 * KB)]
    r_hT_p = [Res(), Res()]
    kn_p = [sb("kn0", [128, 512], BF16, 84 * KB), sb("kn1", [128, 512], BF16, 85 * KB)]
    r_kn_p = [Res(), Res()]
    hn, r_hn = hn_p[0], r_hn_p[0]

    def b_s1a(kt):
        par = kt % 2
        P.dma("sp", xb[par][:], xf[kt * 128:(kt + 1) * 128, :], writes=[r_xb[par]])
        rstd = rms_stats(xb[par][:], r_xb[par], par * 32, D, hn_p[par][:], r_hn_p[par], par)
        P.op("dve", lambda e: e.scalar_tensor_tensor(out=hn_p[par][:], in0=xb[par][:], scalar=rstd, in1=g1t[:],
                                                     op0=ALU.mult, op1=ALU.mult),
             reads=[r_xb[par], r_small_p[par], r_const], writes=[r_hn_p[par]])

    def tr8(src, r_src, pbank, dest_ap, dest_res, evac):
        pT = bank_bf(pbank)

        def tr(e):
            for k in range(8):
                ins = e.transpose(out=pT[:, k * 128:(k + 1) * 128], in_=src[:, k * 128:(k + 1) * 128], identity=ident[:])
            return ins
        P.op("pe", tr, reads=[r_src, r_const], writes=[r_bank[pbank]])
        if evac == "act":
            P.op("act", lambda e: e.copy(out=dest_ap, in_=pT.rearrange("p (k t) -> p k t", k=8)),
                 reads=[r_bank[pbank]], writes=list(dest_res))
        else:
            P.op("dve", lambda e: e.tensor_copy(out=dest_ap, in_=pT.rearrange("p (k t) -> p k t", k=8)),
                 reads=[r_bank[pbank]], writes=list(dest_res))

    def b_s1b(kt):
        par = kt % 2
        tr8(hn_p[par], r_hn_p[par], par * 4, hT_p[par][:], [r_hT_p[par]], "act")

    def b_s2a(kt):
        par = kt % 2
        pb = par * 4
        hT, r_hT = hT_p[par], r_hT_p[par]

        def mm_kv(e):
            for half in range(2):
                for k in range(8):
                    ins = e.matmul(bank[pb + 1 + half][:], lhsT=hT[:, k, :], rhs=wkv[:, k, half * 512:(half + 1) * 512],
                                   start=(k == 0), stop=(k == 7))
            return ins
        P.op("pe", mm_kv, reads=[r_hT, r_wkv], writes=[r_bank[pb + 1], r_bank[pb + 2]])
        P.op("dve", lambda e: e.tensor_copy(out=V_all[:, kt, :], in_=bank[pb + 2][:]),
             reads=[r_bank[pb + 2]], writes=[r_V[kt]])
        head_norm_a(bank[pb + 1][:], r_bank[pb + 1], par)

    def b_s2b(kt):
        par = kt % 2
        pb = par * 4
        kn, r_kn = kn_p[par], r_kn_p[par]
        krs = head_norm_b(par)
        P.op("dve", lambda e: e.tensor_tensor(
            out=kn[:].rearrange("p (h d) -> p h d", h=8), in0=bank[pb + 1][:].rearrange("p (h d) -> p h d", h=8),
            in1=krs.unsqueeze(2).to_broadcast([128, 8, 64]), op=ALU.mult),
            reads=[r_bank[pb + 1], r_small_p[par]], writes=[r_kn])

    def b_s2c(kt):
        par = kt % 2
        pb = par * 4
        kn, r_kn = kn_p[par], r_kn_p[par]
        pkT = bank_bf(pb + 3)

        def tr_k(e):
            for a in range(4):
                ins = e.transpose(out=pkT[:, a * 128:(a + 1) * 128], in_=kn[:, a * 128:(a + 1) * 128], identity=ident[:])
            return ins
        P.op("pe", tr_k, reads=[r_kn, r_const], writes=[r_bank[pb + 3]])
        P.op("dve", lambda e: e.tensor_copy(out=kT_all[:, :, kt * 128:(kt + 1) * 128],
                                            in_=pkT[:, 0:512].rearrange("p (a t) -> p a t", a=4)),
             reads=[r_bank[pb + 3]], writes=[r_kT[kt]])

    b_s1a(0)
    b_s1b(0)
    for kt in range(32):
        if kt + 1 < 32:
            b_s1a(kt + 1)
        b_s2a(kt)
        if kt + 1 < 32:
            b_s1b(kt + 1)
        b_s2b(kt)
        if kt >= 1:
            b_s2c(kt - 1)
    b_s2c(31)

    P.barrier()
    qn32 = sb("qn32", [128, 512], F32, X0 + 10 * KB)
    r_qn32 = Res()
    A0 = 128 * KB
    qn = sb("qn", [128, 512], BF16, A0)
    r_qn = Res()
    hTh = sb("hTh", [128, 8, 128], BF16, A0 + 1 * KB)
    r_hTh = Res()
    ccS = sb("ccS", [128, 512], F32, A0 + 3 * KB)
    r_ccS = Res()
    cub = sb("cub", [128, 768], F32, A0 + 5 * KB)
    r_cub = Res()
    y1 = sb("y1", [128, 512], F32, A0 + 8 * KB)
    y2 = sb("y2", [128, 512], F32, A0 + 10 * KB)
    r_y1 = Res()
    r_y2 = Res()
    cuh = sb("cuh", [128, 4, 8], F32, A0 + 12 * KB)
    r_cuh = Res()

    P.dma("sp", xb[0][:], xh[:, :], writes=[r_xb[0]])
    hn_pc = [hn_p[0], sb("hn1c", [128, D], BF16, A0 + 12 * KB + 512)]
    r_hn_pc = [r_hn_p[0], Res()]
    qn_p = [qn, sb("qn1", [128, 512], BF16, A0 + 14 * KB + 512)]
    r_qn_p = [r_qn, Res()]
    norm_transpose(xb[0][:], r_xb[0], g1t[:], hn_pc[0][:], r_hn_pc[0], 0, hTh[:], [r_hTh], 0)
    for c in range(4):
        def mm_h(e, c=c):
            for which in range(2):
                for k in range(8):
                    m = 4 + which * 4 + c
                    ins = e.matmul(bank[1 + which][:, 0:128], lhsT=wcv[:, k, m * 128:(m + 1) * 128], rhs=hTh[:, k, :],
                                   start=(k == 0), stop=(k == 7))
            return ins
        P.op("pe", mm_h, reads=[r_hTh, r_wcv], writes=[r_bank[1], r_bank[2]])
        P.op("act", lambda e: e.copy(out=qn32[:, 0:8], in_=bank[1][:, 0:8]), reads=[r_bank[1]], writes=[r_qn32])
        P.op("dve", lambda e, c=c: e.tensor_tensor(out=cuh[:, c, :], in0=qn32[:, 0:8], in1=bank[2][:, 0:8], op=ALU.mult),
             reads=[r_qn32, r_bank[2]], writes=[r_cuh])

    def c_s1a(ot):
        par = ot % 2
        P.dma("sp", xb[par][:], xo[ot * 128:(ot + 1) * 128, :], writes=[r_xb[par]])
        rstd = rms_stats(xb[par][:], r_xb[par], par * 32, D, hn_pc[par][:], r_hn_pc[par], par)
        P.op("dve", lambda e: e.scalar_tensor_tensor(out=hn_pc[par][:], in0=xb[par][:], scalar=rstd, in1=g1t[:],
                                                     op0=ALU.mult, op1=ALU.mult),
             reads=[r_xb[par], r_small_p[par], r_const], writes=[r_hn_pc[par]])

    def c_s1b(ot):
        par = ot % 2
        tr8(hn_pc[par], r_hn_pc[par], par * 4, hTo[:, :, ot * 128:(ot + 1) * 128], [r_hTo[ot]], "act")

    def c_s2a(ot):
        par = ot % 2
        pb = par * 4

        def mm_q(e):
            for k in range(8):
                ins = e.matmul(bank[pb + 1][:], lhsT=hTo[:, k, ot * 128:(ot + 1) * 128], rhs=wq[:, k, :],
                               start=(k == 0), stop=(k == 7))
            return ins
        P.op("pe", mm_q, reads=[r_hTo[ot], r_wq], writes=[r_bank[pb + 1]])
        head_norm_a(bank[pb + 1][:], r_bank[pb + 1], par)

    def c_s2b(ot):
        par = ot % 2
        pb = par * 4
        qrs = head_norm_b(par)
        qn, r_qn = qn_p[par], r_qn_p[par]
        P.op("dve", lambda e: e.tensor_tensor(
            out=qn32[:].rearrange("p (h d) -> p h d", h=8), in0=bank[pb + 1][:].rearrange("p (h d) -> p h d", h=8),
            in1=qrs.unsqueeze(2).to_broadcast([128, 8, 64]), op=ALU.mult),
            reads=[r_bank[pb + 1], r_small_p[par]], writes=[r_qn32])
        P.op("dve", lambda e: e.tensor_tensor(
            out=qn[:].rearrange("p (h d) -> p h d", h=8), in0=qn32[:].rearrange("p (h d) -> p h d", h=8),
            in1=gqk[:].unsqueeze(1).to_broadcast([128, 8, 64]), op=ALU.mult),
            reads=[r_qn32, r_const], writes=[r_qn])

    def c_s2c(ot):
        par = ot % 2
        pb = par * 4
        qn, r_qn = qn_p[par], r_qn_p[par]
        pqT = bank_bf(pb + 3)

        def tr_q(e):
            for a in range(4):
                ins = e.transpose(out=pqT[:, a * 128:(a + 1) * 128], in_=qn[:, a * 128:(a + 1) * 128], identity=ident[:])
            return ins
        P.op("pe", tr_q, reads=[r_qn, r_const], writes=[r_bank[pb + 3]])
        P.op("dve", lambda e: e.tensor_copy(out=qT_all[:, :, ot * 128:(ot + 1) * 128],
                                            in_=pqT[:, 0:512].rearrange("p (a t) -> p a t", a=4)),
             reads=[r_bank[pb + 3]], writes=[r_qT[ot]])

    def c_conv(j):
        if True:
            cols = slice(j * 512, (j + 1) * 512)
            for c in range(4):
                pbb = 1 + (c % 2) * 4

                def mm_c(e, c=c, pbb=pbb, cols=cols):
                    for which in range(3):
                        m = which * 4 + c
                        for k in range(8):
                            ins = e.matmul(bank[pbb + which][:], lhsT=wcv[:, k, m * 128:(m + 1) * 128], rhs=hTo[:, k, cols],
                                           start=(k == 0), stop=(k == 7))
                    return ins
                P.op("pe", mm_c, reads=[r_hTo[4 * j + i] for i in range(4)] + [r_wcv],
                     writes=[r_bank[pbb], r_bank[pbb + 1], r_bank[pbb + 2]])
                P.op("act", lambda e, pbb=pbb: e.copy(out=ccS[:], in_=bank[pbb + 1][:]), reads=[r_bank[pbb + 1]], writes=[r_ccS])
                P.op("dve", lambda e, pbb=pbb: e.tensor_tensor(out=cub[:, 2:514], in0=ccS[:], in1=bank[pbb + 2][:], op=ALU.mult),
                     reads=[r_ccS, r_bank[pbb + 2]], writes=[r_cub])
                P.op("dve", lambda e, c=c, j=j: e.tensor_copy(out=cub[:, 0:2], in_=cuh[:, c, 2 * j:2 * j + 2]),
                     reads=[r_cuh], writes=[r_cub])
                P.op("dve", lambda e, c=c: e.tensor_scalar(out=y1[:], in0=cub[:, 0:512], scalar1=cw[:, 0, c:c + 1], scalar2=None,
                                                            op0=ALU.mult),
                     reads=[r_cub, r_const], writes=[r_y1])
                P.op("dve", lambda e, c=c: e.scalar_tensor_tensor(out=y2[:], in0=cub[:, 1:513], scalar=cw[:, 1, c:c + 1], in1=y1[:],
                                                                   op0=ALU.mult, op1=ALU.add),
                     reads=[r_cub, r_y1, r_const], writes=[r_y2])
                P.op("dve", lambda e, c=c: e.scalar_tensor_tensor(out=y1[:], in0=cub[:, 2:514], scalar=cw[:, 2, c:c + 1], in1=y2[:],
                                                                   op0=ALU.mult, op1=ALU.add),
                     reads=[r_cub, r_y2, r_const], writes=[r_y1])
                P.op("dve", lambda e, c=c, pbb=pbb, cols=cols: e.tensor_tensor(out=convT[:, c, cols], in0=y1[:], in1=bank[pbb][:],
                                                                               op=ALU.mult),
                     reads=[r_y1, r_bank[pbb]], writes=[r_convT[j]])

    c_s1a(0)
    c_s1b(0)
    for ot in range(16):
        if ot + 1 < 16:
            c_s1a(ot + 1)
        c_s2a(ot)
        if ot + 1 < 16:
            c_s1b(ot + 1)
        c_s2b(ot)
        if ot >= 1:
            c_s2c(ot - 1)
        if ot % 4 == 3:
            c_conv(ot // 4)
    c_s2c(15)

    P.barrier()
    spb = sb("spb", [128, 32, 512], BF16, W0)
    r_sp = [Res() for _ in range(32)]
    masks = sb("masks", [128, 8, 512], BF16, X0)
    r_mask = Res()
    qposb = sb("qposb", [128, 512], F32, X0 + 4 * KB)
    r_qpos = Res()
    NB = 4
    ab = [sb(f"ab{i}", [128, 512], BF16, X0 + 9 * KB + i * KB) for i in range(NB)]
    r_a = [Res() for _ in range(NB)]
    Rb = [sb(f"Rb{i}", [128, 512], BF16, X0 + 13 * KB + i * KB) for i in range(2)]
    r_R = [Res(), Res()]
    WARM = 1
    r_junk = Res()
    WARM2 = 0

    def gen_attention(Ls):
      for j in range(4):
          L = Ls[j]
          cols = slice(j * 512, (j + 1) * 512)
          P.dma("sp", qposb[:], qpos_d[:, cols].partition_broadcast(128), writes=[r_qpos])
          for i in range(4):
              n = L - 4 + i
              P.op("dve", lambda e, i=i, n=n: e.tensor_scalar(out=masks[:, i, :], in0=qposb[:], scalar1=kpos[:, n:n + 1],
                                                              scalar2=-30000.0, op0=ALU.is_le, op1=ALU.mult),
                   reads=[r_qpos, r_const], writes=[r_mask])
          qres = [r_qT[4 * j + i] for i in range(4)]
          for h in range(8):
              pr = h // 2
              hs = slice((h % 2) * 64, (h % 2) * 64 + 64)
              pO = 4 + (h % 2)
              for it, n in enumerate(range(L - 1, -1, -1)):
                  b = it % NB
                  masked = n >= L - 4

                  def zmm(e, n=n, b=b, hs=hs, pr=pr, cols=cols, masked=masked, i=n - (L - 4)):
                      ins = e.matmul(bank[b][:], lhsT=kT_all[hs, pr, n * 128:(n + 1) * 128], rhs=qT_all[hs, pr, cols],
                                     start=True, stop=not masked)
                      if masked:
                          ins = e.matmul(bank[b][:], lhsT=ident[:], rhs=masks[:, i, :], start=False, stop=True)
                      return ins
                  P.op("pe", zmm, reads=[r_kT[n], r_const] + qres + ([r_mask] if masked else []), writes=[r_bank[b]])
                  if WARM:
                      def warm(e, hs=hs, pr=pr, cols=cols, n=n):
                          for _ in range(WARM):
                              ins = e.matmul(bank[7][:], lhsT=kT_all[hs, pr, n * 128:(n + 1) * 128], rhs=qT_all[hs, pr, cols],
                                             start=True, stop=True)
                          return ins
                      P.op("pe", warm, writes=[r_junk])
                  P.op("act", lambda e, n=n, b=b: e.activation(out=spb[:, n, :], in_=bank[b][:], func=AF.Softplus),
                       reads=[r_bank[b]], writes=[r_sp[n]])
              pend = None
              for it, n in enumerate(range(L - 1, -1, -1)):
                  b = it % NB
                  first = (it == 0)
                  masked = n >= L - 4

                  def cmm(e, n=n, b=b, first=first, rb=it % 2, hs=hs, pr=pr, cols=cols, masked=masked, i=n - (L - 4)):
                      e.matmul(bank[b][:], lhsT=negU, rhs=spb[:, n, :], start=True, stop=False)
                      if not first:
                          e.matmul(bank[b][:], lhsT=negOnes, rhs=Rb[rb][:], start=False, stop=False)
                      if masked:
                          e.matmul(bank[b][:], lhsT=ident[:], rhs=masks[:, i, :], start=False, stop=False)
                      return e.matmul(bank[b][:], lhsT=kT_all[hs, pr, n * 128:(n + 1) * 128], rhs=qT_all[hs, pr, cols],
                                      start=False, stop=True)
                  P.op("pe", cmm, reads=[r_sp[n], r_const, r_kT[n]] + qres + ([] if first else [r_R[it % 2]]) +
                       ([r_mask] if masked else []), writes=[r_bank[b]])
                  if pend is not None:
                      P.op("pe", pend[0], reads=pend[1], writes=[r_bank[pO]])
                  if WARM2:
                      def warm2(e, hs=hs, pr=pr, cols=cols, n=n):
                          for _ in range(WARM2):
                              ins = e.matmul(bank[7][:], lhsT=kT_all[hs, pr, n * 128:(n + 1) * 128], rhs=qT_all[hs, pr, cols],
                                             start=True, stop=True)
                          return ins
                      P.op("pe", warm2, writes=[r_junk])
                  ai = it % NB
                  P.op("act", lambda e, b=b, ai=ai: e.activation(out=ab[ai][:], in_=bank[b][:], func=AF.Exp),
                       reads=[r_bank[b]], writes=[r_a[ai]])
                  if n > 0:
                      if first:
                          P.op("dve", lambda e, n=n: e.tensor_copy(out=Rb[1][:], in_=spb[:, n, :]),
                               reads=[r_sp[n]], writes=[r_R[1]])
                      else:
                          P.op("dve", lambda e, n=n, it=it: e.tensor_tensor(out=Rb[(it + 1) % 2][:], in0=Rb[it % 2][:],
                                                                            in1=spb[:, n, :], op=ALU.add),
                               reads=[r_sp[n], r_R[it % 2]], writes=[r_R[(it + 1) % 2]])
                  kw = {} if h % 2 == 0 else {"tile_position": (0, 64)}
                  pend = ((lambda e, n=n, ai=ai, first=first, last=(n == 0), h=h, hs=hs, pO=pO, kw=kw: e.matmul(
                      bank[pO][hs, :], lhsT=V_all[:, n, h * 64:(h + 1) * 64], rhs=ab[ai][:], start=first, stop=last, **kw)),
                      [r_V[n], r_a[ai]])
              P.op("pe", pend[0], reads=pend[1], writes=[r_bank[pO]])
              P.op("dve", lambda e, hs=hs, pr=pr, cols=cols, pO=pO: e.tensor_copy(out=attnT[hs, pr, cols], in_=bank[pO][hs, :]),
                   reads=[r_bank[pO]], writes=[r_attnT[j]])


    tf_deps = [r_tflag.w]
    P.begin_group(tflag[0:1, 0:1], tf_deps, 1)
    gen_attention((4, 16, 20, 32))
    P.end_group()
    P.begin_group(tflag[0:1, 1:2], tf_deps, 1)
    gen_attention((8, 12, 24, 28))
    P.end_group()

    last = []
    if DEBUG:
        P.barrier()
        stage = sb("stage", [128, 4 * NOWN], F32, 0)
        r_stage = Res()
        P.op("dve", lambda e: e.tensor_copy(out=stage[:], in_=attnT[:].rearrange("p a t -> p (a t)")), writes=[r_stage])
        last.append(P.dma("sp", dbg["attnT"][:, :], stage[:], reads=[r_stage]))
        stage2 = sb("stage2", [128, 4 * NOWN], F32, 32 * KB)
        r_stage2 = Res()
        P.op("dve", lambda e: e.tensor_copy(out=stage2[:], in_=convT[:].rearrange("p a t -> p (a t)")), writes=[r_stage2])
        last.append(P.dma("sp", dbg["convT"][:, :], stage2[:], reads=[r_stage2]))
    if stop_after == "D":
        P.barrier()
        P.build(last + P.gdeps)
        return nc

    P.barrier()
    mT = sb("mT", [128, 8, NOWN], BF16, 0)
    r_mT = [Res() for _ in range(16)]
    wg = sb("wg", [128, 8, 2048], BF16, 32 * KB)
    wsb = sb("wsb", [128, 4, D], BF16, W0)
    wcb = sb("wcb", [128, 4, D], BF16, W0 + 8 * KB)
    r_wg, r_wsb, r_wcb = Res(), Res(), Res()
    P.dma("pool", wsb[:], wsb_d.rearrange("(a p) n -> p a n", p=128), writes=[r_wsb])
    P.dma("pool", wcb[:], wcb_d.rearrange("(a p) n -> p a n", p=128), writes=[r_wcb])
    for half in range(2):
        P.dma("pool", wg[:, :, half * 1024:(half + 1) * 1024],
              w_in[:, 3072 + half * 1024:3072 + (half + 1) * 1024].rearrange("(k p) n -> p k n", p=128), writes=[r_wg])
    wout = sb("wout", [128, 8, D], BF16, 64 * KB)
    r_wout = Res()
    P.dma("pool", wout[:], wout_d.rearrange("(k p) n -> p k n", p=128), writes=[r_wout])
    sgA = [sb(f"sgA{i}", [128, 512], F32, X0 + i * 2 * KB) for i in range(2)]
    sgB = [sb(f"sgB{i}", [128, 512], F32, X0 + 4 * KB + i * 2 * KB) for i in range(2)]
    tA = [sb(f"tA{i}", [128, 512], F32, X0 + 8 * KB + i * 2 * KB) for i in range(2)]
    tB = [sb(f"tB{i}", [128, 512], F32, X0 + 12 * KB + i * 2 * KB) for i in range(2)]
    r_sgA, r_sgB, r_tA, r_tB = [Res(), Res()], [Res(), Res()], [Res(), Res()], [Res(), Res()]
    it = 0
    for dt in range(8):
        dcols = slice(dt * 128, (dt + 1) * 128)
        gacols = slice(dt * 128, (dt + 1) * 128)
        gbcols = slice(1024 + dt * 128, 1024 + (dt + 1) * 128)
        for j in range(4):
            cols = slice(j * 512, (j + 1) * 512)
            s_ = it % 2
            it += 1
            pb = 4 * s_

            def mm_e1(e, pb=pb, dcols=dcols, gacols=gacols, gbcols=gbcols, cols=cols):
                for a in range(4):
                    e.matmul(bank[pb][:], lhsT=wsb[:, a, dcols], rhs=attnT[:, a, cols], start=(a == 0), stop=(a == 3))
                for a in range(4):
                    e.matmul(bank[pb + 1][:], lhsT=wcb[:, a, dcols], rhs=convT[:, a, cols], start=(a == 0), stop=(a == 3))
                for k in range(8):
                    e.matmul(bank[pb + 2][:], lhsT=wg[:, k, gacols], rhs=hTo[:, k, cols], start=(k == 0), stop=(k == 7))
                for k in range(8):
                    ins = e.matmul(bank[pb + 3][:], lhsT=wg[:, k, gbcols], rhs=hTo[:, k, cols], start=(k == 0), stop=(k == 7))
                return ins
            P.op("pe", mm_e1, reads=[r_wsb, r_wcb, r_wg, r_attnT[j], r_convT[j]] + [r_hTo[4 * j + i] for i in range(4)],
                 writes=[r_bank[pb + i] for i in range(4)])
            P.op("act", lambda e, pb=pb, s_=s_: e.activation(out=sgA[s_][:], in_=bank[pb + 2][:], func=AF.Sigmoid),
                 reads=[r_bank[pb + 2]], writes=[r_sgA[s_]])
            P.op("act", lambda e, pb=pb, s_=s_: e.activation(out=sgB[s_][:], in_=bank[pb + 3][:], func=AF.Sigmoid),
                 reads=[r_bank[pb + 3]], writes=[r_sgB[s_]])
            P.op("dve", lambda e, pb=pb, s_=s_: e.tensor_tensor(out=tA[s_][:], in0=sgA[s_][:], in1=bank[pb][:], op=ALU.mult),
                 reads=[r_sgA[s_], r_bank[pb]], writes=[r_tA[s_]])
            P.op("dve", lambda e, pb=pb, s_=s_: e.tensor_tensor(out=tB[s_][:], in0=sgB[s_][:], in1=bank[pb + 1][:], op=ALU.mult),
                 reads=[r_sgB[s_], r_bank[pb + 1]], writes=[r_tB[s_]])
            P.op("pool", lambda e, s_=s_, dt=dt, cols=cols: e.tensor_tensor(out=mT[:, dt, cols], in0=tA[s_][:], in1=tB[s_][:], op=ALU.add),
                 reads=[r_tA[s_], r_tB[s_]], writes=[r_mT[4 * j + i] for i in range(4)])

    P.barrier()
    h2rows = sb("h2rows", [128, 16, D], BF16, 32 * KB)
    r_h2r = [Res() for _ in range(16)]
    x1 = sb("x1", [128, 16, D], F32, 80 * KB)
    r_x1 = [Res() for _ in range(16)]
    wrt = sb("wrt", [128, 8, 36], BF16, W0 + 4 * KB)
    r_wrt = Res()
    P.dma("pool", wrt[:], wr_d.rearrange("(k p) n -> p k n", p=128), writes=[r_wrt])
    lg = sb("lg", [128, 16, 36], F32, W0 + 5 * KB)
    r_lg = Res()
    RT0 = W0 + 8 * KB
    xb2 = [sb(f"xb2_{i}", [128, D], F32, X0 + i * 4 * KB) for i in range(2)]
    r_xb2 = [Res(), Res()]
    h2Tt = [sb(f"h2Tt{i}", [128, 8, 128], BF16, X0 + 8 * KB + i * 2 * KB) for i in range(2)]
    r_h2Tt = [Res(), Res()]
    def e_a(ot):
        par = ot % 2
        x_t, r_x = xb2[par], r_xb2[par]
        P.dma("sp", x_t[:], xo[ot * 128:(ot + 1) * 128, :], writes=[r_x])
        pb = par * 4

        def mm_o(e):
            for half in range(2):
                for dt in range(8):
                    ins = e.matmul(bank[pb + half][:], lhsT=mT[:, dt, ot * 128:(ot + 1) * 128],
                                   rhs=wout[:, dt, half * 512:(half + 1) * 512], start=(dt == 0), stop=(dt == 7))
            return ins
        P.op("pe", mm_o, reads=[r_mT[ot], r_wout], writes=[r_bank[pb], r_bank[pb + 1]])
        for half in range(2):
            P.op("dve", lambda e, half=half: e.tensor_tensor(
                out=x1[:, ot, half * 512:(half + 1) * 512], in0=x_t[:, half * 512:(half + 1) * 512], in1=bank[pb + half][:],
                op=ALU.add), reads=[r_x, r_bank[pb + half]], writes=[r_x1[ot]])

    def e_b(ot):
        par = ot % 2
        rstd = rms_stats(x1[:, ot, :], r_x1[ot], par * 32, D, h2rows[:, ot, :], r_h2r[ot], par)
        P.op("dve", lambda e: e.scalar_tensor_tensor(out=h2rows[:, ot, :], in0=x1[:, ot, :], scalar=rstd, in1=g2t[:],
                                                     op0=ALU.mult, op1=ALU.mult),
             reads=[r_x1[ot], r_small_p[par], r_const], writes=[r_h2r[ot]])

    def e_c(ot):
        par = ot % 2
        pb = par * 4
        hT_t = h2Tt[par]
        tr8(h2rows[:, ot, :], r_h2r[ot], pb + 2, hT_t[:], [r_h2Tt[par]], "act")

        def mm_r(e):
            for k in range(8):
                ins = e.matmul(bank[pb + 3][:, 0:36], lhsT=hT_t[:, k, :], rhs=wrt[:, k, :],
                               start=(k == 0), stop=(k == 7))
            return ins
        P.op("pe", mm_r, reads=[r_h2Tt[par], r_wrt], writes=[r_bank[pb + 3]])
        P.op("dve", lambda e: e.tensor_tensor(out=lg[:, ot, :], in0=bank[pb + 3][:, 0:36], in1=brt[:], op=ALU.add),
             reads=[r_bank[pb + 3], r_const], writes=[r_lg])

    e_a(0)
    e_a(1)
    e_b(0)
    for ot in range(16):
        if ot + 2 < 16:
            e_a(ot + 2)
        if ot + 1 < 16:
            e_b(ot + 1)
        e_c(ot)

    NR = 4
    wgu = [sb(f"wgu{i}", [128, 8, 512], BF16, i * 8 * KB) for i in range(NR)]
    wde = [sb(f"wde{i}", [128, 2, D], BF16, 64 * KB + i * 4 * KB) for i in range(NR)]
    r_wgu = [Res() for _ in range(NR)]
    r_wde = [Res() for _ in range(NR)]
    def load_gu(ex):
        s_ = ex % NR
        P.dma("pool", wgu[s_][:, :, 0:DE], wge_d[ex].rearrange("(k p) f -> p k f", p=128), writes=[r_wgu[s_]])
        P.dma("pool", wgu[s_][:, :, DE:2 * DE], wue_d[ex].rearrange("(k p) f -> p k f", p=128), writes=[r_wgu[s_]])

    def load_d(ex):
        s_ = ex % NR
        P.dma("pool", wde[s_][:], wde_d[ex].rearrange("(f p) d -> p f d", p=128), writes=[r_wde[s_]])

    k_pe = P.cnt["pe"]
    si_pe = (k_pe - 1) // P.SEM_SPLIT
    pe_done = Tok(P.sems["pe"][si_pe], k_pe - si_pe * P.SEM_SPLIT)
    for ex in range(NR):
        P.dma("pool", wgu[ex][:, :, 0:DE], wge_d[ex].rearrange("(k p) f -> p k f", p=128), writes=[r_wgu[ex]], deps=[pe_done])
        P.dma("pool", wgu[ex][:, :, DE:2 * DE], wue_d[ex].rearrange("(k p) f -> p k f", p=128), writes=[r_wgu[ex]], deps=[pe_done])
        P.dma("pool", wde[ex][:], wde_d[ex].rearrange("(f p) d -> p f d", p=128), writes=[r_wde[ex]], deps=[pe_done])

    NT = 16
    off = [RT0]

    def rtile(name, shape, dt=F32):
        n = int(np.prod(shape)) * (2 if dt == BF16 else 4)
        t = sb("rt_" + name, [128] + list(shape), dt, off[0])
        off[0] += (n + 31) // 32 * 32
        return t
    r_rt = Res()

    def dv(fn, reads=(), writes=()):
        P.op("dve", fn, reads=[r_rt] + list(reads), writes=[r_rt] + list(writes))
    gl = lg[:, :, 0:4]
    el = lg[:, :, 4:36]
    gmax = rtile("gmax", [NT, 1])
    gone = rtile("gone", [NT, 4])
    gsh = rtile("gsh", [NT, 4])
    gsum = rtile("gsum", [NT, 1])
    gw = rtile("gw", [NT, 1])
    tmp = rtile("tmp", [NT, 32])
    selx = rtile("selx", [NT, 8])
    m1 = rtile("m1", [NT, 1])
    oh1 = rtile("oh1", [NT, 8])
    sel2 = rtile("sel2", [NT, 8])
    m2 = rtile("m2", [NT, 1])
    oh2 = rtile("oh2", [NT, 8])
    d21 = rtile("d21", [NT, 1])
    e21 = rtile("e21", [NT, 1])
    w1 = rtile("w1", [NT, 1])
    w2 = rtile("w2", [NT, 1])
    ohA = rtile("ohA", [NT, 32])
    ohB = rtile("ohB", [NT, 32])
    ohf = rtile("ohf", [NT, 32], BF16)
    pos = rtile("pos", [NT, 32])
    csum = rtile("csum", [NT, 32])
    carry = rtile("carry", [NT + 1, 32])
    dstA = rtile("dstA", [NT])
    dstB = rtile("dstB", [NT])
    dst_i = rtile("dst_i", [2, NT], mybir.dt.int32)
    cnt_i = rtile("cnt_i", [32], mybir.dt.int32)
    gmax_f = rtile("gmax_f", [4])
    gmax_i = rtile("gmax_i", [4], mybir.dt.int32)
    ebase = rtile("ebase", [32])
    tris = rtile("tris", [128], BF16)
    ones_b = rtile("ones_b", [128], BF16)
    comb = rtile("comb", [NT, 32])
    assert off[0] <= X0, off[0]
    P.dma("sp", ebase[:], ebase_d.partition_broadcast(128), writes=[r_rt])
    P.dma("pool", tris[:], tris_d[:, :], writes=[r_rt])
    P.op("dve", lambda e: e.memset(ones_b[:], 1.0), writes=[r_rt])

    def bc(ap, shape):
        return ap.to_broadcast(shape)
    P.op("dve", lambda e: e.tensor_reduce(out=gmax[:], in_=gl, axis=AX.X, op=ALU.max), reads=[r_lg], writes=[r_rt])
    dv(lambda e: e.tensor_tensor(out=gone[:], in0=gl, in1=bc(gmax[:], [128, NT, 4]), op=ALU.is_equal), reads=[r_lg])
    dv(lambda e: e.tensor_tensor(out=gsh[:], in0=gl, in1=bc(gmax[:], [128, NT, 4]), op=ALU.subtract), reads=[r_lg])
    P.op("act", lambda e: e.activation(out=gsh[:], in_=gsh[:], func=AF.Exp), reads=[r_rt], writes=[r_rt])
    dv(lambda e: e.tensor_reduce(out=gsum[:], in_=gsh[:], axis=AX.X, op=ALU.add))
    dv(lambda e: e.reciprocal(out=gw[:], in_=gsum[:]))
    dv(lambda e: e.tensor_tensor(out=tmp[:].rearrange("p t (g x) -> p t g x", g=4), in0=el.rearrange("p t (g x) -> p t g x", g=4),
                                 in1=bc(gone[:].unsqueeze(3), [128, NT, 4, 8]), op=ALU.mult), reads=[r_lg])
    dv(lambda e: e.tensor_reduce(out=selx[:], in_=tmp[:].rearrange("p t (g x) -> p t x g", g=4), axis=AX.X, op=ALU.add))
    dv(lambda e: e.tensor_reduce(out=m1[:], in_=selx[:], axis=AX.X, op=ALU.max))
    dv(lambda e: e.tensor_tensor(out=oh1[:], in0=selx[:], in1=bc(m1[:], [128, NT, 8]), op=ALU.is_equal))
    dv(lambda e: e.scalar_tensor_tensor(out=sel2[:], in0=oh1[:], scalar=-1e30, in1=selx[:], op0=ALU.mult, op1=ALU.add))
    dv(lambda e: e.tensor_reduce(out=m2[:], in_=sel2[:], axis=AX.X, op=ALU.max))
    dv(lambda e: e.tensor_tensor(out=oh2[:], in0=sel2[:], in1=bc(m2[:], [128, NT, 8]), op=ALU.is_equal))
    dv(lambda e: e.tensor_tensor(out=d21[:], in0=m2[:], in1=m1[:], op=ALU.subtract))
    P.op("act", lambda e: e.activation(out=e21[:], in_=d21[:], func=AF.Exp), reads=[r_rt], writes=[r_rt])
    dv(lambda e: e.tensor_scalar(out=w1[:], in0=e21[:], scalar1=1.0, scalar2=None, op0=ALU.add))
    dv(lambda e: e.reciprocal(out=w1[:], in_=w1[:]))
    dv(lambda e: e.tensor_tensor(out=w2[:], in0=e21[:], in1=w1[:], op=ALU.mult))
    dv(lambda e: e.tensor_tensor(out=w1[:], in0=w1[:], in1=gw[:], op=ALU.mult))
    dv(lambda e: e.tensor_tensor(out=w2[:], in0=w2[:], in1=gw[:], op=ALU.mult))
    dv(lambda e: e.tensor_tensor(out=ohA[:].rearrange("p t (g x) -> p t g x", g=4),
                                 in0=bc(gone[:].unsqueeze(3), [128, NT, 4, 8]),
                                 in1=bc(oh1[:].unsqueeze(2), [128, NT, 4, 8]), op=ALU.mult))
    dv(lambda e: e.tensor_tensor(out=ohB[:].rearrange("p t (g x) -> p t g x", g=4),
                                 in0=bc(gone[:].unsqueeze(3), [128, NT, 4, 8]),
                                 in1=bc(oh2[:].unsqueeze(2), [128, NT, 4, 8]), op=ALU.mult))
    dv(lambda e: e.tensor_tensor(out=ohf[:], in0=ohA[:], in1=ohB[:], op=ALU.add))
    ohf2 = ohf[:].rearrange("p t x -> p (t x)")

    def mm_pos(e):
        e.matmul(bank[0][:], lhsT=tris[:], rhs=ohf2, start=True, stop=True)
        return e.matmul(bank[1][:], lhsT=ones_b[:], rhs=ohf2, start=True, stop=True)
    P.op("pe", mm_pos, reads=[r_rt], writes=[r_bank[0], r_bank[1]])
    P.op("dve", lambda e: e.tensor_copy(out=csum[:].rearrange("p t x -> p (t x)"), in_=bank[1][:]),
         reads=[r_bank[1]], writes=[r_rt])
    dv(lambda e: e.memset(carry[:, 0, :], 0.0))
    for t_ in range(NT):
        dv(lambda e, t_=t_: e.tensor_tensor(out=carry[:, t_ + 1, :], in0=carry[:, t_, :], in1=csum[:, t_, :], op=ALU.add))
    P.op("dve", lambda e: e.tensor_tensor(out=pos[:].rearrange("p t x -> p (t x)"), in0=bank[0][:],
                                          in1=carry[:, 0:NT, :].rearrange("p t x -> p (t x)"), op=ALU.add),
         reads=[r_bank[0], r_rt], writes=[r_rt])
    dv(lambda e: e.tensor_tensor(out=pos[:], in0=pos[:], in1=bc(ebase[:].unsqueeze(1), [128, NT, 32]), op=ALU.add))
    dv(lambda e: e.tensor_tensor(out=tmp[:], in0=pos[:], in1=ohA[:], op=ALU.mult))
    dv(lambda e: e.tensor_reduce(out=dstA[:], in_=tmp[:], axis=AX.X, op=ALU.add))
    dv(lambda e: e.tensor_tensor(out=tmp[:], in0=pos[:], in1=ohB[:], op=ALU.mult))
    dv(lambda e: e.tensor_reduce(out=dstB[:], in_=tmp[:], axis=AX.X, op=ALU.add))
    dv(lambda e: e.tensor_copy(out=dst_i[:, 0, :], in_=dstA[:]))
    dv(lambda e: e.tensor_copy(out=dst_i[:, 1, :], in_=dstB[:]))
    dv(lambda e: e.tensor_copy(out=cnt_i[:], in_=carry[:, NT, :]))
    dv(lambda e: e.tensor_reduce(out=gmax_f[:, 0:1], in_=carry[:, NT, :], axis=AX.X, op=ALU.max))
    dv(lambda e: e.tensor_copy(out=gmax_i[:, 0:1], in_=gmax_f[:, 0:1]))
    if DEBUG:
        dv(lambda e: e.tensor_tensor(out=comb[:], in0=ohA[:], in1=bc(w1[:], [128, NT, 32]), op=ALU.mult))
        dv(lambda e: e.scalar_tensor_tensor(out=tmp[:], in0=ohB[:], scalar=1.0, in1=bc(w2[:], [128, NT, 32]), op0=ALU.mult, op1=ALU.mult))
        dv(lambda e: e.tensor_tensor(out=comb[:], in0=comb[:], in1=tmp[:], op=ALU.add))
        last.append(P.dma("sp", dbg["x1"].rearrange("(t p) d -> p t d", p=128), x1[:], reads=r_x1))
        last.append(P.dma("sp", dbg["comb"].rearrange("(t p) c -> p t c", p=128), comb[:], reads=[r_rt]))
    if stop_after == "E":
        P.barrier()
        P.build(last + P.gdeps)
        return nc

    CAP = NOWN
    Gx = nc.dram_tensor("Gx", [NE * CAP, D], BF16).ap()
    Yg = nc.dram_tensor("Yg", [NE * CAP, D], F32).ap()
    r_Gx, r_Yg = Res(), Res()
    for ot in range(16):
        for s_ in range(2):
            P.idma(Gx[:, :], dst_i[:, s_, ot:ot + 1], h2rows[:, ot, :], None, reads=[r_rt, r_h2r[ot]])
    P.barrier()
    if stop_after == "F0":
        P.build(last + P.gdeps)
        return nc
    F0 = 32 * KB
    Xg = [sb(f"Xg{i}", [128, D], BF16, F0 + i * 2 * KB) for i in range(3)]
    XgT = [sb(f"XgT{i}", [128, 8, 128], BF16, F0 + 6 * KB + i * 2 * KB) for i in range(2)]
    sgs = [sb(f"sgs{i}", [128, DE], F32, F0 + 10 * KB + i * KB) for i in range(2)]
    actb = [sb(f"actb{i}", [128, DE], BF16, F0 + 12 * KB + i * 512) for i in range(2)]
    actT = [sb(f"actT{i}", [128, 2, 128], BF16, F0 + 13 * KB + i * 512) for i in range(2)]
    Ys = [sb(f"Ys{i}", [128, D], F32, 144 * KB + i * 4 * KB) for i in range(2)]
    E0 = 48 * KB
    XgX = sb("XgX", [128, D], BF16, E0)
    XgTX = sb("XgTX", [128, 8, 128], BF16, E0 + 2 * KB)
    sgsX = sb("sgsX", [128, DE], F32, E0 + 4 * KB)
    actbX = sb("actbX", [128, DE], BF16, E0 + 5 * KB)
    actTX = sb("actTX", [128, 2, 128], BF16, E0 + 5 * KB + 512)
    r_Xg = [Res() for _ in range(3)]
    r_XgT, r_sgs, r_actb, r_actT, r_Ys = ([Res(), Res()] for _ in range(5))
    r_XgX, r_XgTX, r_sgsX, r_actbX, r_actTX = (Res() for _ in range(5))
    ys_i = [0]

    def stage_x(xg, r_xg, xgt, r_xgt, pbank):
        pT = bank_bf(pbank)

        def tr_x(e):
            for k in range(8):
                ins = e.transpose(out=pT[:, k * 128:(k + 1) * 128], in_=xg[:, k * 128:(k + 1) * 128], identity=ident[:])
            return ins
        P.op("pe", tr_x, reads=[r_xg, r_const], writes=[r_bank[pbank]])
        P.op("dve", lambda e: e.tensor_copy(out=xgt[:], in_=pT.rearrange("p (k t) -> p k t", k=8)),
             reads=[r_bank[pbank]], writes=[r_xgt])

    def stage_gu(ex, xgt, r_xgt, sg_, r_sg, ab_, r_ab, pbank):
        s_ = ex % NR

        def mm_gu(e):
            for k in range(8):
                ins = e.matmul(bank[pbank][:], lhsT=xgt[:, k, :], rhs=wgu[s_][:, k, :], start=(k == 0), stop=(k == 7))
            return ins
        P.op("pe", mm_gu, reads=[r_xgt, r_wgu[s_]], writes=[r_bank[pbank]])
        P.op("act", lambda e: e.activation(out=sg_[:], in_=bank[pbank][:, 0:DE], func=AF.Silu),
             reads=[r_bank[pbank]], writes=[r_sg])
        P.op("dve", lambda e: e.tensor_tensor(out=ab_[:], in0=sg_[:], in1=bank[pbank][:, DE:2 * DE], op=ALU.mult),
             reads=[r_sg, r_bank[pbank]], writes=[r_ab])

    def stage_d(ex, row0, ab_, r_ab, at_, r_at):
        s_ = ex % NR
        pA = bank_bf(4)

        def tr_a(e):
            for f in range(2):
                ins = e.transpose(out=pA[:, f * 128:(f + 1) * 128], in_=ab_[:, f * 128:(f + 1) * 128], identity=ident[:])
            return ins
        P.op("pe", tr_a, reads=[r_ab, r_const], writes=[r_bank[4]])
        P.op("dve", lambda e: e.tensor_copy(out=at_[:], in_=pA[:, 0:256].rearrange("p (f t) -> p f t", f=2)),
             reads=[r_bank[4]], writes=[r_at])

        def mm_d(e):
            for half in range(2):
                for f in range(2):
                    ins = e.matmul(bank[5 + half][:], lhsT=at_[:, f, :], rhs=wde[s_][:, f, half * 512:(half + 1) * 512],
                                   start=(f == 0), stop=(f == 1))
            return ins
        P.op("pe", mm_d, reads=[r_at, r_wde[s_]], writes=[r_bank[5], r_bank[6]])
        yb = ys_i[0] % 2
        ys_i[0] += 1
        P.op("dve", lambda e: e.tensor_copy(out=Ys[yb][:, 0:512], in_=bank[5][:]), reads=[r_bank[5]], writes=[r_Ys[yb]])
        P.op("dve", lambda e: e.tensor_copy(out=Ys[yb][:, 512:1024], in_=bank[6][:]), reads=[r_bank[6]], writes=[r_Ys[yb]])
        P.dma("sp", Yg[row0:row0 + 128, :], Ys[yb][:], reads=[r_Ys[yb]])

    ST = STATIC_TILES

    def load_x(u):
        ex, half = u // ST, u % ST
        r0 = ex * CAP + half * 128
        P.dma("act", Xg[u % 3][:], Gx[r0:r0 + 128, :], reads=[r_Gx], writes=[r_Xg[u % 3]])

    NU = ST * NE
    load_x(0)
    for it in range(NU + 2):
        if it + 1 < NU:
            load_x(it + 1)
        if it < NU:
            u = it
            stage_x(Xg[u % 3], r_Xg[u % 3], XgT[u % 2], r_XgT[u % 2], u % 2)
        if 0 <= it - 2 < NU:
            u = it - 2
            stage_d(u // ST, (u // ST) * CAP + (u % ST) * 128, actb[u % 2], r_actb[u % 2], actT[u % 2], r_actT[u % 2])
        if 0 <= it - 1 < NU:
            u = it - 1
            stage_gu(u // ST, XgT[u % 2], r_XgT[u % 2], sgs[u % 2], r_sgs[u % 2], actb[u % 2], r_actb[u % 2], 2 + u % 2)
        if it >= ST + 1 and (it - ST - 1) % ST == 0:
            exn = (it - ST - 1) // ST + NR
            if exn < NE:
                load_gu(exn)
                load_d(exn)

    cdeps = [r_rt.w]
    for g in range(1):
        P.begin_group(gmax_i[0:1, 0:1], cdeps, 128 * ST + 1)
        for ex in range(NE):
            P.begin_expert(cnt_i[0:1, ex:ex + 1], cdeps, k0=ST)
            for kt in range(ST, CAP // 128):
                P.begin_tile()
                if kt == ST:
                    load_gu(ex)
                    load_d(ex)
                row0 = ex * CAP + kt * 128
                P.dma("sp", XgX[:], Gx[row0:row0 + 128, :], reads=[r_Gx], writes=[r_XgX])
                stage_x(XgX, r_XgX, XgTX, r_XgTX, 7)
                stage_gu(ex, XgTX, r_XgTX, sgsX, r_sgsX, actbX, r_actbX, 7)
                stage_d(ex, row0, actbX, r_actbX, actTX, r_actTX)
            P.end_expert()
        P.end_group()

    P.barrier()
    if stop_after == "F1":
        P.build(last + P.gdeps)
        return nc
    NG = 8
    gbuf = [sb(f"gbuf{i}", [128, D], F32, i * 4 * KB) for i in range(NG)]
    r_gb = [Res() for _ in range(NG)]
    gi = 0
    for ot in range(16):
        for s_ in range(2):
            g_ = gbuf[gi % NG]
            r_g = r_gb[gi % NG]
            gi += 1
            P.idma(g_[:], None, Yg[:, :], dst_i[:, s_, ot:ot + 1], reads=[r_Yg, r_rt], writes=[r_g])
            wsc = (w1 if s_ == 0 else w2)
            P.op("dve", lambda e, g_=g_, ot=ot, wsc=wsc: e.scalar_tensor_tensor(
                out=x1[:, ot, :], in0=g_[:], scalar=wsc[:, ot, :], in1=x1[:, ot, :], op0=ALU.mult, op1=ALU.add),
                reads=[r_g, r_rt], writes=[r_x1[ot]])
        last.append(P.dma("sp", out_d[ot * 128:(ot + 1) * 128, :], x1[:, ot, :], reads=[r_x1[ot]]))
    P.barrier()
    P.build(last + P.gdeps)
    return nc


def make_inputs(core, inputs):
    b = core // 2
    chunks = CHUNKS[core % 2]
    x = np.asarray(inputs["x"], dtype=np.float32)
    xb = x[b]
    xo = np.concatenate([xb[c * 512:(c + 1) * 512] for c in chunks], axis=0)
    xh = np.zeros((128, D), np.float32)
    for j, c in enumerate(chunks):
        if c > 0:
            xh[2 * j:2 * j + 2] = xb[c * 512 - 2:c * 512]
    qpos = np.concatenate([np.arange(c * 512, (c + 1) * 512) for c in chunks]).astype(np.float32)[None, :]
    kpos = (np.arange(32)[None, :] * 128 + np.arange(128)[:, None]).astype(np.float32)
    negU = -(np.arange(128)[:, None] >= np.arange(128)[None, :]).astype(np.float32)
    cst = np.concatenate([negU, -np.ones((128, 128), np.float32)], axis=1)
    ebase = (np.arange(NE) * NOWN).astype(np.float32)[None, :]
    tris = (np.arange(128)[:, None] < np.arange(128)[None, :]).astype(np.float32)
    f = lambda k: np.ascontiguousarray(np.asarray(inputs[k], dtype=np.float32)[0])
    wr = np.concatenate([f("w_router_group"), f("w_router_expert")], axis=1)
    br = np.concatenate([f("b_router_group"), f("b_router_expert")])[None, :]
    return {
        "tflag": np.array([[1 - core % 2, core % 2]], dtype=np.int32),
        "xf": np.ascontiguousarray(xb), "xo": np.ascontiguousarray(xo), "xh": xh, "qpos": qpos, "kpos": kpos,
        "w_in": f("w_in"), "g1": f("norm_mix_g")[None, :], "gq": f("q_norm_g")[None, :], "gk": f("k_norm_g")[None, :],
        "convw": f("conv_w"), "wsb": f("w_sb_branch"), "wcb": f("w_conv_branch"), "wout": f("w_out"),
        "g2": f("norm_ffn_g")[None, :], "wr": np.ascontiguousarray(wr), "br": np.ascontiguousarray(br),
        "wge": f("w_gate_e"), "wue": f("w_up_e"), "wde": f("w_down_e"),
        "ident": np.eye(128, dtype=np.float32), "cst": cst, "ebase": ebase, "tris": tris,
    }


def kernel(**inputs):
    nc = build_program()
    in_maps = [make_inputs(c, inputs) for c in range(8)]
    res = run_bass_kernel_spmd(nc, in_maps, core_ids=list(range(8)))
    out = np.zeros((4, SEQ, D), np.float32)
    for c in range(8):
        b = c // 2
        o = res.results[c]["out"]
        for j, ch in enumerate(CHUNKS[c % 2]):
            out[b, ch * 512:(ch + 1) * 512] = o[j * 512:(j + 1) * 512]
    return out
```
